# Optimizing a Trainium2 kernel written in Bass

```python
import math
import jax, jax.numpy as jnp
from jax import lax
import numpy as np

D_MODEL = 1024
BATCH = 8
SEQ = 8192
DEPTH = 1

MLA_HEADS = 8
Q_LORA = 256
KV_LORA = 128
QK_NOPE = 64
QK_ROPE = 32
V_HEAD = 64
QK_HEAD = QK_NOPE + QK_ROPE
ROPE_THETA = 10000.0
Q_BLOCK = 128
M_HEADS = 4
M_HEAD = 128
M_WIDTH = M_HEADS * M_HEAD
CONV_K = 5
CHUNK = 128
FFN_DIM = 2816
EPS = 1e-6
IN_SPLITS = (Q_LORA, KV_LORA, QK_ROPE, M_WIDTH, M_WIDTH, 4 * M_HEADS, D_MODEL, D_MODEL)
IN_DIM = Q_LORA + KV_LORA + QK_ROPE + 2 * M_WIDTH + 4 * M_HEADS + 2 * D_MODEL

kernel_name = "hybrid_mla_mlstm_macaron_block"


def rmsnorm(x, w):
    xf = x.astype(jnp.float32)
    y = xf * lax.rsqrt(jnp.mean(xf * xf, axis=-1, keepdims=True) + EPS)
    return (y * w.astype(jnp.float32)).astype(x.dtype)


def swiglu(x, w_gate, w_up, w_down):
    return (jax.nn.silu(x @ w_gate) * (x @ w_up)) @ w_down


def rope_tables(positions):
    half = QK_ROPE // 2
    inv = ROPE_THETA ** (-jnp.arange(half, dtype=jnp.float32) / half)
    ang = positions.astype(jnp.float32)[..., None] * inv
    return jnp.cos(ang)[:, :, None, :], jnp.sin(ang)[:, :, None, :]


def apply_rope(x, cos, sin):
    half = QK_ROPE // 2
    x1, x2 = x[..., :half], x[..., half:]
    c, s = cos.astype(x.dtype), sin.astype(x.dtype)
    return jnp.concatenate([x1 * c - x2 * s, x2 * c + x1 * s], axis=-1)


def block_attention(q, k, v):
    B, H, S, dq = q.shape
    nb = S // Q_BLOCK
    qb = q.reshape(B, H, nb, Q_BLOCK, dq).transpose(2, 0, 1, 3, 4)
    scale = QK_HEAD ** -0.5

    def one(qblk):
        s = jnp.einsum('bhqd,bhkd->bhqk', qblk, k).astype(jnp.float32) * scale
        p = jax.nn.softmax(s, axis=-1)
        return jnp.einsum('bhqk,bhkd->bhqd', p.astype(v.dtype), v)

    o = lax.map(one, qb)
    return o.transpose(1, 0, 3, 2, 4).reshape(B, S, H * v.shape[-1])


def mla_branch(c_q, c_kv, k_pe, positions, q_a_norm_w, w_uq, kv_a_norm_w, w_uk, w_uv,
               q_norm_w, k_norm_w):
    B, S, _ = c_q.shape
    q = (rmsnorm(c_q, q_a_norm_w) @ w_uq).reshape(B, S, MLA_HEADS, QK_HEAD)
    ckv = rmsnorm(c_kv, kv_a_norm_w)
    k_nope = (ckv @ w_uk).reshape(B, S, MLA_HEADS, QK_NOPE)
    v = (ckv @ w_uv).reshape(B, S, MLA_HEADS, V_HEAD)
    k = jnp.concatenate(
        [k_nope, jnp.broadcast_to(k_pe[:, :, None, :], (B, S, MLA_HEADS, QK_ROPE))], axis=-1)
    q = rmsnorm(q, q_norm_w)
    k = rmsnorm(k, k_norm_w)
    cos, sin = rope_tables(positions)
    q = jnp.concatenate([q[..., :QK_NOPE], apply_rope(q[..., QK_NOPE:], cos, sin)], axis=-1)
    k = jnp.concatenate([k[..., :QK_NOPE], apply_rope(k[..., QK_NOPE:], cos, sin)], axis=-1)
    return block_attention(q.transpose(0, 2, 1, 3), k.transpose(0, 2, 1, 3),
                           v.transpose(0, 2, 1, 3))


def centred_conv(x, w, b):
    C = x.shape[-1]
    y = lax.conv_general_dilated(
        x, w.astype(x.dtype)[:, None, :], window_strides=(1,),
        padding=[(CONV_K // 2, CONV_K // 2)], dimension_numbers=('NWC', 'WIO', 'NWC'),
        feature_group_count=C)
    return y + b


def mlstm_scan(q, k, v, i_pre, logf):
    B, H, S, dk = q.shape
    dv = v.shape[-1]
    nc = S // CHUNK
    to_chunks = lambda t: jnp.moveaxis(t.reshape(B, H, nc, CHUNK, *t.shape[3:]), 2, 0)
    mask = jnp.tril(jnp.ones((CHUNK, CHUNK), dtype=bool))

    def step(carry, inp):
        C, n, m = carry
        qc, kc, vc, ic, fc = inp
        b = jnp.cumsum(fc, axis=-1)
        logw = b[..., :, None] - b[..., None, :] + ic[..., None, :]
        logw = jnp.where(mask, logw, -jnp.inf)
        inter = b + m[..., None]
        m_t = jnp.maximum(jnp.max(logw, axis=-1), inter)
        sc = jnp.einsum('bhtd,bhsd->bhts', qc, kc) * jnp.exp(logw - m_t[..., None])
        inter_w = jnp.exp(inter - m_t)
        num = jnp.einsum('bhts,bhsv->bhtv', sc, vc) + inter_w[..., None] * jnp.einsum('bhtk,bhkv->bhtv', qc, C)
        den = jnp.sum(sc, axis=-1) + inter_w * jnp.einsum('bhtk,bhk->bht', qc, n)
        h = num / jnp.maximum(jnp.abs(den), jnp.exp(-m_t))[..., None]
        bL = b[..., -1]
        g = bL[..., None] - b + ic
        m_new = jnp.maximum(bL + m, jnp.max(g, axis=-1))
        decay = jnp.exp(bL + m - m_new)
        ws = jnp.exp(g - m_new[..., None])
        C_new = decay[..., None, None] * C + jnp.einsum('bhs,bhsk,bhsv->bhkv', ws, kc, vc)
        n_new = decay[..., None] * n + jnp.einsum('bhs,bhsk->bhk', ws, kc)
        return (C_new, n_new, m_new), h

    init = (jnp.zeros((B, H, dk, dv), jnp.float32), jnp.zeros((B, H, dk), jnp.float32),
            jnp.zeros((B, H), jnp.float32))
    _, hs = lax.scan(step, init, (to_chunks(q), to_chunks(k), to_chunks(v),
                                  to_chunks(i_pre), to_chunks(logf)))
    return jnp.moveaxis(hs, 0, 2).reshape(B, H, S, dv)


def mlstm_branch(m_in, o_pre, gates, conv_w, conv_b, w_mq, w_mk, w_mv, b_igate, b_fgate,
                 m_norm_w, m_skip):
    B, S, _ = m_in.shape
    xc = jax.nn.silu(centred_conv(m_in, conv_w, conv_b))
    xch = xc.reshape(B, S, M_HEADS, M_HEAD)
    xmh = m_in.reshape(B, S, M_HEADS, M_HEAD)
    f32 = jnp.float32
    q = jnp.einsum('bshd,hde->bhse', xch, w_mq).astype(f32)
    k = (jnp.einsum('bshd,hde->bhse', xch, w_mk) * (M_HEAD ** -0.5)).astype(f32)
    v = jnp.einsum('bshd,hde->bhse', xmh, w_mv).astype(f32)
    gf = gates.astype(f32)
    i_pre = (gf[..., :2 * M_HEADS] + b_igate.astype(f32)).transpose(0, 2, 1)
    logf = jax.nn.log_sigmoid(gf[..., 2 * M_HEADS:] + b_fgate.astype(f32)).transpose(0, 2, 1)
    flip = lambda t: jnp.flip(t, axis=2)
    h_fwd = mlstm_scan(q, k, v, i_pre[:, :M_HEADS], logf[:, :M_HEADS])
    h_bwd = flip(mlstm_scan(flip(q), flip(k), flip(v), flip(i_pre[:, M_HEADS:]),
                            flip(logf[:, M_HEADS:])))
    h_cell = (h_fwd + h_bwd).transpose(0, 2, 1, 3)
    hn = rmsnorm(h_cell, m_norm_w.reshape(M_HEADS, M_HEAD)).reshape(B, S, M_WIDTH)
    return jax.nn.sigmoid(o_pre) * (hn.astype(m_in.dtype) + m_skip * xc)


def setup_inputs(seed: int = 0) -> dict:
    key = jax.random.key(seed)
    ks = iter(jax.random.split(key, 48))
    f32 = jnp.float32
    nrm = lambda shape, fan_in: jax.random.normal(next(ks), shape, f32) * fan_in ** -0.5
    gain = lambda shape: 1.0 + 0.02 * jax.random.normal(next(ks), shape, f32)
    L = DEPTH
    x = jax.random.normal(next(ks), (BATCH, SEQ, D_MODEL), f32)
    offs = jax.random.randint(next(ks), (BATCH, 1), 0, 1024, dtype=jnp.int32)
    positions = jnp.arange(SEQ, dtype=jnp.int32)[None, :] + offs
    f_bias = jnp.tile(jnp.linspace(3.0, 6.0, M_HEADS, dtype=f32), 2)[None, :]
    return {
        "x": x,
        "positions": positions,
        "ffn1_norm_w": gain((L, D_MODEL)),
        "ffn1_w_gate": nrm((L, D_MODEL, FFN_DIM), D_MODEL),
        "ffn1_w_up": nrm((L, D_MODEL, FFN_DIM), D_MODEL),
        "ffn1_w_down": nrm((L, FFN_DIM, D_MODEL), FFN_DIM),
        "mix_norm_w": gain((L, D_MODEL)),
        "w_in": nrm((L, D_MODEL, IN_DIM), D_MODEL),
        "q_a_norm_w": gain((L, Q_LORA)),
        "w_uq": nrm((L, Q_LORA, MLA_HEADS * QK_HEAD), Q_LORA),
        "kv_a_norm_w": gain((L, KV_LORA)),
        "w_uk": nrm((L, KV_LORA, MLA_HEADS * QK_NOPE), KV_LORA),
        "w_uv": nrm((L, KV_LORA, MLA_HEADS * V_HEAD), KV_LORA),
        "q_norm_w": gain((L, QK_HEAD)),
        "k_norm_w": gain((L, QK_HEAD)),
        "w_branch_a": nrm((L, MLA_HEADS * V_HEAD, D_MODEL), MLA_HEADS * V_HEAD),
        "conv_w": nrm((L, CONV_K, M_WIDTH), CONV_K),
        "conv_b": 0.02 * jax.random.normal(next(ks), (L, M_WIDTH), f32),
        "w_mq": nrm((L, M_HEADS, M_HEAD, M_HEAD), M_HEAD),
        "w_mk": nrm((L, M_HEADS, M_HEAD, M_HEAD), M_HEAD),
        "w_mv": nrm((L, M_HEADS, M_HEAD, M_HEAD), M_HEAD),
        "b_igate": 0.1 * jax.random.normal(next(ks), (L, 2 * M_HEADS), f32),
        "b_fgate": f_bias + 0.1 * jax.random.normal(next(ks), (L, 2 * M_HEADS), f32),
        "m_norm_w": gain((L, M_WIDTH)),
        "m_skip": gain((L, M_WIDTH)),
        "w_branch_b": nrm((L, M_WIDTH, D_MODEL), M_WIDTH),
        "w_out": nrm((L, D_MODEL, D_MODEL), D_MODEL),
        "ffn2_norm_w": gain((L, D_MODEL)),
        "ffn2_w_gate": nrm((L, D_MODEL, FFN_DIM), D_MODEL),
        "ffn2_w_up": nrm((L, D_MODEL, FFN_DIM), D_MODEL),
        "ffn2_w_down": nrm((L, FFN_DIM, D_MODEL), FFN_DIM),
        "final_norm_w": gain((L, D_MODEL)),
    }


def reference(x, positions, ffn1_norm_w, ffn1_w_gate, ffn1_w_up, ffn1_w_down, mix_norm_w, w_in,
              q_a_norm_w, w_uq, kv_a_norm_w, w_uk, w_uv, q_norm_w, k_norm_w, w_branch_a,
              conv_w, conv_b, w_mq, w_mk, w_mv, b_igate, b_fgate, m_norm_w, m_skip, w_branch_b,
              w_out, ffn2_norm_w, ffn2_w_gate, ffn2_w_up, ffn2_w_down, final_norm_w):
    split_points = [sum(IN_SPLITS[:j + 1]) for j in range(len(IN_SPLITS) - 1)]
    for l in range(DEPTH):
        h = rmsnorm(x, ffn1_norm_w[l])
        x = x + 0.5 * swiglu(h, ffn1_w_gate[l], ffn1_w_up[l], ffn1_w_down[l])
        h = rmsnorm(x, mix_norm_w[l])
        p = h @ w_in[l]
        c_q, c_kv, k_pe, m_in, o_pre, gates, g_a, g_b = jnp.split(p, split_points, axis=-1)
        a = mla_branch(c_q, c_kv, k_pe, positions, q_a_norm_w[l], w_uq[l], kv_a_norm_w[l],
                       w_uk[l], w_uv[l], q_norm_w[l], k_norm_w[l])
        bm = mlstm_branch(m_in, o_pre, gates, conv_w[l], conv_b[l], w_mq[l], w_mk[l], w_mv[l],
                          b_igate[l], b_fgate[l], m_norm_w[l], m_skip[l])
        merged = jax.nn.sigmoid(g_a) * (a @ w_branch_a[l]) + jax.nn.sigmoid(g_b) * (bm @ w_branch_b[l])
        x = x + merged @ w_out[l]
        h = rmsnorm(x, ffn2_norm_w[l])
        x = x + 0.5 * swiglu(h, ffn2_w_gate[l], ffn2_w_up[l], ffn2_w_down[l])
        x = rmsnorm(x, final_norm_w[l])
    return x
```

```python
import math
from contextlib import ExitStack
from collections import defaultdict

import numpy as np
import ml_dtypes
import concourse.bass as bass
import concourse.mybir as mybir
from concourse.bass_utils import run_bass_kernel_spmd

F32 = mybir.dt.float32
BF16 = mybir.dt.bfloat16
I32 = mybir.dt.int32
AF = mybir.ActivationFunctionType
ALU = mybir.AluOpType
AX = mybir.AxisListType

S = 8192
D = 1024
FF = 2816
NHC = FF // 128
T = 512
NT = S // T
NCH = S // 128
IN_DIM = 3504
EPS = 1e-6
ATT_SCALE = 96 ** -0.5


class _Op:
    __slots__ = ("eng", "fn", "deps", "dma", "prev")

    def __init__(self, eng, fn, deps, dma):
        self.eng, self.fn, self.deps, self.dma, self.prev = eng, fn, deps, dma, None


class Prog:
    DMAQ = {"sp": 8, "pool": 4}

    def __init__(self, nc, sems):
        self.nc = nc
        self.sems = sems
        self.next_sem = 0
        self.dma_sems = {q: [self._alloc() for _ in range(k)] for q, k in self.DMAQ.items()}
        self.dma_cnt = {q: [0] * k for q, k in self.DMAQ.items()}
        self.dma_last = {q: [None] * k for q, k in self.DMAQ.items()}
        self.dma_n = {q: 0 for q in self.DMAQ}
        self._reset()

    def _alloc(self):
        i = self.next_sem
        self.next_sem += 1
        assert i < len(self.sems), "out of semaphores"
        return i

    def _reset(self):
        self.ops = []
        self.lastw = {}
        self.readers = defaultdict(list)
        self.fence_deps = set()
        self.fenced = set()

    def fence(self):
        self.fence_deps = set(range(len(self.ops)))
        self.fenced = set()

    def op(self, eng, fn, r=(), w=(), dma=False):
        idx = len(self.ops)
        deps = set()
        for k in r:
            if k in self.lastw:
                deps.add(self.lastw[k])
        for k in w:
            if k in self.lastw:
                deps.add(self.lastw[k])
            deps.update(self.readers.get(k, ()))
        for k in w:
            self.lastw[k] = idx
            self.readers[k] = []
        for k in r:
            self.readers[k].append(idx)
        if self.fence_deps and eng not in self.fenced:
            deps |= self.fence_deps
            self.fenced.add(eng)
        deps.discard(idx)
        self.ops.append(_Op(eng, fn, deps, dma))
        return idx

    def pe(self, fn, r=(), w=()):
        return self.op("pe", fn, r, w)

    def act(self, fn, r=(), w=()):
        return self.op("act", fn, r, w)

    def dve(self, fn, r=(), w=()):
        return self.op("dve", fn, r, w)

    def pool(self, fn, r=(), w=()):
        return self.op("pool", fn, r, w)

    def dma(self, q, out, in_, r=(), w=(), **kw):
        return self.op(q, lambda e, out=out, in_=in_, kw=kw: e.dma_start(out=out, in_=in_, **kw), r, w, dma=True)

    def flush(self):
        nc = self.nc
        ops = self.ops
        n = len(ops)
        signal = [False] * n

        def skip(p, c):
            return p.eng == "pe" and c.eng == "pe" and not p.dma and not c.dma

        for o in ops:
            for d in o.deps:
                if not skip(ops[d], o):
                    signal[d] = True
        csem = {}
        ccnt = {}
        ticket = [None] * n
        per_eng = {e: [] for e in ("pe", "act", "dve", "pool", "sp")}
        for i, o in enumerate(ops):
            per_eng[o.eng].append(i)
            if o.dma:
                q = o.eng
                K = len(self.dma_sems[q])
                j = self.dma_n[q] % K
                self.dma_n[q] += 1
                o.prev = self.dma_last[q][j]
                self.dma_cnt[q][j] += 1
                ticket[i] = (self.dma_sems[q][j], 16 * self.dma_cnt[q][j])
                self.dma_last[q][j] = ticket[i]
            elif signal[i]:
                e = o.eng
                if e not in csem or ccnt[e] >= 30000:
                    csem[e] = self._alloc()
                    ccnt[e] = 0
                ccnt[e] += 1
                ticket[i] = (csem[e], ccnt[e])
        sems = self.sems

        def emit(engname, eng):
            waited = {}
            for i in per_eng[engname]:
                o = ops[i]
                need = {}
                for d in o.deps:
                    if skip(ops[d], o):
                        continue
                    s, v = ticket[d]
                    if need.get(s, 0) < v:
                        need[s] = v
                if o.prev is not None:
                    s, v = o.prev
                    if need.get(s, 0) < v:
                        need[s] = v
                for s, v in need.items():
                    if waited.get(s, 0) < v:
                        eng.wait_ge(sems[s], v)
                        waited[s] = v
                inst = o.fn(eng)
                if ticket[i] is not None:
                    inst.then_inc(sems[ticket[i][0]], 16 if o.dma else 1)
            if engname in self.dma_last:
                for t in self.dma_last[engname]:
                    if t is not None and waited.get(t[0], 0) < t[1]:
                        eng.wait_ge(sems[t[0]], t[1])

        with nc.Block() as blk:
            if per_eng["pe"]:
                @blk.tensor
                def _(e):
                    emit("pe", e)
            if per_eng["act"]:
                @blk.scalar
                def _(e):
                    emit("act", e)
            if per_eng["dve"]:
                @blk.vector
                def _(e):
                    emit("dve", e)
            if per_eng["pool"]:
                @blk.gpsimd
                def _(e):
                    emit("pool", e)
            if per_eng["sp"]:
                @blk.sync
                def _(e):
                    emit("sp", e)
        self._reset()


class Rot:
    def __init__(self, alloc, name, n, shape, dt):
        self.t = [alloc(f"{name}{i}", shape, dt) for i in range(n)]
        self.name = name
        self.i = -1

    def next(self):
        self.i += 1
        j = self.i % len(self.t)
        return self.t[j], (self.name, j)


USE_POOL_POW = False


def rstd_pool(P, out_ap, in_ap, n, mh_ap, r, w):
    if not USE_POOL_POW:
        P.act(lambda e: e.activation(out=out_ap, in_=in_ap, func=AF.Sqrt, scale=1.0 / n, bias=EPS), r=r, w=w)
        P.dve(lambda e: e.reciprocal(out=out_ap, in_=out_ap), r=w, w=w)
        return
    P.pool(lambda e: e.tensor_scalar(out=out_ap, in0=in_ap, scalar1=1.0 / n, scalar2=EPS, op0=ALU.mult, op1=ALU.add),
           r=r, w=w)
    P.pool(lambda e: e.tensor_tensor(out=out_ap, in0=out_ap, in1=mh_ap, op=ALU.pow), r=w, w=w)


def bc(ap2d, shape, axis):
    return ap2d.unsqueeze(axis).to_broadcast(list(shape))


def emit_norm_T(P, x_ap, x_key, nw, hn, hn_key, ss, rs, col, epsb, ident, pT, pT_key, hT_view, hT_key,
                evac="act", n=D):
    nk = n // 128
    P.act(lambda e: e.activation(out=hn[:, 0:n], in_=x_ap, func=AF.Square, accum_out=ss[:, col:col + 1]),
          r=[x_key], w=[hn_key, ("ss", col)])
    P.act(lambda e: e.activation(out=rs[:, col:col + 1], in_=ss[:, col:col + 1], func=AF.Sqrt,
                                 scale=1.0 / n, bias=epsb[:, 0:1]),
          r=[("ss", col)], w=[("rs", col)])
    P.dve(lambda e: e.reciprocal(out=rs[:, col:col + 1], in_=rs[:, col:col + 1]),
          r=[("rs", col)], w=[("rs", col)])
    P.dve(lambda e: e.scalar_tensor_tensor(out=hn[:, 0:n], in0=x_ap, scalar=rs[:, col:col + 1], in1=nw,
                                           op0=ALU.mult, op1=ALU.mult),
          r=[x_key, ("rs", col)], w=[hn_key])

    def tr(e):
        inst = None
        for k in range(nk):
            inst = e.transpose(pT[:, k * 128:(k + 1) * 128], hn[:, k * 128:(k + 1) * 128], ident[:])
        return inst

    P.pe(tr, r=[hn_key], w=[pT_key])
    src = pT[:, 0:n].rearrange("p (k t) -> p k t", k=nk)
    if evac == "act":
        P.act(lambda e: e.copy(out=hT_view, in_=src), r=[pT_key], w=[hT_key])
    else:
        P.dve(lambda e: e.tensor_copy(out=hT_view, in_=src), r=[pT_key], w=[hT_key])


def load_consts(P, A, cins, need_cf=False):
    ident = A("ident", [128, 128], BF16)
    cf = A("cf32", [128, 512], F32) if need_cf else None
    epsb = A("mhalf", [128, 16], F32)
    P.dma("sp", ident[:], cins["c_ident"], w=["ident"])
    if need_cf:
        P.dma("sp", cf[:], cins["c_f32"], w=["cf"])
    P.dve(lambda e: e.memset(epsb[:], -0.5), w=["epsb"])
    return ident, cf, epsb


def phase_ffn(P, nc, name, src, dst, norm_w, wg, wu, wd, cins, final_w=None):
    with ExitStack() as st:
        def A(nm, shape, dt):
            return st.enter_context(nc.sbuf_tensor(f"{name}_{nm}", shape, dt))

        def PS(nm, shape, dt):
            return st.enter_context(nc.psum_tensor(f"{name}_{nm}", shape, dt))

        wgt = A("wg", [128, 8, FF], BF16)
        wut = A("wu", [128, 8, FF], BF16)
        wdt = A("wd", [128, NHC, D], BF16)
        xt = [A(f"xt{b}", [128, 4, D], F32) for b in range(2)]
        hT = A("hT", [128, 8, T], BF16)
        actb = A("act", [128, NHC, T], BF16)
        hn = A("hn", [128, D], BF16)
        sg = [A(f"sg{b}", [128, T], BF16) for b in range(2)]
        nw = A("nw", [128, D], F32)
        fw = A("fw", [128, D], F32) if final_w is not None else None
        ncol = NT * 4 * (2 if final_w is not None else 1)
        ss = A("ss", [128, ncol], F32)
        rs = A("rs", [128, ncol], F32)
        ident, cf, epsb = load_consts(P, A, cins)
        P.dve(lambda e: e.memset(epsb[:], EPS), r=["epsb"], w=["epsb"])
        pT = PS("pT", [128, D], BF16)
        pG = [PS(f"pG{b}", [128, T], F32) for b in range(2)]
        pU = [PS(f"pU{b}", [128, T], F32) for b in range(2)]
        pD = [PS(f"pD{b}", [128, T], F32) for b in range(2)]

        P.dve(lambda e: e.memset(ss[:], 0.0), w=[("ss", c) for c in range(ncol)])
        P.dma("sp", nw[:], norm_w.partition_broadcast(128), w=["nw"])
        if fw is not None:
            P.dma("sp", fw[:], final_w.partition_broadcast(128), w=["fw"])
        P.fence()
        for (wt, wsrc, key) in ((wgt, wg, "wg"), (wut, wu, "wu")):
            v = wsrc.rearrange("(k p) n -> p k n", p=128)
            for q in range(4):
                c0, c1 = q * 704, (q + 1) * 704
                P.dma("pool", wt[:, :, c0:c1], v[:, :, c0:c1], w=[(key, q)])
        vd = wd.rearrange("(k p) n -> p k n", p=128)
        for q in range(2):
            P.dma("pool", wdt[:, q * 11:(q + 1) * 11, :], vd[:, q * 11:(q + 1) * 11, :], w=[("wd", q)])

        def xview(ap, i):
            return ap[i * T:(i + 1) * T, :].rearrange("(j p) d -> p j d", p=128)

        def load(i):
            b = i % 2
            P.dma("sp", xt[b][:], xview(src, i), w=[("xt", b, j) for j in range(4)])

        def norm_sub(i, j):
            b = i % 2
            col = i * 4 + j
            emit_norm_T(P, xt[b][:, j, :], ("xt", b, j), nw[:], hn, "hn", ss, rs, col, epsb, ident, pT, "pT",
                        hT[:, :, j * 128:(j + 1) * 128], ("hT", j))

        load(0)
        for j in range(4):
            norm_sub(0, j)
        for i in range(NT):
            b = i % 2
            if i + 1 < NT:
                load(i + 1)
            for hc in range(NHC):
                q = hc * 128 // 704
                q2 = (hc * 128 + 127) // 704
                g, u = pG[hc % 2], pU[hc % 2]

                def mm_g(e, hc=hc, g=g):
                    inst = None
                    for k in range(8):
                        inst = e.matmul(g[:], lhsT=wgt[:, k, hc * 128:(hc + 1) * 128], rhs=hT[:, k, :],
                                        start=(k == 0), stop=(k == 7))
                    return inst

                def mm_u(e, hc=hc, u=u):
                    inst = None
                    for k in range(8):
                        inst = e.matmul(u[:], lhsT=wut[:, k, hc * 128:(hc + 1) * 128], rhs=hT[:, k, :],
                                        start=(k == 0), stop=(k == 7))
                    return inst

                hkeys = [("hT", j) for j in range(4)]
                P.pe(mm_g, r=hkeys + [("wg", q), ("wg", q2)], w=[("pG", hc % 2)])
                P.pe(mm_u, r=hkeys + [("wu", q), ("wu", q2)], w=[("pU", hc % 2)])
                sgb = sg[hc % 2]
                P.act(lambda e, g=g, sgb=sgb: e.activation(out=sgb[:], in_=g[:], func=AF.Silu),
                      r=[("pG", hc % 2)], w=[("sg", hc % 2)])
                P.dve(lambda e, u=u, sgb=sgb, hc=hc: e.tensor_tensor(out=actb[:, hc, :], in0=u[:], in1=sgb[:],
                                                                     op=ALU.mult),
                      r=[("pU", hc % 2), ("sg", hc % 2)], w=[("act", hc)])
            nd = 0
            for j in range(4):
                for half in range(2):
                    d_ = pD[nd % 2]
                    nd += 1

                    def mm_d(e, j=j, half=half, d_=d_):
                        inst = None
                        for hc in range(NHC):
                            inst = e.matmul(d_[:], lhsT=actb[:, hc, j * 128:(j + 1) * 128],
                                            rhs=wdt[:, hc, half * 512:(half + 1) * 512],
                                            start=(hc == 0), stop=(hc == NHC - 1))
                        return inst

                    P.pe(mm_d, r=[("act", hc) for hc in range(NHC)] + [("wd", 0), ("wd", 1)],
                         w=[("pD", (nd - 1) % 2)])
                    xs = xt[b][:, j, half * 512:(half + 1) * 512]
                    P.dve(lambda e, d_=d_, xs=xs: e.scalar_tensor_tensor(out=xs, in0=d_[:], scalar=0.5, in1=xs,
                                                                         op0=ALU.mult, op1=ALU.add),
                          r=[("pD", (nd - 1) % 2), ("xt", b, j)], w=[("xt", b, j)])
                if final_w is not None:
                    col = NT * 4 + i * 4 + j
                    xj = xt[b][:, j, :]
                    P.act(lambda e, xj=xj, col=col: e.activation(out=hn[:], in_=xj, func=AF.Square,
                                                                 accum_out=ss[:, col:col + 1]),
                          r=[("xt", b, j)], w=["hn", ("ss", col)])
                    P.act(lambda e, col=col: e.activation(out=rs[:, col:col + 1], in_=ss[:, col:col + 1],
                                                          func=AF.Sqrt, scale=1.0 / D, bias=epsb[:, 0:1]),
                          r=[("ss", col)], w=[("rs", col)])
                    P.dve(lambda e, col=col: e.reciprocal(out=rs[:, col:col + 1], in_=rs[:, col:col + 1]),
                          r=[("rs", col)], w=[("rs", col)])
                    P.dve(lambda e, xj=xj, col=col: e.scalar_tensor_tensor(out=xj, in0=xj, scalar=rs[:, col:col + 1],
                                                                           in1=fw[:], op0=ALU.mult, op1=ALU.mult),
                          r=[("xt", b, j), ("rs", col), "fw"], w=[("xt", b, j)])
                if i + 1 < NT and j >= 1:
                    for jj in ((0, 1) if j == 1 else (2,) if j == 2 else (3,)):
                        norm_sub(i + 1, jj)
            P.dma("sp", xview(dst, i), xt[b][:], r=[("xt", b, j) for j in range(4)])
        P.flush()


def phase_inproj(P, nc, x1s, pos, w, scr, cins):
    name = "ip"
    with ExitStack() as st:
        def A(nm, shape, dt):
            return st.enter_context(nc.sbuf_tensor(f"{name}_{nm}", shape, dt))

        def PS(nm, shape, dt):
            return st.enter_context(nc.psum_tensor(f"{name}_{nm}", shape, dt))

        win = A("win", [128, 8, IN_DIM], BF16)
        wuq = A("wuq", [128, 2, 768], BF16)
        wuk = A("wuk", [128, 512], BF16)
        wuv = A("wuv", [128, 512], BF16)
        xt = [A(f"xt{b}", [128, 4, D], F32) for b in range(2)]
        hT = A("hT", [128, 8, T], BF16)
        hn = A("hn", [128, D], BF16)
        nw = A("nw", [128, D], F32)
        qaw = A("qaw", [128, 384], F32)
        qkw = A("qkw", [128, 2, 96], F32)
        ss = A("ss", [128, NT * 4], F32)
        rs = A("rs", [128, NT * 4], F32)
        ss2 = A("ss2", [128, NT * 4, 2], F32)
        rs2 = A("rs2", [128, NT * 4, 2], F32)
        ss16 = A("ss16", [128, 4, 16], F32)
        rs16 = A("rs16", [128, 4, 16], F32)
        stage = Rot(A, "stg", 2, [128, 4, T], F32)
        cn = Rot(A, "cn", 2, [128, 384], BF16)
        cnT = Rot(A, "cnT", 2, [128, 3, 128], BF16)
        kpe = Rot(A, "kpe", 2, [128, 32], F32)
        gst = A("gst", [128, 4, 16], F32)
        vst = Rot(A, "vst", 2, [128, 512], BF16)
        qkf = Rot(A, "qkf", 2, [128, 16, 96], F32)
        sq = A("sq", [128, 16, 96], F32)
        rtmp = Rot(A, "rtmp", 1, [128, 4, 16, 16], F32)
        qkb = Rot(A, "qkb", 2, [128, 16, 96], BF16)
        qkTs = Rot(A, "qkTs", 1, [128, 16, T], BF16)
        posi = A("posi", [128, NCH], I32)
        posf = A("posf", [128, NCH], F32)
        inv = A("inv", [128, 16], F32)
        ang = qkf.t[0][:].rearrange("p h d -> p (h d)")[:, 0:1024].rearrange("p (c f) -> p c f", f=16)
        kf = qkf.t[1][:].rearrange("p h d -> p (h d)")[:, 0:1024].rearrange("p (c f) -> p c f", f=16)
        ki_t = A("ki", [128, NCH, 16], I32)
        ki = ki_t[:]
        cosT = A("cos", [128, NCH, 16], F32)
        sinT = A("sin", [128, NCH, 16], F32)
        hpi = A("hpi", [128, 1], F32)
        ident, cf, _mh = load_consts(P, A, cins)
        epsb = A("epsv", [128, 1], F32)
        P.dve(lambda e: e.memset(epsb[:], EPS), w=["epsv"])
        pT = PS("pT", [128, D], BF16)
        pF = [PS(f"pF{b}", [128, T], F32) for b in range(2)]
        pA = PS("pA", [128, T], F32)
        pQ = PS("pQ", [128, 1024], F32)
        pK, pV = pF[0], pF[1]
        pX = PS("pX", [128, 8 * 128], BF16)

        P.dve(lambda e: e.memset(ss[:], 0.0), w=[("ss", c) for c in range(NT * 4)])
        P.dve(lambda e: e.memset(ss2[:], 0.0), w=[("ss2", c) for c in range(NT * 4)])
        P.dve(lambda e: e.memset(hpi[:], math.pi / 2), w=["hpi"])
        P.dma("sp", nw[:], w["mix_norm_w"].partition_broadcast(128), w=["nw"])
        P.dma("sp", qaw[:, 0:256], w["q_a_norm_w"].partition_broadcast(128), w=["qaw"])
        P.dma("sp", qaw[:, 256:384], w["kv_a_norm_w"].partition_broadcast(128), w=["qaw2"])
        P.dma("sp", qkw[:, 0, :], w["q_norm_w"].partition_broadcast(128), w=["qkw0"])
        P.dma("sp", qkw[:, 1, :], w["k_norm_w"].partition_broadcast(128), w=["qkw1"])
        P.dma("sp", inv[:], cins["c_inv"].partition_broadcast(128), w=["inv"])
        P.dma("sp", posi[:], pos.rearrange("(c p) -> p c", p=128), w=["posi"], allow_slow_non_contiguous=True)
        wv = w["w_in"].rearrange("(k p) n -> p k n", p=128)
        P.dma("pool", win[:, :, 0:416], wv[:, :, 0:416], w=["winA"])
        P.dma("pool", win[:, :, 416:432], wv[:, :, 1440:1456], w=["winA2"])
        P.dma("pool", win[:, :, 432:1456], wv[:, :, 416:1440], w=[("winF", 0)])
        P.dma("pool", win[:, :, 1456:2480], wv[:, :, 1456:2480], w=[("winF", 1)])
        P.dma("pool", win[:, :, 2480:3504], wv[:, :, 2480:3504], w=[("winF", 2)])
        P.dma("pool", wuq[:], w["w_uq"].rearrange("(k p) n -> p k n", p=128), w=["wuq"])
        P.dma("pool", wuk[:], w["w_uk"], w=["wuk"])
        P.dma("pool", wuv[:], w["w_uv"], w=["wuv"])

        P.dve(lambda e: e.tensor_copy(out=posf[:], in_=posi[:]), r=["posi"], w=["posf"])
        P.dve(lambda e: e.tensor_tensor(out=ang, in0=bc(posf[:], [128, NCH, 16], 2),
                                        in1=bc(inv[:], [128, NCH, 16], 1), op=ALU.mult),
              r=["posf", "inv"], w=["ang"])
        P.dve(lambda e: e.tensor_scalar(out=kf, in0=ang, scalar1=1.0 / (2 * math.pi), scalar2=None,
                                        op0=ALU.mult), r=["ang"], w=["kf"])
        P.dve(lambda e: e.tensor_copy(out=ki, in_=kf), r=["kf"], w=["ki"])
        P.dve(lambda e: e.tensor_copy(out=kf, in_=ki), r=["ki"], w=["kf"])
        C1 = 6.28125
        C2 = 2 * math.pi - C1
        P.dve(lambda e: e.scalar_tensor_tensor(out=ang, in0=kf, scalar=-C1, in1=ang, op0=ALU.mult,
                                               op1=ALU.add), r=["kf", "ang"], w=["ang"])
        P.dve(lambda e: e.scalar_tensor_tensor(out=ang, in0=kf, scalar=-C2, in1=ang, op0=ALU.mult,
                                               op1=ALU.add), r=["kf", "ang"], w=["ang"])
        P.dve(lambda e: e.tensor_scalar(out=ang, in0=ang, scalar1=-3.1415925, scalar2=3.1415925,
                                        op0=ALU.max, op1=ALU.min), r=["ang"], w=["ang"])
        P.act(lambda e: e.activation(out=sinT[:], in_=ang, func=AF.Sin), r=["ang"], w=["sin"])
        P.dve(lambda e: e.scalar_tensor_tensor(out=kf, in0=ang, scalar=-1.0, in1=ang, op0=ALU.mult,
                                               op1=ALU.max), r=["ang"], w=["kf"])
        P.act(lambda e: e.activation(out=cosT[:], in_=kf, func=AF.Sin, scale=-1.0, bias=hpi[:]),
              r=["kf", "hpi"], w=["cos"])

        P.fence()

        def xview(ap, i):
            return ap[i * T:(i + 1) * T, :].rearrange("(j p) d -> p j d", p=128)

        def load(i):
            b = i % 2
            P.dma("sp", xt[b][:], xview(x1s, i), w=[("xt", b, j) for j in range(4)])

        def norm_sub(i, j):
            b = i % 2
            col = i * 4 + j
            emit_norm_T(P, xt[b][:, j, :], ("xt", b, j), nw[:], hn, "hn", ss, rs, col, epsb, ident, pT, "pT",
                        hT[:, :, j * 128:(j + 1) * 128], ("hT", j), evac="dve")

        fm_groups = [
            (scr["minT"], 432, None), (scr["sopT"], 944, AF.Sigmoid),
            (scr["sgT"][0:512, :], 1456, AF.Sigmoid), (scr["sgT"][512:1024, :], 1968, AF.Sigmoid),
            (scr["sgT"][1024:1536, :], 2480, AF.Sigmoid), (scr["sgT"][1536:2048, :], 2992, AF.Sigmoid),
        ]
        hkeys = [("hT", j) for j in range(4)]
        wkeys = ["winA", "winA2"] + [("winF", q) for q in range(3)]
        nf = 0
        load(0)
        for j in range(4):
            norm_sub(0, j)
        for i in range(NT):
            b = i % 2
            t0 = i * T
            if i + 1 < NT:
                load(i + 1)
            for (dram, col0, func) in fm_groups:
                stg, skey = stage.next()
                for cc in range(4):
                    pf = pF[nf % 2]
                    pkey = ("pF", nf % 2)
                    nf += 1

                    def mm_f(e, pf=pf, c0=col0 + cc * 128):
                        inst = None
                        for k in range(8):
                            inst = e.matmul(pf[:], lhsT=win[:, k, c0:c0 + 128], rhs=hT[:, k, :],
                                            start=(k == 0), stop=(k == 7))
                        return inst

                    P.pe(mm_f, r=hkeys + wkeys, w=[pkey])
                    if func is None:
                        P.dve(lambda e, pf=pf, stg=stg, cc=cc: e.tensor_copy(out=stg[:, cc, :], in_=pf[:]),
                              r=[pkey], w=[skey + (cc,)])
                    else:
                        P.act(lambda e, pf=pf, stg=stg, cc=cc, func=func: e.activation(out=stg[:, cc, :], in_=pf[:],
                                                                                      func=func),
                              r=[pkey], w=[skey + (cc,)])
                P.dma("sp", dram[:, t0:t0 + T].rearrange("(c p) t -> p c t", p=128), stg[:],
                      r=[skey + (cc,) for cc in range(4)])
            qs, qskey = qkTs.next()
            for j in range(4):
                c = i * 4 + j
                tok0 = t0 + j * 128

                def mm_a(e, j=j):
                    inst = None
                    for k in range(8):
                        inst = e.matmul(pA[:, 0:432], lhsT=hT[:, k, j * 128:(j + 1) * 128], rhs=win[:, k, 0:432],
                                        start=(k == 0), stop=(k == 7))
                    return inst

                P.pe(mm_a, r=hkeys + wkeys, w=["pA"])
                cnb, cnkey = cn.next()
                kp, kpkey = kpe.next()
                P.act(lambda e, c=c: e.activation(out=hn[:, 0:256], in_=pA[:, 0:256], func=AF.Square,
                                                  accum_out=ss2[:, c, 0:1]), r=["pA"], w=["hn", ("ss2", c)])
                P.act(lambda e, c=c: e.activation(out=hn[:, 256:384], in_=pA[:, 256:384], func=AF.Square,
                                                  accum_out=ss2[:, c, 1:2]), r=["pA"], w=["hn", ("ss2", c)])
                P.act(lambda e, c=c: e.activation(out=rs2[:, c, 0:1], in_=ss2[:, c, 0:1], func=AF.Sqrt,
                                                  scale=1.0 / 256, bias=epsb[:]), r=[("ss2", c)], w=[("rs2", c)])
                P.act(lambda e, c=c: e.activation(out=rs2[:, c, 1:2], in_=ss2[:, c, 1:2], func=AF.Sqrt,
                                                  scale=1.0 / 128, bias=epsb[:]), r=[("rs2", c)], w=[("rs2", c)])
                P.dve(lambda e, c=c: e.reciprocal(out=rs2[:, c, :], in_=rs2[:, c, :]), r=[("rs2", c)],
                      w=[("rs2", c)])
                P.dve(lambda e, c=c, cnb=cnb: e.scalar_tensor_tensor(out=cnb[:, 0:256], in0=pA[:, 0:256],
                                                                     scalar=rs2[:, c, 0:1], in1=qaw[:, 0:256],
                                                                     op0=ALU.mult, op1=ALU.mult),
                      r=["pA", ("rs2", c), "qaw"], w=[cnkey + (0,)])
                P.dve(lambda e, c=c, cnb=cnb: e.scalar_tensor_tensor(out=cnb[:, 256:384], in0=pA[:, 256:384],
                                                                     scalar=rs2[:, c, 1:2], in1=qaw[:, 256:384],
                                                                     op0=ALU.mult, op1=ALU.mult),
                      r=["pA", ("rs2", c), "qaw2"], w=[cnkey + (1,)])
                P.dve(lambda e, kp=kp: e.tensor_copy(out=kp[:], in_=pA[:, 384:416]), r=["pA"], w=[kpkey])
                P.dve(lambda e, j=j: e.tensor_copy(out=gst[:, j, :], in_=pA[:, 416:432]), r=["pA"], w=[("gst", j)])
                cT, cTkey = cnT.next()

                def tr3(e, cnb=cnb):
                    inst = None
                    for k in range(3):
                        inst = e.transpose(pT[:, k * 128:(k + 1) * 128], cnb[:, k * 128:(k + 1) * 128], ident[:])
                    return inst

                P.pe(tr3, r=[cnkey + (0,), cnkey + (1,)], w=["pT"])
                P.dve(lambda e, cT=cT: e.tensor_copy(out=cT[:], in_=pT[:, 0:384].rearrange("p (k t) -> p k t", k=3)),
                      r=["pT"], w=[cTkey])

                def mm_q(e, cT=cT):
                    e.matmul(pQ[:, 0:512], lhsT=cT[:, 0, :], rhs=wuq[:, 0, 0:512], start=True, stop=False)
                    e.matmul(pQ[:, 0:512], lhsT=cT[:, 1, :], rhs=wuq[:, 1, 0:512], start=False, stop=True)
                    e.matmul(pQ[:, 512:768], lhsT=cT[:, 0, :], rhs=wuq[:, 0, 512:768], start=True, stop=False)
                    return e.matmul(pQ[:, 512:768], lhsT=cT[:, 1, :], rhs=wuq[:, 1, 512:768], start=False, stop=True)

                P.pe(mm_q, r=[cTkey, "wuq"], w=["pQ"])
                P.pe(lambda e, cT=cT: e.matmul(pK[:], lhsT=cT[:, 2, :], rhs=wuk[:], start=True, stop=True),
                     r=[cTkey, "wuk"], w=[("pF", 0)])
                P.pe(lambda e, cT=cT: e.matmul(pV[:], lhsT=cT[:, 2, :], rhs=wuv[:], start=True, stop=True),
                     r=[cTkey, "wuv"], w=[("pF", 1)])
                vs, vskey = vst.next()
                P.act(lambda e, vs=vs: e.copy(out=vs[:], in_=pV[:]), r=[("pF", 1)], w=[vskey])
                P.dma("sp", scr["vS"][tok0:tok0 + 128, :], vs[:], r=[vskey])
                qf, qfkey = qkf.next()
                P.act(lambda e, qf=qf: e.copy(out=qf[:, 0:8, :].rearrange("p h d -> p (h d)"), in_=pQ[:, 0:768]),
                      r=["pQ"], w=[qfkey + ("q",)])
                P.dve(lambda e, qf=qf: e.tensor_copy(out=qf[:, 8:16, 0:64],
                                                     in_=pK[:].rearrange("p (h d) -> p h d", h=8)),
                      r=[("pF", 0)], w=[qfkey + ("k",)])
                P.pool(lambda e, qf=qf, kp=kp: e.tensor_copy(out=qf[:, 8:16, 64:96], in_=bc(kp[:], [128, 8, 32], 1)),
                       r=[kpkey], w=[qfkey + ("kp",)])
                qkeys = [qfkey + (s_,) for s_ in ("q", "k", "kp")]
                P.pool(lambda e, qf=qf: e.tensor_tensor(out=sq[:], in0=qf[:], in1=qf[:], op=ALU.mult),
                       r=qkeys, w=["sq"])
                P.dve(lambda e, j=j: e.tensor_reduce(out=ss16[:, j, :], in_=sq[:], axis=AX.X, op=ALU.add),
                      r=["sq"], w=[("ss16", j)])
                P.act(lambda e, j=j: e.activation(out=rs16[:, j, :], in_=ss16[:, j, :], func=AF.Sqrt,
                                                  scale=1.0 / 96, bias=epsb[:]), r=[("ss16", j)], w=[("rs16", j)])
                P.dve(lambda e, j=j: e.reciprocal(out=rs16[:, j, :], in_=rs16[:, j, :]), r=[("rs16", j)],
                      w=[("rs16", j)])
                P.dve(lambda e, qf=qf, j=j: e.tensor_tensor(out=qf[:], in0=qf[:],
                                                            in1=bc(rs16[:, j, :], [128, 16, 96], 2), op=ALU.mult),
                      r=qkeys + [("rs16", j)], w=qkeys)
                qb, qbkey = qkb.next()
                for hh in range(2):
                    eng = P.pool if hh == 0 else P.dve
                    eng(lambda e, qf=qf, qb=qb, hh=hh: e.tensor_tensor(
                        out=qb[:, hh * 8:(hh + 1) * 8, 0:64], in0=qf[:, hh * 8:(hh + 1) * 8, 0:64],
                        in1=bc(qkw[:, hh, 0:64], [128, 8, 64], 1), op=ALU.mult),
                        r=qkeys + ["qkw0", "qkw1"], w=[qbkey + ("n", hh)])
                    eng(lambda e, qf=qf, hh=hh: e.tensor_tensor(
                        out=qf[:, hh * 8:(hh + 1) * 8, 64:96], in0=qf[:, hh * 8:(hh + 1) * 8, 64:96],
                        in1=bc(qkw[:, hh, 64:96], [128, 8, 32], 1), op=ALU.mult),
                        r=qkeys + ["qkw0", "qkw1"], w=qkeys)
                rt, rtkey = rtmp.next()
                cb_ = bc(cosT[:, c, :], [128, 16, 16], 1)
                sb_ = bc(sinT[:, c, :], [128, 16, 16], 1)
                x1_ = lambda qf: qf[:, :, 64:80]
                x2_ = lambda qf: qf[:, :, 80:96]
                P.pool(lambda e, qf=qf, rt=rt, cb_=cb_: e.tensor_tensor(out=rt[:, 0], in0=x1_(qf), in1=cb_, op=ALU.mult),
                       r=qkeys + ["cos"], w=[rtkey + (0,)])
                P.pool(lambda e, qf=qf, rt=rt, sb_=sb_: e.tensor_tensor(out=rt[:, 1], in0=x2_(qf), in1=sb_, op=ALU.mult),
                       r=qkeys + ["sin"], w=[rtkey + (1,)])
                P.dve(lambda e, qf=qf, rt=rt, cb_=cb_: e.tensor_tensor(out=rt[:, 2], in0=x2_(qf), in1=cb_, op=ALU.mult),
                      r=qkeys + ["cos"], w=[rtkey + (2,)])
                P.dve(lambda e, qf=qf, rt=rt, sb_=sb_: e.tensor_tensor(out=rt[:, 3], in0=x1_(qf), in1=sb_, op=ALU.mult),
                      r=qkeys + ["sin"], w=[rtkey + (3,)])
                P.pool(lambda e, qb=qb, rt=rt: e.tensor_tensor(out=qb[:, :, 64:80], in0=rt[:, 0], in1=rt[:, 1],
                                                               op=ALU.subtract),
                       r=[rtkey + (0,), rtkey + (1,)], w=[qbkey + ("r1",)])
                P.dve(lambda e, qb=qb, rt=rt: e.tensor_tensor(out=qb[:, :, 80:96], in0=rt[:, 2], in1=rt[:, 3],
                                                              op=ALU.add),
                      r=[rtkey + (2,), rtkey + (3,)], w=[qbkey + ("r2",)])
                qbkeys = [qbkey + ("n", 0), qbkey + ("n", 1), qbkey + ("r1",), qbkey + ("r2",)]

                for h0 in range(2):
                    def tr8(e, qb=qb, h0=h0):
                        inst = None
                        for hh in range(8):
                            inst = e.transpose(pX[0:96, hh * 128:(hh + 1) * 128], qb[:, h0 * 8 + hh, :], ident[:])
                        return inst

                    P.pe(tr8, r=qbkeys, w=["pX"])
                    P.act(lambda e, qs=qs, j=j, h0=h0: e.copy(
                        out=qs[0:96, h0 * 8:(h0 + 1) * 8, j * 128:(j + 1) * 128],
                        in_=pX[0:96, :].rearrange("p (h t) -> p h t", h=8)), r=["pX"], w=[qskey + (j, h0)])
                if i + 1 < NT:
                    norm_sub(i + 1, j)
            P.dma("sp", scr["gates"][t0:t0 + T, :].rearrange("(j p) g -> p j g", p=128), gst[:],
                  r=[("gst", j) for j in range(4)])
            P.dma("sp", scr["qkT"][:, :, t0:t0 + T].rearrange("h d t -> d h t"), qs[0:96, :, :],
                  r=[qskey + (j, h0) for j in range(4) for h0 in range(2)])
        P.flush()


def phase_attn(P, nc, scr, cins):
    name = "at"
    with ExitStack() as st:
        def A(nm, shape, dt):
            return st.enter_context(nc.sbuf_tensor(f"{name}_{nm}", shape, dt))

        def PS(nm, shape, dt):
            return st.enter_context(nc.psum_tensor(f"{name}_{nm}", shape, dt))

        kt = Rot(A, "kt", 2, [128, S], BF16)
        qt = Rot(A, "qt", 2, [128, S], BF16)
        va = Rot(A, "va", 2, [128, NCH, 65], BF16)
        pt = Rot(A, "pt", 3, [128, 2 * T], BF16)
        rden = Rot(A, "rden", 2, [128, T], F32)
        rbs = Rot(A, "rbs", 2, [64, T], F32)
        ast = Rot(A, "ast", 2, [64, T], BF16)
        ones = A("ones", [128, 64], F32)
        pS = [PS(f"pS{b}", [128, 2 * T], F32) for b in range(2)]
        pO = [PS(f"pO{b}", [128, T], F32) for b in range(2)]
        pR = PS("pR", [128, T], F32)
        P.dve(lambda e: e.memset(ones[:], 1.0), w=["ones"])
        for b in range(2):
            P.dve(lambda e, b=b: e.memset(va.t[b][:, :, 64:65], 1.0), w=[("va1", b)])
        ns = 0
        no = 0
        NP2 = NCH // 2
        tail2 = None
        for h in range(8):
            ktb, ktkey = kt.next()
            qtb, qtkey = qt.next()
            vab, vakey = va.next()
            P.dma("sp", ktb[0:96, :], scr["qkT"][8 + h], w=[ktkey])
            P.dma("sp", qtb[0:96, :], scr["qkT"][h], w=[qtkey])
            for q4 in range(4):
                P.dma("sp", vab[:, q4 * 16:(q4 + 1) * 16, 0:64],
                      scr["vS"][q4 * 2048:(q4 + 1) * 2048, h * 64:(h + 1) * 64].rearrange("(c p) d -> p c d", p=128),
                      w=[vakey + (q4,)])
            vkeys = [vakey + (q4,) for q4 in range(4)] + [("va1", va.i % 2)]
            for g in range(NT):
                po = pO[no % 2]
                pokey = ("pO", no % 2)
                no += 1
                qsl = qtb[0:96, g * T:(g + 1) * T]
                pend = []

                def qk(kp):
                    nonlocal ns
                    ps = pS[ns % 2]
                    pskey = ("pS", ns % 2)
                    ns += 1

                    def mm(e, ps=ps, kp=kp, ktb=ktb, qsl=qsl):
                        e.matmul(ps[:, 0:T], lhsT=ktb[0:96, (2 * kp) * 128:(2 * kp + 1) * 128], rhs=qsl, start=True,
                                 stop=True)
                        return e.matmul(ps[:, T:2 * T], lhsT=ktb[0:96, (2 * kp + 1) * 128:(2 * kp + 2) * 128], rhs=qsl,
                                        start=True, stop=True)

                    P.pe(mm, r=[ktkey, qtkey], w=[pskey])
                    return ps, pskey

                pend.append(qk(0))
                for kp in range(NP2):
                    if kp + 1 < NP2:
                        pend.append(qk(kp + 1))
                    ps, pskey = pend.pop(0)
                    ptb, ptkey = pt.next()
                    P.act(lambda e, ps=ps, ptb=ptb: e.activation(out=ptb[:], in_=ps[:], func=AF.Exp, scale=ATT_SCALE),
                          r=[pskey], w=[ptkey])

                    def pv(e, ptb=ptb, kp=kp, po=po, vab=vab):
                        e.matmul(po[0:65, :], lhsT=vab[:, 2 * kp, :], rhs=ptb[:, 0:T], start=(kp == 0), stop=False)
                        return e.matmul(po[0:65, :], lhsT=vab[:, 2 * kp + 1, :], rhs=ptb[:, T:2 * T], start=False,
                                        stop=(kp == NP2 - 1))

                    P.pe(pv, r=[ptkey] + vkeys, w=[pokey])
                    if kp == 2 and tail2 is not None:
                        tail2()
                        tail2 = None
                rd, rdkey = rden.next()
                rb, rbkey = rbs.next()
                ab, abkey = ast.next()
                P.dve(lambda e, rd=rd, po=po: e.reciprocal(out=rd[64:65, :], in_=po[64:65, :]), r=[pokey], w=[rdkey])

                def tail(rd=rd, rdkey=rdkey, rb=rb, rbkey=rbkey, ab=ab, abkey=abkey, po=po, pokey=pokey, h=h, g=g):
                    P.pe(lambda e: e.matmul(pR[0:64, :], lhsT=ones[64:65, 0:64], rhs=rd[64:65, :], start=True,
                                            stop=True), r=[rdkey, "ones"], w=["pR"])
                    P.dve(lambda e: e.tensor_copy(out=rb[:], in_=pR[0:64, :]), r=["pR"], w=[rbkey])
                    P.dve(lambda e: e.tensor_tensor(out=ab[:], in0=po[0:64, :], in1=rb[:], op=ALU.mult),
                          r=[pokey, rbkey], w=[abkey])
                    P.dma("sp", scr["aT"][h * 64:(h + 1) * 64, g * T:(g + 1) * T], ab[:], r=[abkey])

                tail2 = tail
        tail2()
        P.flush()


def phase_mlstm(P, nc, w, scr, cins):
    name = "ml"
    with ExitStack() as st:
        def A(nm, shape, dt):
            return st.enter_context(nc.sbuf_tensor(f"{name}_{nm}", shape, dt))

        def PS(nm, shape, dt):
            return st.enter_context(nc.psum_tensor(f"{name}_{nm}", shape, dt))

        ident, cf, epsb = load_consts(P, A, cins, need_cf=True)
        identf = cf[:, 0:128]
        triLE = cf[:, 128:256]
        triGE = cf[:, 256:384]
        onesf = cf[:, 384:512]
        G = A("G", [128, NCH, 16], F32)
        bi = A("bi", [128, 8], F32)
        bf_ = A("bf", [128, 8], F32)
        GI = A("GI", [128, 2, NCH, 4], F32)
        GF = A("GF", [128, 2, NCH, 4], F32)
        t1 = A("t1", [128, 512], F32)
        t2 = A("t2", [128, 512], F32)
        LF = A("LF", [128, 512], F32)
        Bc = A("Bc", [128, 512], F32)
        CS = A("CS", [128, 512], F32)
        BL = A("BL", [128, 512], F32)
        MXB = A("MXB", [128, 512], F32)
        mxcol = A("mxcol", [128, 4], F32)
        dg = A("dg", [128, 512], F32)
        M_all = A("M_all", [128, 512], F32)
        DARG = A("DARG", [128, 512], F32)
        DEC = A("DEC", [128, 512], F32)
        A_all = A("A_all", [128, 512], F32)
        THR = A("THR", [128, 512], F32)
        mcur = A("mcur", [128, 8], F32)
        wq = A("wq", [128, 4, 128], BF16)
        wk = A("wk", [128, 4, 128], BF16)
        wvv = A("wv", [128, 4, 128], BF16)
        cw = A("cw", [128, 4, 5], F32)
        cbias = A("cb", [128, 4], F32)
        mnw = A("mnw", [128, 4], F32)
        msk = A("msk", [128, 4], F32)
        p0 = PS("p0", [128, 512], F32)
        p1 = PS("p1", [128, 512], F32)
        pS_ = PS("pS", [128, 512], F32)
        pKV = PS("pKV", [128, 1024], F32)
        pN = PS("pN", [128, 1024], F32)
        pH = PS("pH", [128, 512], F32)

        for q4 in range(4):
            P.dma("sp", G[:, q4 * 16:(q4 + 1) * 16, :],
                  scr["gates"][q4 * 2048:(q4 + 1) * 2048, :].rearrange("(c p) g -> p c g", p=128), w=[("G", q4)])
        gk = [("G", q4) for q4 in range(4)]
        P.dma("sp", bi[:], w["b_igate"].partition_broadcast(128), w=["bi"])
        P.dma("sp", bf_[:], w["b_fgate"].partition_broadcast(128), w=["bf"])
        for (wt, key) in ((wq, "w_mq"), (wk, "w_mk"), (wvv, "w_mv")):
            P.dma("pool", wt[:], w[key].rearrange("h d e -> d h e"), w=[key])
        for jj in range(5):
            P.dma("sp", cw[:, :, jj], w["conv_w"][jj].rearrange("(k p) -> p k", p=128), w=[("cw", jj)],
                  allow_slow_non_contiguous=True)
        P.dma("sp", cbias[:], w["conv_b"].rearrange("(k p) -> p k", p=128), w=["cb"], allow_slow_non_contiguous=True)
        P.dma("sp", mnw[:], w["m_norm_w"].rearrange("(k p) -> p k", p=128), w=["mnw"], allow_slow_non_contiguous=True)
        P.dma("sp", msk[:], w["m_skip"].rearrange("(k p) -> p k", p=128), w=["msk"], allow_slow_non_contiguous=True)

        dgw = A("dgw", [128, 4, 5, 128], BF16)
        cbr = A("cbr", [1, 512], F32)
        cbr2 = A("cbr2", [1, 512], F32)
        brow = A("brow", [1, 2, 512], BF16)
        onesb = A("onesb", [1, 128], BF16)
        P.dma("sp", cbr[:], w["conv_b"].rearrange("(o n) -> o n", o=1), w=["cbr"])
        P.dve(lambda e: e.memset(onesb[:], 1.0), w=["onesb"])
        P.fence()
        for hc in range(4):
            for jj in range(5):
                P.dve(lambda e, hc=hc, jj=jj: e.tensor_scalar(out=dgw[:, hc, jj, :], in0=identf,
                                                              scalar1=cw[:, hc, jj:jj + 1], scalar2=None,
                                                              op0=ALU.mult), w=[("dgw", hc, jj)])
        P.dve(lambda e: e.tensor_copy(out=brow[:, 0, :], in_=cbr[:]), w=["brow0"])
        P.dve(lambda e: e.tensor_copy(out=cbr2[:], in_=brow[:, 0, :]), r=["brow0"], w=["cbr2"])
        P.dve(lambda e: e.tensor_tensor(out=cbr2[:], in0=cbr[:], in1=cbr2[:], op=ALU.subtract), r=["cbr2"], w=["cbr2"])
        P.dve(lambda e: e.tensor_copy(out=brow[:, 1, :], in_=cbr2[:]), r=["cbr2"], w=["brow1"])
        for d in range(2):
            P.dve(lambda e, d=d: e.tensor_tensor(out=GI[:, d], in0=G[:, :, d * 4:d * 4 + 4],
                                                 in1=bc(bi[:, d * 4:d * 4 + 4], [128, NCH, 4], 1), op=ALU.add),
                  r=gk + ["bi"], w=[("GI", d)])
            P.dve(lambda e, d=d: e.tensor_tensor(out=GF[:, d], in0=G[:, :, 8 + d * 4:12 + d * 4],
                                                 in1=bc(bf_[:, d * 4:d * 4 + 4], [128, NCH, 4], 1), op=ALU.add),
                  r=gk + ["bf"], w=[("GF", d)])
        GFf = GF[:].rearrange("p d c h -> p (d c h)")
        GIf = GI[:].rearrange("p d c h -> p (d c h)")
        gfk = [("GF", 0), ("GF", 1)]
        gik = [("GI", 0), ("GI", 1)]
        P.dve(lambda e: e.scalar_tensor_tensor(out=t1[:], in0=GFf, scalar=-1.0, in1=GFf, op0=ALU.mult, op1=ALU.max),
              r=gfk, w=["t1"])
        P.act(lambda e: e.activation(out=t1[:], in_=t1[:], func=AF.Exp, scale=-1.0), r=["t1"], w=["t1"])
        P.act(lambda e: e.activation(out=t1[:], in_=t1[:], func=AF.Ln, bias=1.0), r=["t1"], w=["t1"])
        P.dve(lambda e: e.tensor_scalar(out=t2[:], in0=GFf, scalar1=0.0, scalar2=None, op0=ALU.min), r=gfk, w=["t2"])
        P.dve(lambda e: e.tensor_tensor(out=LF[:], in0=t2[:], in1=t1[:], op=ALU.subtract), r=["t1", "t2"], w=["LF"])
        P.pe(lambda e: e.matmul(p0[:, 0:256], lhsT=triLE, rhs=LF[:, 0:256], start=True, stop=True),
             r=["LF", "cf"], w=["p0a"])
        P.pe(lambda e: e.matmul(p0[:, 256:512], lhsT=triGE, rhs=LF[:, 256:512], start=True, stop=True),
             r=["LF", "cf"], w=["p0b"])
        P.pe(lambda e: e.matmul(p1[:], lhsT=onesf, rhs=LF[:], start=True, stop=True), r=["LF", "cf"], w=["p1"])
        P.dve(lambda e: e.tensor_copy(out=Bc[:], in_=p0[:]), r=["p0a", "p0b"], w=["Bc"])
        P.act(lambda e: e.copy(out=BL[:], in_=p1[:]), r=["p1"], w=["BL"])
        P.dve(lambda e: e.tensor_tensor(out=CS[:], in0=GIf, in1=Bc[:], op=ALU.subtract), r=gik + ["Bc"], w=["CS"])

        def trf(e):
            inst = None
            for k in range(4):
                inst = e.transpose(p0[:, k * 128:(k + 1) * 128], CS[:, k * 128:(k + 1) * 128], identf)
            return inst

        P.pe(trf, r=["CS", "cf", "Bc"], w=["p0a", "p0b"])
        P.dve(lambda e: e.tensor_reduce(out=mxcol[:], in_=p0[:].rearrange("p (k s) -> p k s", k=4), axis=AX.X,
                                        op=ALU.max), r=["p0a", "p0b"], w=["mxcol"])
        for k in range(4):
            P.dve(lambda e, k=k: e.tensor_scalar(out=dg[:, k * 128:(k + 1) * 128], in0=identf,
                                                 scalar1=mxcol[:, k:k + 1], scalar2=None, op0=ALU.mult),
                  r=["mxcol", "cf"], w=[("dg", k)])
        P.pe(lambda e: e.matmul(p1[:], lhsT=onesf, rhs=dg[:], start=True, stop=True),
             r=[("dg", k) for k in range(4)] + ["cf", "BL"], w=["p1"])
        P.dve(lambda e: e.tensor_copy(out=MXB[:], in_=p1[:]), r=["p1"], w=["MXB"])
        P.dve(lambda e: e.memset(mcur[:], 0.0), w=[("mcur", 0), ("mcur", 1)])
        for jstep in range(NCH):
            for d in range(2):
                c = jstep if d == 0 else NCH - 1 - jstep
                sl = slice(d * 256 + c * 4, d * 256 + c * 4 + 4)
                mc = mcur[:, d * 4:d * 4 + 4]
                eng = P.dve
                eng(lambda e, sl=sl, mc=mc: e.tensor_tensor(out=M_all[:, sl], in0=mc, in1=MXB[:, sl], op=ALU.max),
                    r=[("mcur", d), "MXB"], w=[("M", d, c)])
                eng(lambda e, sl=sl, mc=mc: e.tensor_tensor(out=DARG[:, sl], in0=mc, in1=M_all[:, sl],
                                                            op=ALU.subtract),
                    r=[("mcur", d), ("M", d, c)], w=[("DARG", d, c)])
                eng(lambda e, sl=sl, mc=mc: e.tensor_tensor(out=mc, in0=M_all[:, sl], in1=BL[:, sl], op=ALU.add),
                    r=[("M", d, c), "BL", ("DARG", d, c)], w=[("mcur", d)])
        mk = [("M", d, c) for d in range(2) for c in range(NCH)]
        dk = [("DARG", d, c) for d in range(2) for c in range(NCH)]
        P.act(lambda e: e.activation(out=DEC[:], in_=DARG[:], func=AF.Exp), r=dk, w=["DEC"])
        P.dve(lambda e: e.tensor_tensor(out=t1[:], in0=CS[:], in1=M_all[:], op=ALU.subtract), r=["CS"] + mk, w=["t1"])
        P.act(lambda e: e.activation(out=A_all[:], in_=t1[:], func=AF.Exp), r=["t1"], w=["A_all"])
        P.dve(lambda e: e.tensor_tensor(out=t2[:], in0=Bc[:], in1=M_all[:], op=ALU.add), r=["Bc"] + mk, w=["t2"])
        P.act(lambda e: e.activation(out=THR[:], in_=t2[:], func=AF.Exp, scale=-1.0), r=["t2"], w=["THR"])
        P.flush()

        mt = Rot(A, "mt", 3, [128, 4, 132], F32)
        xcT = Rot(A, "xcT", 3, [128, 4, 128], F32)
        xcb = Rot(A, "xcb", 2, [128, 4, 128], BF16)
        mb = Rot(A, "mb", 2, [128, 4, 132], BF16)
        qTb = Rot(A, "qTb", 2, [128, 4, 128], BF16)
        kTb = Rot(A, "kTb", 2, [128, 4, 128], BF16)
        kb = Rot(A, "kb", 2, [128, 4, 128], BF16)
        vab = Rot(A, "vab", 2, [128, 4, 129], BF16)
        scm = Rot(A, "scm", 2, [128, 4, 128], BF16)
        Cd = Rot(A, "Cd", 2, [128, 4, 129], F32)
        KVs = Rot(A, "KVs", 2, [128, 4, 129], F32)
        Cdb = Rot(A, "Cdb", 2, [128, 4, 129], BF16)
        Call = A("Call", [128, 4, 129], F32)
        sm = Rot(A, "sm", 2, [128, 4, 4], F32)
        hout = Rot(A, "hout", 2, [128, 4, 128], F32)
        hbt = Rot(A, "hbt", 2, [128, 4, 128], F32)
        sopt = Rot(A, "sopt", 2, [128, 4, 128], F32)
        hsq = A("hsq", [128, 4, 128], F32)
        hss = Rot(A, "hss", 2, [128, 2, 4], F32)
        ut = Rot(A, "ut", 2, [128, 4, 128], F32)
        bmst = Rot(A, "bmst", 2, [128, 4, T], BF16)
        mask = {0: triLE, 1: triGE}

        for d in (1, 0):
            P.dve(lambda e: e.memset(Call[:], 0.0), w=["Call"])
            order = range(NCH) if d == 0 else range(NCH - 1, -1, -1)
            bst = {"bms": None, "key": None}

            def chunk_body(c, d=d, bst=bst):
                t0 = c * 128
                sl = slice(d * 256 + c * 4, d * 256 + c * 4 + 4)
                m_, mkey = mt.next()
                lo, hi = max(t0 - 2, 0), min(t0 + 130, S)
                o0 = lo - (t0 - 2)
                wl = [mkey]
                if c == 0:
                    P.pool(lambda e, m_=m_: e.memset(m_[:, :, 0:2], 0.0), w=[mkey + ("z",)])
                    wl.append(mkey + ("z",))
                if c == NCH - 1:
                    P.pool(lambda e, m_=m_: e.memset(m_[:, :, 130:132], 0.0), w=[mkey + ("z",)])
                    wl.append(mkey + ("z",))
                P.dma("sp", m_[:, :, o0:o0 + (hi - lo)], scr["minT"][:, lo:hi].rearrange("(k p) t -> p k t", p=128),
                      w=[mkey], r=[mkey + ("z",)])
                mkeys = [mkey]
                mbb, mbkey = mb.next()
                P.pool(lambda e, mbb=mbb, m_=m_: e.tensor_copy(out=mbb[:], in_=m_[:]), r=mkeys, w=[mbkey])

                def mm_conv(e, mbb=mbb):
                    inst = None
                    for hc in range(4):
                        o_ = p0[:, hc * 128:(hc + 1) * 128]
                        for jj in range(5):
                            e.matmul(o_, lhsT=dgw[:, hc, jj, :], rhs=mbb[:, hc, jj:jj + 128], start=(jj == 0), stop=False)
                        e.matmul(o_, lhsT=brow[0:1, 0, hc * 128:(hc + 1) * 128], rhs=onesb[0:1, :], start=False, stop=False)
                        inst = e.matmul(o_, lhsT=brow[0:1, 1, hc * 128:(hc + 1) * 128], rhs=onesb[0:1, :], start=False,
                                        stop=True)
                    return inst

                P.pe(mm_conv, r=[mbkey, "dgw", "brow"], w=["p0"])
                xb, xbkey = xcb.next()
                P.act(lambda e, xb=xb: e.activation(out=xb[:].rearrange("p h t -> p (h t)"), in_=p0[:], func=AF.Silu),
                      r=["p0"], w=[xbkey])
                if d == 0:
                    xf, xfkey = xcT.next()
                    P.act(lambda e, xf=xf: e.activation(out=xf[:].rearrange("p h t -> p (h t)"), in_=p0[:],
                                                        func=AF.Silu), r=["p0"], w=[xfkey])
                qT_, qTkey = qTb.next()
                kT_, kTkey = kTb.next()
                k_, kkey = kb.next()
                va_, vakey = vab.next()

                def proj(e, wt, x, out_p, tmaj):
                    inst = None
                    for h in range(4):
                        if tmaj:
                            inst = e.matmul(out_p[:, h * 128:(h + 1) * 128], lhsT=x[:, h, :], rhs=wt[:, h, :],
                                            start=True, stop=True)
                        else:
                            inst = e.matmul(out_p[:, h * 128:(h + 1) * 128], lhsT=wt[:, h, :], rhs=x[:, h, :],
                                            start=True, stop=True)
                    return inst

                P.pe(lambda e, xb=xb: proj(e, wq, xb, p1, False), r=[xbkey, "w_mq"], w=["p1"])
                P.act(lambda e, qT_=qT_: e.copy(out=qT_[:].rearrange("p h t -> p (h t)"), in_=p1[:]), r=["p1"],
                      w=[qTkey])
                P.pe(lambda e, xb=xb: proj(e, wk, xb, p0, False), r=[xbkey, "w_mk"], w=["p0"])
                P.act(lambda e, kT_=kT_: e.mul(out=kT_[:].rearrange("p h t -> p (h t)"), in_=p0[:], mul=128 ** -0.5),
                      r=["p0"], w=[kTkey])
                P.pe(lambda e, xb=xb: proj(e, wk, xb, p1, True), r=[xbkey, "w_mk"], w=["p1"])
                P.act(lambda e, k_=k_: e.mul(out=k_[:].rearrange("p h t -> p (h t)"), in_=p1[:], mul=128 ** -0.5),
                      r=["p1"], w=[kkey])
                P.pe(lambda e, mbb=mbb: proj(e, wvv, mbb[:, :, 2:130], p0, True), r=[mbkey, "w_mv"], w=["p0"])
                P.dve(lambda e, va_=va_, sl=sl: e.tensor_tensor(
                    out=va_[:, :, 0:128], in0=p0[:].rearrange("p (h e) -> p h e", h=4),
                    in1=bc(A_all[:, sl], [128, 4, 128], 2), op=ALU.mult), r=["p0", "A_all"], w=[vakey + (0,)])
                P.dve(lambda e, va_=va_, sl=sl: e.tensor_copy(out=va_[:, :, 128:129], in_=A_all[:, sl].unsqueeze(2)),
                      r=["A_all"], w=[vakey + (1,)])
                vakeys = [vakey + (0,), vakey + (1,)]

                def mm_s(e, kT_=kT_, qT_=qT_):
                    inst = None
                    for h in range(4):
                        inst = e.matmul(pS_[:, h * 128:(h + 1) * 128], lhsT=kT_[:, h, :], rhs=qT_[:, h, :],
                                        start=True, stop=True)
                    return inst

                P.pe(mm_s, r=[kTkey, qTkey], w=["pS"])
                sc_, sckey = scm.next()
                P.dve(lambda e, sc_=sc_, d=d: e.tensor_tensor(out=sc_[:], in0=pS_[:].rearrange("p (h t) -> p h t", h=4),
                                                              in1=bc(mask[d], [128, 4, 128], 1), op=ALU.mult),
                      r=["pS", "cf"], w=[sckey])

                def mm_kv(e, k_=k_, va_=va_):
                    inst = None
                    for h in range(4):
                        inst = e.matmul(pKV[:, h * 256:h * 256 + 129], lhsT=k_[:, h, :], rhs=va_[:, h, :],
                                        start=True, stop=True)
                    return inst

                P.pe(mm_kv, r=[kkey] + vakeys, w=["pKV"])
                kv3 = pKV[:].rearrange("p (h x) -> p h x", h=4)[:, :, 0:129]
                kvs_, kvskey = KVs.next()
                P.dve(lambda e, kvs_=kvs_: e.tensor_copy(out=kvs_[:], in_=kv3), r=["pKV"], w=[kvskey])
                yield
                cd_, cdkey = Cd.next()
                cdb_, cdbkey = Cdb.next()
                P.dve(lambda e, cd_=cd_, sl=sl: e.tensor_tensor(out=cd_[:], in0=Call[:],
                                                                in1=bc(DEC[:, sl], [128, 4, 129], 2), op=ALU.mult),
                      r=["Call", "DEC"], w=[cdkey])
                P.dve(lambda e, cd_=cd_, kvs_=kvs_: e.tensor_tensor(out=Call[:], in0=cd_[:], in1=kvs_[:], op=ALU.add),
                      r=[cdkey, kvskey], w=["Call"])
                P.act(lambda e, cd_=cd_, cdb_=cdb_: e.copy(out=cdb_[:], in_=cd_[:]), r=[cdkey], w=[cdbkey])
                yield

                def mm_n(e, sc_=sc_, va_=va_, qT_=qT_, cdb_=cdb_):
                    inst = None
                    for h in range(4):
                        e.matmul(pN[:, h * 256:h * 256 + 129], lhsT=sc_[:, h, :], rhs=va_[:, h, :], start=True,
                                 stop=False)
                        inst = e.matmul(pN[:, h * 256:h * 256 + 129], lhsT=qT_[:, h, :], rhs=cdb_[:, h, :],
                                        start=False, stop=True)
                    return inst

                P.pe(mm_n, r=[sckey, qTkey, cdbkey] + vakeys, w=["pN"])
                n3 = pN[:].rearrange("p (h x) -> p h x", h=4)
                yield
                s_, skey = sm.next()
                den = n3[:, :, 128]
                P.dve(lambda e, s_=s_: e.tensor_copy(out=s_[:, 3, :], in_=den), r=["pN"], w=[skey + (3,)])
                P.dve(lambda e, s_=s_: e.scalar_tensor_tensor(out=s_[:, 0, :], in0=s_[:, 3, :], scalar=-1.0,
                                                              in1=s_[:, 3, :], op0=ALU.mult, op1=ALU.max),
                      r=[skey + (3,)], w=[skey + (0,)])
                P.dve(lambda e, s_=s_, sl=sl: e.tensor_tensor(out=s_[:, 1, :], in0=s_[:, 0, :], in1=THR[:, sl],
                                                              op=ALU.max), r=[skey + (0,), "THR"], w=[skey + (1,)])
                P.dve(lambda e, s_=s_: e.reciprocal(out=s_[:, 2, :], in_=s_[:, 1, :]), r=[skey + (1,)],
                      w=[skey + (2,)])
                ho, hokey = hout.next()
                P.dve(lambda e, ho=ho, s_=s_: e.tensor_tensor(out=ho[:], in0=n3[:, :, 0:128],
                                                              in1=bc(s_[:, 2, :], [128, 4, 128], 2), op=ALU.mult),
                      r=["pN", skey + (2,)], w=[hokey])
                if d == 1:
                    P.dma("sp", scr["hb"][t0:t0 + 128, :], ho[:].rearrange("p h e -> p (h e)"), r=[hokey])
                    return
                hb_, hbkey = hbt.next()
                so_, sokey = sopt.next()
                P.dma("sp", hb_[:].rearrange("p h e -> p (h e)"), scr["hb"][t0:t0 + 128, :], w=[hbkey])
                P.dma("sp", so_[:], scr["sopT"][:, t0:t0 + 128].rearrange("(k p) t -> p k t", p=128), w=[sokey])
                P.pool(lambda e, ho=ho, hb_=hb_: e.tensor_tensor(out=hb_[:], in0=ho[:], in1=hb_[:], op=ALU.add),
                       r=[hokey, hbkey], w=[hbkey])
                P.pool(lambda e, hb_=hb_: e.tensor_tensor(out=hsq[:], in0=hb_[:], in1=hb_[:], op=ALU.mult),
                       r=[hbkey], w=["hsq"])
                hs_, hskey = hss.next()
                P.dve(lambda e, hs_=hs_: e.tensor_reduce(out=hs_[:, 0, :], in_=hsq[:], axis=AX.X, op=ALU.add),
                      r=["hsq"], w=[hskey + (0,)])
                rstd_pool(P, hs_[:, 1, :], hs_[:, 0, :], 128, epsb[:, 0:4], [hskey + (0,)], [hskey + (1,)])
                P.pool(lambda e, hb_=hb_, hs_=hs_: e.tensor_tensor(out=hb_[:], in0=hb_[:],
                                                                   in1=bc(hs_[:, 1, :], [128, 4, 128], 2),
                                                                   op=ALU.mult), r=[hbkey, hskey + (1,)], w=[hbkey])

                def trh(e, hb_=hb_):
                    inst = None
                    for h in range(4):
                        inst = e.transpose(pH[:, h * 128:(h + 1) * 128], hb_[:, h, :], identf)
                    return inst

                P.pe(trh, r=[hbkey, "cf"], w=["pH"])
                u_, ukey = ut.next()
                for h in range(4):
                    P.act(lambda e, u_=u_, h=h: e.activation(out=u_[:, h, :], in_=pH[:, h * 128:(h + 1) * 128],
                                                             func=AF.Identity, scale=mnw[:, h:h + 1]),
                          r=["pH", "mnw"], w=[ukey + (h,)])
                    P.dve(lambda e, u_=u_, h=h, xf=xf: e.scalar_tensor_tensor(
                        out=u_[:, h, :], in0=xf[:, h, :], scalar=msk[:, h:h + 1], in1=u_[:, h, :], op0=ALU.mult,
                        op1=ALU.add), r=[ukey + (h,), xfkey, "msk"], w=[ukey + (h,)])
                if c % 4 == 0:
                    bst["bms"], bst["key"] = bmst.next()
                bms, bmskey = bst["bms"], bst["key"]
                cj = c % 4
                P.dve(lambda e, u_=u_, so_=so_, bms=bms, cj=cj: e.tensor_tensor(
                    out=bms[:, :, cj * 128:(cj + 1) * 128], in0=u_[:], in1=so_[:], op=ALU.mult),
                    r=[ukey + (h,) for h in range(4)] + [sokey], w=[bmskey + (cj,)])
                if cj == 3:
                    tt0 = (c - 3) * 128
                    P.dma("sp", scr["bmT"][:, tt0:tt0 + T].rearrange("(k p) t -> p k t", p=128), bms[:],
                          r=[bmskey + (q,) for q in range(4)])

            order = list(order)
            n_ = len(order)
            gens = {c: chunk_body(c) for c in order}
            next(gens[order[0]])
            for t in range(n_ + 1):
                if t < n_:
                    next(gens[order[t]])
                if t >= 1:
                    for _ in gens.pop(order[t - 1]):
                        pass
                if t < n_:
                    next(gens[order[t]])
                if t + 1 < n_:
                    next(gens[order[t + 1]])
            P.flush()


def phase_merge(P, nc, x1s, w, scr, cins):
    name = "mg"
    with ExitStack() as st:
        def A(nm, shape, dt):
            return st.enter_context(nc.sbuf_tensor(f"{name}_{nm}", shape, dt))

        def PS(nm, shape, dt):
            return st.enter_context(nc.psum_tensor(f"{name}_{nm}", shape, dt))

        wa = A("wa", [128, 4, D], BF16)
        wb = A("wb", [128, 4, D], BF16)
        wo = A("wo", [128, 8, D], BF16)
        xt = Rot(A, "xt", 2, [128, 4, D], F32)
        at = Rot(A, "at", 2, [128, 4, T], BF16)
        bt = Rot(A, "bt", 2, [128, 4, T], BF16)
        sga = Rot(A, "sga", 2, [128, 8, T], F32)
        sgb = Rot(A, "sgb", 2, [128, 8, T], F32)
        mT = Rot(A, "mT", 2, [128, 8, T], BF16)
        ta = Rot(A, "ta", 2, [128, T], F32)
        tb = Rot(A, "tb", 2, [128, T], F32)
        pA_ = [PS(f"pA{b}", [128, T], F32) for b in range(2)]
        pB_ = [PS(f"pB{b}", [128, T], F32) for b in range(2)]
        pD = [PS(f"pD{b}", [128, T], F32) for b in range(2)]
        P.dma("pool", wa[:], w["w_branch_a"].rearrange("(k p) n -> p k n", p=128), w=["wa"])
        P.dma("pool", wb[:], w["w_branch_b"].rearrange("(k p) n -> p k n", p=128), w=["wb"])
        P.dma("pool", wo[:], w["w_out"].rearrange("(k p) n -> p k n", p=128), w=["wo"])

        def xview(ap, i):
            return ap[i * T:(i + 1) * T, :].rearrange("(j p) d -> p j d", p=128)

        bufs = {}

        def load(i):
            t0 = i * T
            x_, xk = xt.next()
            a_, ak = at.next()
            b_, bk = bt.next()
            ga_, gak = sga.next()
            gb_, gbk = sgb.next()
            P.dma("sp", a_[:], scr["aT"][:, t0:t0 + T].rearrange("(k p) t -> p k t", p=128), w=[ak])
            P.dma("sp", b_[:], scr["bmT"][:, t0:t0 + T].rearrange("(k p) t -> p k t", p=128), w=[bk])
            P.dma("sp", ga_[:], scr["sgT"][0:1024, t0:t0 + T].rearrange("(k p) t -> p k t", p=128), w=[gak])
            P.dma("sp", gb_[:], scr["sgT"][1024:2048, t0:t0 + T].rearrange("(k p) t -> p k t", p=128), w=[gbk])
            P.dma("sp", x_[:], xview(x1s, i), w=[xk + (j,) for j in range(4)])
            bufs[i] = (x_, xk, a_, ak, b_, bk, ga_, gak, gb_, gbk)

        load(0)
        na = 0
        nd = 0
        for i in range(NT):
            if i + 1 < NT:
                load(i + 1)
            x_, xk, a_, ak, b_, bk, ga_, gak, gb_, gbk = bufs.pop(i)
            m_, mk = mT.next()
            for cc in range(8):
                pa, pb = pA_[na % 2], pB_[na % 2]
                pak, pbk = ("pA", na % 2), ("pB", na % 2)
                na += 1

                def mm_a(e, pa=pa, cc=cc, a_=a_):
                    inst = None
                    for k in range(4):
                        inst = e.matmul(pa[:], lhsT=wa[:, k, cc * 128:(cc + 1) * 128], rhs=a_[:, k, :],
                                        start=(k == 0), stop=(k == 3))
                    return inst

                def mm_b(e, pb=pb, cc=cc, b_=b_):
                    inst = None
                    for k in range(4):
                        inst = e.matmul(pb[:], lhsT=wb[:, k, cc * 128:(cc + 1) * 128], rhs=b_[:, k, :],
                                        start=(k == 0), stop=(k == 3))
                    return inst

                P.pe(mm_a, r=[ak, "wa"], w=[pak])
                P.pe(mm_b, r=[bk, "wb"], w=[pbk])
                ta_, tak = ta.next()
                tb_, tbk = tb.next()
                P.dve(lambda e, ta_=ta_, pa=pa, ga_=ga_, cc=cc: e.tensor_tensor(out=ta_[:], in0=pa[:],
                                                                                in1=ga_[:, cc, :], op=ALU.mult),
                      r=[pak, gak], w=[tak])
                P.dve(lambda e, tb_=tb_, pb=pb, gb_=gb_, cc=cc: e.tensor_tensor(out=tb_[:], in0=pb[:],
                                                                                in1=gb_[:, cc, :], op=ALU.mult),
                      r=[pbk, gbk], w=[tbk])
                P.pool(lambda e, ta_=ta_, tb_=tb_, m_=m_, cc=cc: e.tensor_tensor(out=m_[:, cc, :], in0=ta_[:],
                                                                                 in1=tb_[:], op=ALU.add),
                       r=[tak, tbk], w=[mk + (cc,)])
            mkeys = [mk + (cc,) for cc in range(8)]
            for j in range(4):
                for half in range(2):
                    d_ = pD[nd % 2]
                    dk_ = ("pD", nd % 2)
                    nd += 1

                    def mm_o(e, d_=d_, j=j, half=half, m_=m_):
                        inst = None
                        for k in range(8):
                            inst = e.matmul(d_[:], lhsT=m_[:, k, j * 128:(j + 1) * 128],
                                            rhs=wo[:, k, half * 512:(half + 1) * 512], start=(k == 0), stop=(k == 7))
                        return inst

                    P.pe(mm_o, r=mkeys + ["wo"], w=[dk_])
                    xs = x_[:, j, half * 512:(half + 1) * 512]
                    P.dve(lambda e, d_=d_, xs=xs: e.tensor_tensor(out=xs, in0=d_[:], in1=xs, op=ALU.add),
                          r=[dk_, xk + (j,)], w=[xk + (j,)])
            P.dma("sp", xview(x1s, i), x_[:], r=[xk + (j,) for j in range(4)])
        P.flush()


W_NAMES = ["ffn1_norm_w", "ffn1_w_gate", "ffn1_w_up", "ffn1_w_down", "mix_norm_w", "w_in", "q_a_norm_w", "w_uq",
           "kv_a_norm_w", "w_uk", "w_uv", "q_norm_w", "k_norm_w", "w_branch_a", "conv_w", "conv_b", "w_mq", "w_mk",
           "w_mv", "b_igate", "b_fgate", "m_norm_w", "m_skip", "w_branch_b", "w_out", "ffn2_norm_w", "ffn2_w_gate",
           "ffn2_w_up", "ffn2_w_down", "final_norm_w"]
W_SHAPES = {
    "ffn1_norm_w": [D], "ffn1_w_gate": [D, FF], "ffn1_w_up": [D, FF], "ffn1_w_down": [FF, D], "mix_norm_w": [D],
    "w_in": [D, IN_DIM], "q_a_norm_w": [256], "w_uq": [256, 768], "kv_a_norm_w": [128], "w_uk": [128, 512],
    "w_uv": [128, 512], "q_norm_w": [96], "k_norm_w": [96], "w_branch_a": [512, D], "conv_w": [5, 512],
    "conv_b": [512], "w_mq": [4, 128, 128], "w_mk": [4, 128, 128], "w_mv": [4, 128, 128], "b_igate": [8],
    "b_fgate": [8], "m_norm_w": [512], "m_skip": [512], "w_branch_b": [512, D], "w_out": [D, D],
    "ffn2_norm_w": [D], "ffn2_w_gate": [D, FF], "ffn2_w_up": [D, FF], "ffn2_w_down": [FF, D], "final_norm_w": [D],
}


def build_program(phases=("ffn1", "inproj", "attn", "mlstm", "merge", "ffn2"), debug=()):
    nc = bass.Bass("TRN2", target_bir_lowering=False)
    x = nc.dram_tensor("x", [S, D], F32, kind="ExternalInput").ap()
    pos = nc.dram_tensor("positions", [S], I32, kind="ExternalInput").ap()
    w = {k: nc.dram_tensor(k, W_SHAPES[k], F32, kind="ExternalInput").ap() for k in W_NAMES}
    cins = {
        "c_ident": nc.dram_tensor("c_ident", [128, 128], BF16, kind="ExternalInput").ap(),
        "c_f32": nc.dram_tensor("c_f32", [128, 512], F32, kind="ExternalInput").ap(),
        "c_inv": nc.dram_tensor("c_inv", [16], F32, kind="ExternalInput").ap(),
    }
    out = nc.dram_tensor("out", [S, D], F32, kind="ExternalOutput").ap()

    def scratch(nm, shape, dt):
        kind = "ExternalOutput" if nm in debug else "Internal"
        return nc.dram_tensor("s_" + nm, shape, dt, kind=kind).ap()

    x1s = scratch("x1", [S, D], F32)
    scr = {
        "sgT": scratch("sgT", [2048, S], F32),
        "minT": scratch("minT", [512, S], F32),
        "sopT": scratch("sopT", [512, S], F32),
        "gates": scratch("gates", [S, 16], F32),
        "qkT": scratch("qkT", [16, 96, S], BF16),
        "vS": scratch("vS", [S, 512], BF16),
        "aT": scratch("aT", [512, S], BF16),
        "bmT": scratch("bmT", [512, S], BF16),
        "hb": scratch("hb", [S, 512], F32),
    }
    with ExitStack() as top:
        sems = [top.enter_context(nc.semaphore(f"sem{i}")) for i in range(96)]
        P = Prog(nc, sems)
        if "ffn1" in phases:
            phase_ffn(P, nc, "f1", x, x1s, w["ffn1_norm_w"], w["ffn1_w_gate"], w["ffn1_w_up"], w["ffn1_w_down"], cins)
        if "inproj" in phases:
            phase_inproj(P, nc, x1s, pos, w, scr, cins)
        if "attn" in phases:
            phase_attn(P, nc, scr, cins)
        if "mlstm" in phases:
            phase_mlstm(P, nc, w, scr, cins)
        if "merge" in phases:
            phase_merge(P, nc, x1s, w, scr, cins)
        if "ffn2" in phases:
            phase_ffn(P, nc, "f2", x1s, out, w["ffn2_norm_w"], w["ffn2_w_gate"], w["ffn2_w_up"], w["ffn2_w_down"],
                      cins, final_w=w["final_norm_w"])
    return nc


def make_consts():
    ident = np.eye(128, dtype=np.float32)
    s_idx = np.arange(128)[:, None]
    t_idx = np.arange(128)[None, :]
    cf = np.concatenate([ident, (s_idx <= t_idx).astype(np.float32), (s_idx >= t_idx).astype(np.float32),
                         np.ones((128, 128), np.float32)], axis=1)
    half = 16
    inv = (np.float32(10000.0) ** (-np.arange(half, dtype=np.float32) / np.float32(half))).astype(np.float32)
    return {"c_ident": ident.astype(ml_dtypes.bfloat16), "c_f32": np.ascontiguousarray(cf), "c_inv": inv}


def make_in_maps(inputs, n_cores=8):
    consts = make_consts()
    maps = []
    for b in range(n_cores):
        m = {"x": np.ascontiguousarray(inputs["x"][b]), "positions": np.ascontiguousarray(inputs["positions"][b])}
        for k in W_NAMES:
            m[k] = np.ascontiguousarray(np.asarray(inputs[k])[0])
        m.update(consts)
        maps.append(m)
    return maps


def kernel(**inputs):
    inputs = {k: np.asarray(v) for k, v in inputs.items()}
    nc = build_program()
    in_maps = make_in_maps(inputs, 8)
    res = run_bass_kernel_spmd(nc, in_maps, core_ids=list(range(8)))
    return np.stack([np.asarray(r["out"]) for r in res.results], axis=0).astype(np.float32)
```

```python
import math
from contextlib import ExitStack
from collections import defaultdict

import numpy as np
import ml_dtypes
import concourse.bass as bass
import concourse.mybir as mybir
from concourse.bass_utils import run_bass_kernel_spmd

F32 = mybir.dt.float32
BF16 = mybir.dt.bfloat16
I32 = mybir.dt.int32
AF = mybir.ActivationFunctionType
ALU = mybir.AluOpType
AX = mybir.AxisListType

S = 8192
D = 1024
FF = 2816
NHC = FF // 128
T = 512
NT = S // T
NCH = S // 128
IN_DIM = 3504
EPS = 1e-6
ATT_SCALE = 96 ** -0.5


class _Op:
    __slots__ = ("eng", "fn", "deps", "dma", "prev")

    def __init__(self, eng, fn, deps, dma):
        self.eng, self.fn, self.deps, self.dma, self.prev = eng, fn, deps, dma, None


class Prog:
    DMAQ = {"sp": 8, "pool": 4}

    def __init__(self, nc, sems):
        self.nc = nc
        self.sems = sems
        self.next_sem = 0
        self.dma_sems = {q: [self._alloc() for _ in range(k)] for q, k in self.DMAQ.items()}
        self.dma_cnt = {q: [0] * k for q, k in self.DMAQ.items()}
        self.dma_last = {q: [None] * k for q, k in self.DMAQ.items()}
        self.dma_n = {q: 0 for q in self.DMAQ}
        self._reset()

    def _alloc(self):
        i = self.next_sem
        self.next_sem += 1
        assert i < len(self.sems), "out of semaphores"
        return i

    def _reset(self):
        self.ops = []
        self.lastw = {}
        self.readers = defaultdict(list)
        self.fence_deps = set()
        self.fenced = set()

    def fence(self):
        self.fence_deps = set(range(len(self.ops)))
        self.fenced = set()

    def op(self, eng, fn, r=(), w=(), dma=False):
        idx = len(self.ops)
        deps = set()
        for k in r:
            if k in self.lastw:
                deps.add(self.lastw[k])
        for k in w:
            if k in self.lastw:
                deps.add(self.lastw[k])
            deps.update(self.readers.get(k, ()))
        for k in w:
            self.lastw[k] = idx
            self.readers[k] = []
        for k in r:
            self.readers[k].append(idx)
        if self.fence_deps and eng not in self.fenced:
            deps |= self.fence_deps
            self.fenced.add(eng)
        deps.discard(idx)
        self.ops.append(_Op(eng, fn, deps, dma))
        return idx

    def pe(self, fn, r=(), w=()):
        return self.op("pe", fn, r, w)

    def act(self, fn, r=(), w=()):
        return self.op("act", fn, r, w)

    def dve(self, fn, r=(), w=()):
        return self.op("dve", fn, r, w)

    def pool(self, fn, r=(), w=()):
        return self.op("pool", fn, r, w)

    def dma(self, q, out, in_, r=(), w=(), **kw):
        return self.op(q, lambda e, out=out, in_=in_, kw=kw: e.dma_start(out=out, in_=in_, **kw), r, w, dma=True)

    def flush(self):
        nc = self.nc
        ops = self.ops
        n = len(ops)
        signal = [False] * n

        def skip(p, c):
            return p.eng == "pe" and c.eng == "pe" and not p.dma and not c.dma

        for o in ops:
            for d in o.deps:
                if not skip(ops[d], o):
                    signal[d] = True
        csem = {}
        ccnt = {}
        ticket = [None] * n
        per_eng = {e: [] for e in ("pe", "act", "dve", "pool", "sp")}
        for i, o in enumerate(ops):
            per_eng[o.eng].append(i)
            if o.dma:
                q = o.eng
                K = len(self.dma_sems[q])
                j = self.dma_n[q] % K
                self.dma_n[q] += 1
                o.prev = self.dma_last[q][j]
                self.dma_cnt[q][j] += 1
                ticket[i] = (self.dma_sems[q][j], 16 * self.dma_cnt[q][j])
                self.dma_last[q][j] = ticket[i]
            elif signal[i]:
                e = o.eng
                if e not in csem or ccnt[e] >= 30000:
                    csem[e] = self._alloc()
                    ccnt[e] = 0
                ccnt[e] += 1
                ticket[i] = (csem[e], ccnt[e])
        sems = self.sems

        def emit(engname, eng):
            waited = {}
            for i in per_eng[engname]:
                o = ops[i]
                need = {}
                for d in o.deps:
                    if skip(ops[d], o):
                        continue
                    s, v = ticket[d]
                    if need.get(s, 0) < v:
                        need[s] = v
                if o.prev is not None:
                    s, v = o.prev
                    if need.get(s, 0) < v:
                        need[s] = v
                for s, v in need.items():
                    if waited.get(s, 0) < v:
                        eng.wait_ge(sems[s], v)
                        waited[s] = v
                inst = o.fn(eng)
                if ticket[i] is not None:
                    inst.then_inc(sems[ticket[i][0]], 16 if o.dma else 1)
            if engname in self.dma_last:
                for t in self.dma_last[engname]:
                    if t is not None and waited.get(t[0], 0) < t[1]:
                        eng.wait_ge(sems[t[0]], t[1])

        with nc.Block() as blk:
            if per_eng["pe"]:
                @blk.tensor
                def _(e):
                    emit("pe", e)
            if per_eng["act"]:
                @blk.scalar
                def _(e):
                    emit("act", e)
            if per_eng["dve"]:
                @blk.vector
                def _(e):
                    emit("dve", e)
            if per_eng["pool"]:
                @blk.gpsimd
                def _(e):
                    emit("pool", e)
            if per_eng["sp"]:
                @blk.sync
                def _(e):
                    emit("sp", e)
        self._reset()


class Rot:
    def __init__(self, alloc, name, n, shape, dt):
        self.t = [alloc(f"{name}{i}", shape, dt) for i in range(n)]
        self.name = name
        self.i = -1

    def next(self):
        self.i += 1
        j = self.i % len(self.t)
        return self.t[j], (self.name, j)


USE_POOL_POW = False


def rstd_pool(P, out_ap, in_ap, n, mh_ap, r, w):
    if not USE_POOL_POW:
        P.act(lambda e: e.activation(out=out_ap, in_=in_ap, func=AF.Sqrt, scale=1.0 / n, bias=EPS), r=r, w=w)
        P.dve(lambda e: e.reciprocal(out=out_ap, in_=out_ap), r=w, w=w)
        return
    P.pool(lambda e: e.tensor_scalar(out=out_ap, in0=in_ap, scalar1=1.0 / n, scalar2=EPS, op0=ALU.mult, op1=ALU.add),
           r=r, w=w)
    P.pool(lambda e: e.tensor_tensor(out=out_ap, in0=out_ap, in1=mh_ap, op=ALU.pow), r=w, w=w)


def bc(ap2d, shape, axis):
    return ap2d.unsqueeze(axis).to_broadcast(list(shape))


def emit_norm_T(P, x_ap, x_key, nw, hn, hn_key, ss, rs, col, epsb, ident, pT, pT_key, hT_view, hT_key,
                evac="act", n=D):
    nk = n // 128
    P.act(lambda e: e.activation(out=hn[:, 0:n], in_=x_ap, func=AF.Square, accum_out=ss[:, col:col + 1]),
          r=[x_key], w=[hn_key, ("ss", col)])
    P.act(lambda e: e.activation(out=rs[:, col:col + 1], in_=ss[:, col:col + 1], func=AF.Sqrt,
                                 scale=1.0 / n, bias=epsb[:, 0:1]),
          r=[("ss", col)], w=[("rs", col)])
    P.dve(lambda e: e.reciprocal(out=rs[:, col:col + 1], in_=rs[:, col:col + 1]),
          r=[("rs", col)], w=[("rs", col)])
    P.dve(lambda e: e.scalar_tensor_tensor(out=hn[:, 0:n], in0=x_ap, scalar=rs[:, col:col + 1], in1=nw,
                                           op0=ALU.mult, op1=ALU.mult),
          r=[x_key, ("rs", col)], w=[hn_key])

    def tr(e):
        inst = None
        for k in range(nk):
            inst = e.transpose(pT[:, k * 128:(k + 1) * 128], hn[:, k * 128:(k + 1) * 128], ident[:])
        return inst

    P.pe(tr, r=[hn_key], w=[pT_key])
    src = pT[:, 0:n].rearrange("p (k t) -> p k t", k=nk)
    if evac == "act":
        P.act(lambda e: e.copy(out=hT_view, in_=src), r=[pT_key], w=[hT_key])
    else:
        P.dve(lambda e: e.tensor_copy(out=hT_view, in_=src), r=[pT_key], w=[hT_key])


def load_consts(P, A, cins, need_cf=False):
    ident = A("ident", [128, 128], BF16)
    cf = A("cf32", [128, 512], F32) if need_cf else None
    epsb = A("mhalf", [128, 16], F32)
    P.dma("sp", ident[:], cins["c_ident"], w=["ident"])
    if need_cf:
        P.dma("sp", cf[:], cins["c_f32"], w=["cf"])
    P.dve(lambda e: e.memset(epsb[:], -0.5), w=["epsb"])
    return ident, cf, epsb


def phase_ffn(P, nc, name, src, dst, norm_w, wg, wu, wd, cins, final_w=None):
    with ExitStack() as st:
        def A(nm, shape, dt):
            return st.enter_context(nc.sbuf_tensor(f"{name}_{nm}", shape, dt))

        def PS(nm, shape, dt):
            return st.enter_context(nc.psum_tensor(f"{name}_{nm}", shape, dt))

        wgt = A("wg", [128, 8, FF], BF16)
        wut = A("wu", [128, 8, FF], BF16)
        wdt = A("wd", [128, NHC, D], BF16)
        xt = [A(f"xt{b}", [128, 4, D], F32) for b in range(2)]
        hT = A("hT", [128, 8, T], BF16)
        actb = A("act", [128, NHC, T], BF16)
        hn = A("hn", [128, D], BF16)
        sg = [A(f"sg{b}", [128, T], BF16) for b in range(2)]
        nw = A("nw", [128, D], F32)
        fw = A("fw", [128, D], F32) if final_w is not None else None
        ncol = NT * 4 * (2 if final_w is not None else 1)
        ss = A("ss", [128, ncol], F32)
        rs = A("rs", [128, ncol], F32)
        ident, cf, epsb = load_consts(P, A, cins)
        P.dve(lambda e: e.memset(epsb[:], EPS), r=["epsb"], w=["epsb"])
        pT = PS("pT", [128, D], BF16)
        pG = [PS(f"pG{b}", [128, T], F32) for b in range(2)]
        pU = [PS(f"pU{b}", [128, T], F32) for b in range(2)]
        pD = [PS(f"pD{b}", [128, T], F32) for b in range(2)]

        P.dve(lambda e: e.memset(ss[:], 0.0), w=[("ss", c) for c in range(ncol)])
        P.dma("sp", nw[:], norm_w.partition_broadcast(128), w=["nw"])
        if fw is not None:
            P.dma("sp", fw[:], final_w.partition_broadcast(128), w=["fw"])
        P.fence()
        for (wt, wsrc, key) in ((wgt, wg, "wg"), (wut, wu, "wu")):
            v = wsrc.rearrange("(k p) n -> p k n", p=128)
            for q in range(4):
                c0, c1 = q * 704, (q + 1) * 704
                P.dma("pool", wt[:, :, c0:c1], v[:, :, c0:c1], w=[(key, q)])
        vd = wd.rearrange("(k p) n -> p k n", p=128)
        for q in range(2):
            P.dma("pool", wdt[:, q * 11:(q + 1) * 11, :], vd[:, q * 11:(q + 1) * 11, :], w=[("wd", q)])

        def xview(ap, i):
            return ap[i * T:(i + 1) * T, :].rearrange("(j p) d -> p j d", p=128)

        def load(i):
            b = i % 2
            P.dma("sp", xt[b][:], xview(src, i), w=[("xt", b, j) for j in range(4)])

        def norm_sub(i, j):
            b = i % 2
            col = i * 4 + j
            emit_norm_T(P, xt[b][:, j, :], ("xt", b, j), nw[:], hn, "hn", ss, rs, col, epsb, ident, pT, "pT",
                        hT[:, :, j * 128:(j + 1) * 128], ("hT", j))

        load(0)
        for j in range(4):
            norm_sub(0, j)
        for i in range(NT):
            b = i % 2
            if i + 1 < NT:
                load(i + 1)
            for hc in range(NHC):
                q = hc * 128 // 704
                q2 = (hc * 128 + 127) // 704
                g, u = pG[hc % 2], pU[hc % 2]

                def mm_g(e, hc=hc, g=g):
                    inst = None
                    for k in range(8):
                        inst = e.matmul(g[:], lhsT=wgt[:, k, hc * 128:(hc + 1) * 128], rhs=hT[:, k, :],
                                        start=(k == 0), stop=(k == 7))
                    return inst

                def mm_u(e, hc=hc, u=u):
                    inst = None
                    for k in range(8):
                        inst = e.matmul(u[:], lhsT=wut[:, k, hc * 128:(hc + 1) * 128], rhs=hT[:, k, :],
                                        start=(k == 0), stop=(k == 7))
                    return inst

                hkeys = [("hT", j) for j in range(4)]
                P.pe(mm_g, r=hkeys + [("wg", q), ("wg", q2)], w=[("pG", hc % 2)])
                P.pe(mm_u, r=hkeys + [("wu", q), ("wu", q2)], w=[("pU", hc % 2)])
                sgb = sg[hc % 2]
                P.act(lambda e, g=g, sgb=sgb: e.activation(out=sgb[:], in_=g[:], func=AF.Silu),
                      r=[("pG", hc % 2)], w=[("sg", hc % 2)])
                P.dve(lambda e, u=u, sgb=sgb, hc=hc: e.tensor_tensor(out=actb[:, hc, :], in0=u[:], in1=sgb[:],
                                                                     op=ALU.mult),
                      r=[("pU", hc % 2), ("sg", hc % 2)], w=[("act", hc)])
            nd = 0
            for j in range(4):
                for half in range(2):
                    d_ = pD[nd % 2]
                    nd += 1

                    def mm_d(e, j=j, half=half, d_=d_):
                        inst = None
                        for hc in range(NHC):
                            inst = e.matmul(d_[:], lhsT=actb[:, hc, j * 128:(j + 1) * 128],
                                            rhs=wdt[:, hc, half * 512:(half + 1) * 512],
                                            start=(hc == 0), stop=(hc == NHC - 1))
                        return inst

                    P.pe(mm_d, r=[("act", hc) for hc in range(NHC)] + [("wd", 0), ("wd", 1)],
                         w=[("pD", (nd - 1) % 2)])
                    xs = xt[b][:, j, half * 512:(half + 1) * 512]
                    P.dve(lambda e, d_=d_, xs=xs: e.scalar_tensor_tensor(out=xs, in0=d_[:], scalar=0.5, in1=xs,
                                                                         op0=ALU.mult, op1=ALU.add),
                          r=[("pD", (nd - 1) % 2), ("xt", b, j)], w=[("xt", b, j)])
                if final_w is not None:
                    col = NT * 4 + i * 4 + j
                    xj = xt[b][:, j, :]
                    P.act(lambda e, xj=xj, col=col: e.activation(out=hn[:], in_=xj, func=AF.Square,
                                                                 accum_out=ss[:, col:col + 1]),
                          r=[("xt", b, j)], w=["hn", ("ss", col)])
                    P.act(lambda e, col=col: e.activation(out=rs[:, col:col + 1], in_=ss[:, col:col + 1],
                                                          func=AF.Sqrt, scale=1.0 / D, bias=epsb[:, 0:1]),
                          r=[("ss", col)], w=[("rs", col)])
                    P.dve(lambda e, col=col: e.reciprocal(out=rs[:, col:col + 1], in_=rs[:, col:col + 1]),
                          r=[("rs", col)], w=[("rs", col)])
                    P.dve(lambda e, xj=xj, col=col: e.scalar_tensor_tensor(out=xj, in0=xj, scalar=rs[:, col:col + 1],
                                                                           in1=fw[:], op0=ALU.mult, op1=ALU.mult),
                          r=[("xt", b, j), ("rs", col), "fw"], w=[("xt", b, j)])
                if i + 1 < NT and j >= 1:
                    for jj in ((0, 1) if j == 1 else (2,) if j == 2 else (3,)):
                        norm_sub(i + 1, jj)
            P.dma("sp", xview(dst, i), xt[b][:], r=[("xt", b, j) for j in range(4)])
        P.flush()


def phase_inproj(P, nc, x1s, pos, w, scr, cins):
    name = "ip"
    with ExitStack() as st:
        def A(nm, shape, dt):
            return st.enter_context(nc.sbuf_tensor(f"{name}_{nm}", shape, dt))

        def PS(nm, shape, dt):
            return st.enter_context(nc.psum_tensor(f"{name}_{nm}", shape, dt))

        win = A("win", [128, 8, IN_DIM], BF16)
        wuq = A("wuq", [128, 2, 768], BF16)
        wuk = A("wuk", [128, 512], BF16)
        wuv = A("wuv", [128, 512], BF16)
        xt = [A(f"xt{b}", [128, 4, D], F32) for b in range(2)]
        hT = A("hT", [128, 8, T], BF16)
        hn = A("hn", [128, D], BF16)
        nw = A("nw", [128, D], F32)
        qaw = A("qaw", [128, 384], F32)
        qkw = A("qkw", [128, 2, 96], F32)
        ss = A("ss", [128, NT * 4], F32)
        rs = A("rs", [128, NT * 4], F32)
        ss2 = A("ss2", [128, NT * 4, 2], F32)
        rs2 = A("rs2", [128, NT * 4, 2], F32)
        ss16 = A("ss16", [128, 4, 16], F32)
        rs16 = A("rs16", [128, 4, 16], F32)
        stage = Rot(A, "stg", 2, [128, 4, T], F32)
        cn = Rot(A, "cn", 2, [128, 384], BF16)
        cnT = Rot(A, "cnT", 2, [128, 3, 128], BF16)
        kpe = Rot(A, "kpe", 2, [128, 32], F32)
        gst = A("gst", [128, 4, 16], F32)
        vst = Rot(A, "vst", 2, [128, 512], BF16)
        qkf = Rot(A, "qkf", 2, [128, 16, 96], F32)
        sq = A("sq", [128, 16, 96], F32)
        rtmp = Rot(A, "rtmp", 1, [128, 4, 16, 16], F32)
        qkb = Rot(A, "qkb", 2, [128, 16, 96], BF16)
        qkTs = Rot(A, "qkTs", 1, [128, 16, T], BF16)
        posi = A("posi", [128, NCH], I32)
        posf = A("posf", [128, NCH], F32)
        inv = A("inv", [128, 16], F32)
        ang = qkf.t[0][:].rearrange("p h d -> p (h d)")[:, 0:1024].rearrange("p (c f) -> p c f", f=16)
        kf = qkf.t[1][:].rearrange("p h d -> p (h d)")[:, 0:1024].rearrange("p (c f) -> p c f", f=16)
        ki_t = A("ki", [128, NCH, 16], I32)
        ki = ki_t[:]
        cosT = A("cos", [128, NCH, 16], F32)
        sinT = A("sin", [128, NCH, 16], F32)
        hpi = A("hpi", [128, 1], F32)
        ident, cf, _mh = load_consts(P, A, cins)
        epsb = A("epsv", [128, 1], F32)
        P.dve(lambda e: e.memset(epsb[:], EPS), w=["epsv"])
        pT = PS("pT", [128, D], BF16)
        pF = [PS(f"pF{b}", [128, T], F32) for b in range(2)]
        pA = PS("pA", [128, T], F32)
        pQ = PS("pQ", [128, 1024], F32)
        pK, pV = pF[0], pF[1]
        pX = PS("pX", [128, 8 * 128], BF16)

        P.dve(lambda e: e.memset(ss[:], 0.0), w=[("ss", c) for c in range(NT * 4)])
        P.dve(lambda e: e.memset(ss2[:], 0.0), w=[("ss2", c) for c in range(NT * 4)])
        P.dve(lambda e: e.memset(hpi[:], math.pi / 2), w=["hpi"])
        P.dma("sp", nw[:], w["mix_norm_w"].partition_broadcast(128), w=["nw"])
        P.dma("sp", qaw[:, 0:256], w["q_a_norm_w"].partition_broadcast(128), w=["qaw"])
        P.dma("sp", qaw[:, 256:384], w["kv_a_norm_w"].partition_broadcast(128), w=["qaw2"])
        P.dma("sp", qkw[:, 0, :], w["q_norm_w"].partition_broadcast(128), w=["qkw0"])
        P.dma("sp", qkw[:, 1, :], w["k_norm_w"].partition_broadcast(128), w=["qkw1"])
        P.dma("sp", inv[:], cins["c_inv"].partition_broadcast(128), w=["inv"])
        P.dma("sp", posi[:], pos.rearrange("(c p) -> p c", p=128), w=["posi"], allow_slow_non_contiguous=True)
        wv = w["w_in"].rearrange("(k p) n -> p k n", p=128)
        P.dma("pool", win[:, :, 0:416], wv[:, :, 0:416], w=["winA"])
        P.dma("pool", win[:, :, 416:432], wv[:, :, 1440:1456], w=["winA2"])
        P.dma("pool", win[:, :, 432:1456], wv[:, :, 416:1440], w=[("winF", 0)])
        P.dma("pool", win[:, :, 1456:2480], wv[:, :, 1456:2480], w=[("winF", 1)])
        P.dma("pool", win[:, :, 2480:3504], wv[:, :, 2480:3504], w=[("winF", 2)])
        P.dma("pool", wuq[:], w["w_uq"].rearrange("(k p) n -> p k n", p=128), w=["wuq"])
        P.dma("pool", wuk[:], w["w_uk"], w=["wuk"])
        P.dma("pool", wuv[:], w["w_uv"], w=["wuv"])

        P.dve(lambda e: e.tensor_copy(out=posf[:], in_=posi[:]), r=["posi"], w=["posf"])
        P.dve(lambda e: e.tensor_tensor(out=ang, in0=bc(posf[:], [128, NCH, 16], 2),
                                        in1=bc(inv[:], [128, NCH, 16], 1), op=ALU.mult),
              r=["posf", "inv"], w=["ang"])
        P.dve(lambda e: e.tensor_scalar(out=kf, in0=ang, scalar1=1.0 / (2 * math.pi), scalar2=None,
                                        op0=ALU.mult), r=["ang"], w=["kf"])
        P.dve(lambda e: e.tensor_copy(out=ki, in_=kf), r=["kf"], w=["ki"])
        P.dve(lambda e: e.tensor_copy(out=kf, in_=ki), r=["ki"], w=["kf"])
        C1 = 6.28125
        C2 = 2 * math.pi - C1
        P.dve(lambda e: e.scalar_tensor_tensor(out=ang, in0=kf, scalar=-C1, in1=ang, op0=ALU.mult,
                                               op1=ALU.add), r=["kf", "ang"], w=["ang"])
        P.dve(lambda e: e.scalar_tensor_tensor(out=ang, in0=kf, scalar=-C2, in1=ang, op0=ALU.mult,
                                               op1=ALU.add), r=["kf", "ang"], w=["ang"])
        P.dve(lambda e: e.tensor_scalar(out=ang, in0=ang, scalar1=-3.1415925, scalar2=3.1415925,
                                        op0=ALU.max, op1=ALU.min), r=["ang"], w=["ang"])
        P.act(lambda e: e.activation(out=sinT[:], in_=ang, func=AF.Sin), r=["ang"], w=["sin"])
        P.dve(lambda e: e.scalar_tensor_tensor(out=kf, in0=ang, scalar=-1.0, in1=ang, op0=ALU.mult,
                                               op1=ALU.max), r=["ang"], w=["kf"])
        P.act(lambda e: e.activation(out=cosT[:], in_=kf, func=AF.Sin, scale=-1.0, bias=hpi[:]),
              r=["kf", "hpi"], w=["cos"])

        P.fence()

        def xview(ap, i):
            return ap[i * T:(i + 1) * T, :].rearrange("(j p) d -> p j d", p=128)

        def load(i):
            b = i % 2
            P.dma("sp", xt[b][:], xview(x1s, i), w=[("xt", b, j) for j in range(4)])

        def norm_sub(i, j):
            b = i % 2
            col = i * 4 + j
            emit_norm_T(P, xt[b][:, j, :], ("xt", b, j), nw[:], hn, "hn", ss, rs, col, epsb, ident, pT, "pT",
                        hT[:, :, j * 128:(j + 1) * 128], ("hT", j), evac="dve")

        fm_groups = [
            (scr["minT"], 432, None), (scr["sopT"], 944, AF.Sigmoid),
            (scr["sgT"][0:512, :], 1456, AF.Sigmoid), (scr["sgT"][512:1024, :], 1968, AF.Sigmoid),
            (scr["sgT"][1024:1536, :], 2480, AF.Sigmoid), (scr["sgT"][1536:2048, :], 2992, AF.Sigmoid),
        ]
        hkeys = [("hT", j) for j in range(4)]
        wkeys = ["winA", "winA2"] + [("winF", q) for q in range(3)]
        nf = 0
        load(0)
        for j in range(4):
            norm_sub(0, j)
        for i in range(NT):
            b = i % 2
            t0 = i * T
            if i + 1 < NT:
                load(i + 1)
            for (dram, col0, func) in fm_groups:
                stg, skey = stage.next()
                for cc in range(4):
                    pf = pF[nf % 2]
                    pkey = ("pF", nf % 2)
                    nf += 1

                    def mm_f(e, pf=pf, c0=col0 + cc * 128):
                        inst = None
                        for k in range(8):
                            inst = e.matmul(pf[:], lhsT=win[:, k, c0:c0 + 128], rhs=hT[:, k, :],
                                            start=(k == 0), stop=(k == 7))
                        return inst

                    P.pe(mm_f, r=hkeys + wkeys, w=[pkey])
                    if func is None:
                        P.dve(lambda e, pf=pf, stg=stg, cc=cc: e.tensor_copy(out=stg[:, cc, :], in_=pf[:]),
                              r=[pkey], w=[skey + (cc,)])
                    else:
                        P.act(lambda e, pf=pf, stg=stg, cc=cc, func=func: e.activation(out=stg[:, cc, :], in_=pf[:],
                                                                                      func=func),
                              r=[pkey], w=[skey + (cc,)])
                P.dma("sp", dram[:, t0:t0 + T].rearrange("(c p) t -> p c t", p=128), stg[:],
                      r=[skey + (cc,) for cc in range(4)])
            qs, qskey = qkTs.next()
            for j in range(4):
                c = i * 4 + j
                tok0 = t0 + j * 128

                def mm_a(e, j=j):
                    inst = None
                    for k in range(8):
                        inst = e.matmul(pA[:, 0:432], lhsT=hT[:, k, j * 128:(j + 1) * 128], rhs=win[:, k, 0:432],
                                        start=(k == 0), stop=(k == 7))
                    return inst

                P.pe(mm_a, r=hkeys + wkeys, w=["pA"])
                cnb, cnkey = cn.next()
                kp, kpkey = kpe.next()
                P.act(lambda e, c=c: e.activation(out=hn[:, 0:256], in_=pA[:, 0:256], func=AF.Square,
                                                  accum_out=ss2[:, c, 0:1]), r=["pA"], w=["hn", ("ss2", c)])
                P.act(lambda e, c=c: e.activation(out=hn[:, 256:384], in_=pA[:, 256:384], func=AF.Square,
                                                  accum_out=ss2[:, c, 1:2]), r=["pA"], w=["hn", ("ss2", c)])
                P.act(lambda e, c=c: e.activation(out=rs2[:, c, 0:1], in_=ss2[:, c, 0:1], func=AF.Sqrt,
                                                  scale=1.0 / 256, bias=epsb[:]), r=[("ss2", c)], w=[("rs2", c)])
                P.act(lambda e, c=c: e.activation(out=rs2[:, c, 1:2], in_=ss2[:, c, 1:2], func=AF.Sqrt,
                                                  scale=1.0 / 128, bias=epsb[:]), r=[("rs2", c)], w=[("rs2", c)])
                P.dve(lambda e, c=c: e.reciprocal(out=rs2[:, c, :], in_=rs2[:, c, :]), r=[("rs2", c)],
                      w=[("rs2", c)])
                P.dve(lambda e, c=c, cnb=cnb: e.scalar_tensor_tensor(out=cnb[:, 0:256], in0=pA[:, 0:256],
                                                                     scalar=rs2[:, c, 0:1], in1=qaw[:, 0:256],
                                                                     op0=ALU.mult, op1=ALU.mult),
                      r=["pA", ("rs2", c), "qaw"], w=[cnkey + (0,)])
                P.dve(lambda e, c=c, cnb=cnb: e.scalar_tensor_tensor(out=cnb[:, 256:384], in0=pA[:, 256:384],
                                                                     scalar=rs2[:, c, 1:2], in1=qaw[:, 256:384],
                                                                     op0=ALU.mult, op1=ALU.mult),
                      r=["pA", ("rs2", c), "qaw2"], w=[cnkey + (1,)])
                P.dve(lambda e, kp=kp: e.tensor_copy(out=kp[:], in_=pA[:, 384:416]), r=["pA"], w=[kpkey])
                P.dve(lambda e, j=j: e.tensor_copy(out=gst[:, j, :], in_=pA[:, 416:432]), r=["pA"], w=[("gst", j)])
                cT, cTkey = cnT.next()

                def tr3(e, cnb=cnb):
                    inst = None
                    for k in range(3):
                        inst = e.transpose(pT[:, k * 128:(k + 1) * 128], cnb[:, k * 128:(k + 1) * 128], ident[:])
                    return inst

                P.pe(tr3, r=[cnkey + (0,), cnkey + (1,)], w=["pT"])
                P.dve(lambda e, cT=cT: e.tensor_copy(out=cT[:], in_=pT[:, 0:384].rearrange("p (k t) -> p k t", k=3)),
                      r=["pT"], w=[cTkey])

                def mm_q(e, cT=cT):
                    e.matmul(pQ[:, 0:512], lhsT=cT[:, 0, :], rhs=wuq[:, 0, 0:512], start=True, stop=False)
                    e.matmul(pQ[:, 0:512], lhsT=cT[:, 1, :], rhs=wuq[:, 1, 0:512], start=False, stop=True)
                    e.matmul(pQ[:, 512:768], lhsT=cT[:, 0, :], rhs=wuq[:, 0, 512:768], start=True, stop=False)
                    return e.matmul(pQ[:, 512:768], lhsT=cT[:, 1, :], rhs=wuq[:, 1, 512:768], start=False, stop=True)

                P.pe(mm_q, r=[cTkey, "wuq"], w=["pQ"])
                P.pe(lambda e, cT=cT: e.matmul(pK[:], lhsT=cT[:, 2, :], rhs=wuk[:], start=True, stop=True),
                     r=[cTkey, "wuk"], w=[("pF", 0)])
                P.pe(lambda e, cT=cT: e.matmul(pV[:], lhsT=cT[:, 2, :], rhs=wuv[:], start=True, stop=True),
                     r=[cTkey, "wuv"], w=[("pF", 1)])
                vs, vskey = vst.next()
                P.act(lambda e, vs=vs: e.copy(out=vs[:], in_=pV[:]), r=[("pF", 1)], w=[vskey])
                P.dma("sp", scr["vS"][tok0:tok0 + 128, :], vs[:], r=[vskey])
                qf, qfkey = qkf.next()
                P.act(lambda e, qf=qf: e.copy(out=qf[:, 0:8, :].rearrange("p h d -> p (h d)"), in_=pQ[:, 0:768]),
                      r=["pQ"], w=[qfkey + ("q",)])
                P.dve(lambda e, qf=qf: e.tensor_copy(out=qf[:, 8:16, 0:64],
                                                     in_=pK[:].rearrange("p (h d) -> p h d", h=8)),
                      r=[("pF", 0)], w=[qfkey + ("k",)])
                P.pool(lambda e, qf=qf, kp=kp: e.tensor_copy(out=qf[:, 8:16, 64:96], in_=bc(kp[:], [128, 8, 32], 1)),
                       r=[kpkey], w=[qfkey + ("kp",)])
                qkeys = [qfkey + (s_,) for s_ in ("q", "k", "kp")]
                P.pool(lambda e, qf=qf: e.tensor_tensor(out=sq[:], in0=qf[:], in1=qf[:], op=ALU.mult),
                       r=qkeys, w=["sq"])
                P.dve(lambda e, j=j: e.tensor_reduce(out=ss16[:, j, :], in_=sq[:], axis=AX.X, op=ALU.add),
                      r=["sq"], w=[("ss16", j)])
                P.act(lambda e, j=j: e.activation(out=rs16[:, j, :], in_=ss16[:, j, :], func=AF.Sqrt,
                                                  scale=1.0 / 96, bias=epsb[:]), r=[("ss16", j)], w=[("rs16", j)])
                P.dve(lambda e, j=j: e.reciprocal(out=rs16[:, j, :], in_=rs16[:, j, :]), r=[("rs16", j)],
                      w=[("rs16", j)])
                P.dve(lambda e, qf=qf, j=j: e.tensor_tensor(out=qf[:], in0=qf[:],
                                                            in1=bc(rs16[:, j, :], [128, 16, 96], 2), op=ALU.mult),
                      r=qkeys + [("rs16", j)], w=qkeys)
                qb, qbkey = qkb.next()
                for hh in range(2):
                    eng = P.pool if hh == 0 else P.dve
                    eng(lambda e, qf=qf, qb=qb, hh=hh: e.tensor_tensor(
                        out=qb[:, hh * 8:(hh + 1) * 8, 0:64], in0=qf[:, hh * 8:(hh + 1) * 8, 0:64],
                        in1=bc(qkw[:, hh, 0:64], [128, 8, 64], 1), op=ALU.mult),
                        r=qkeys + ["qkw0", "qkw1"], w=[qbkey + ("n", hh)])
                    eng(lambda e, qf=qf, hh=hh: e.tensor_tensor(
                        out=qf[:, hh * 8:(hh + 1) * 8, 64:96], in0=qf[:, hh * 8:(hh + 1) * 8, 64:96],
                        in1=bc(qkw[:, hh, 64:96], [128, 8, 32], 1), op=ALU.mult),
                        r=qkeys + ["qkw0", "qkw1"], w=qkeys)
                rt, rtkey = rtmp.next()
                cb_ = bc(cosT[:, c, :], [128, 16, 16], 1)
                sb_ = bc(sinT[:, c, :], [128, 16, 16], 1)
                x1_ = lambda qf: qf[:, :, 64:80]
                x2_ = lambda qf: qf[:, :, 80:96]
                P.pool(lambda e, qf=qf, rt=rt, cb_=cb_: e.tensor_tensor(out=rt[:, 0], in0=x1_(qf), in1=cb_, op=ALU.mult),
                       r=qkeys + ["cos"], w=[rtkey + (0,)])
                P.pool(lambda e, qf=qf, rt=rt, sb_=sb_: e.tensor_tensor(out=rt[:, 1], in0=x2_(qf), in1=sb_, op=ALU.mult),
                       r=qkeys + ["sin"], w=[rtkey + (1,)])
                P.dve(lambda e, qf=qf, rt=rt, cb_=cb_: e.tensor_tensor(out=rt[:, 2], in0=x2_(qf), in1=cb_, op=ALU.mult),
                      r=qkeys + ["cos"], w=[rtkey + (2,)])
                P.dve(lambda e, qf=qf, rt=rt, sb_=sb_: e.tensor_tensor(out=rt[:, 3], in0=x1_(qf), in1=sb_, op=ALU.mult),
                      r=qkeys + ["sin"], w=[rtkey + (3,)])
                P.pool(lambda e, qb=qb, rt=rt: e.tensor_tensor(out=qb[:, :, 64:80], in0=rt[:, 0], in1=rt[:, 1],
                                                               op=ALU.subtract),
                       r=[rtkey + (0,), rtkey + (1,)], w=[qbkey + ("r1",)])
                P.dve(lambda e, qb=qb, rt=rt: e.tensor_tensor(out=qb[:, :, 80:96], in0=rt[:, 2], in1=rt[:, 3],
                                                              op=ALU.add),
                      r=[rtkey + (2,), rtkey + (3,)], w=[qbkey + ("r2",)])
                qbkeys = [qbkey + ("n", 0), qbkey + ("n", 1), qbkey + ("r1",), qbkey + ("r2",)]

                for h0 in range(2):
                    def tr8(e, qb=qb, h0=h0):
                        inst = None
                        for hh in range(8):
                            inst = e.transpose(pX[0:96, hh * 128:(hh + 1) * 128], qb[:, h0 * 8 + hh, :], ident[:])
                        return inst

                    P.pe(tr8, r=qbkeys, w=["pX"])
                    P.act(lambda e, qs=qs, j=j, h0=h0: e.copy(
                        out=qs[0:96, h0 * 8:(h0 + 1) * 8, j * 128:(j + 1) * 128],
                        in_=pX[0:96, :].rearrange("p (h t) -> p h t", h=8)), r=["pX"], w=[qskey + (j, h0)])
                if i + 1 < NT:
                    norm_sub(i + 1, j)
            P.dma("sp", scr["gates"][t0:t0 + T, :].rearrange("(j p) g -> p j g", p=128), gst[:],
                  r=[("gst", j) for j in range(4)])
            P.dma("sp", scr["qkT"][:, :, t0:t0 + T].rearrange("h d t -> d h t"), qs[0:96, :, :],
                  r=[qskey + (j, h0) for j in range(4) for h0 in range(2)])
        P.flush()


def phase_attn(P, nc, scr, cins):
    name = "at"
    with ExitStack() as st:
        def A(nm, shape, dt):
            return st.enter_context(nc.sbuf_tensor(f"{name}_{nm}", shape, dt))

        def PS(nm, shape, dt):
            return st.enter_context(nc.psum_tensor(f"{name}_{nm}", shape, dt))

        kt = Rot(A, "kt", 2, [128, S], BF16)
        qt = Rot(A, "qt", 2, [128, S], BF16)
        va = Rot(A, "va", 2, [128, NCH, 65], BF16)
        pt = Rot(A, "pt", 3, [128, 2 * T], BF16)
        rden = Rot(A, "rden", 2, [128, T], F32)
        rbs = Rot(A, "rbs", 2, [64, T], F32)
        ast = Rot(A, "ast", 2, [64, T], BF16)
        ones = A("ones", [128, 64], F32)
        pS = [PS(f"pS{b}", [128, 2 * T], F32) for b in range(2)]
        pO = [PS(f"pO{b}", [128, T], F32) for b in range(2)]
        pR = PS("pR", [128, T], F32)
        P.dve(lambda e: e.memset(ones[:], 1.0), w=["ones"])
        for b in range(2):
            P.dve(lambda e, b=b: e.memset(va.t[b][:, :, 64:65], 1.0), w=[("va1", b)])
        ns = 0
        no = 0
        NP2 = NCH // 2
        tail2 = None
        def head_loads(h):
            ktb, ktkey = kt.next()
            qtb, qtkey = qt.next()
            vab, vakey = va.next()
            P.dma("sp", ktb[0:96, :], scr["qkT"][8 + h], w=[ktkey])
            P.dma("sp", qtb[0:96, :], scr["qkT"][h], w=[qtkey])
            for q4 in range(4):
                P.dma("sp", vab[:, q4 * 16:(q4 + 1) * 16, 0:64],
                      scr["vS"][q4 * 2048:(q4 + 1) * 2048, h * 64:(h + 1) * 64].rearrange("(c p) d -> p c d", p=128),
                      w=[vakey + (q4,)])
            vkeys = [vakey + (q4,) for q4 in range(4)] + [("va1", va.i % 2)]
            return ktb, ktkey, qtb, qtkey, vab, vkeys

        hl = {0: head_loads(0)}
        for h in range(8):
            ktb, ktkey, qtb, qtkey, vab, vkeys = hl.pop(h)
            if h + 1 < 8:
                hl[h + 1] = head_loads(h + 1)
            for g in range(NT):
                po = pO[no % 2]
                pokey = ("pO", no % 2)
                no += 1
                qsl = qtb[0:96, g * T:(g + 1) * T]
                pend = []

                def qk(kp):
                    nonlocal ns
                    ps = pS[ns % 2]
                    pskey = ("pS", ns % 2)
                    ns += 1

                    def mm(e, ps=ps, kp=kp, ktb=ktb, qsl=qsl):
                        e.matmul(ps[:, 0:T], lhsT=ktb[0:96, (2 * kp) * 128:(2 * kp + 1) * 128], rhs=qsl, start=True,
                                 stop=True)
                        return e.matmul(ps[:, T:2 * T], lhsT=ktb[0:96, (2 * kp + 1) * 128:(2 * kp + 2) * 128], rhs=qsl,
                                        start=True, stop=True)

                    P.pe(mm, r=[ktkey, qtkey], w=[pskey])
                    return ps, pskey

                pend.append(qk(0))
                for kp in range(NP2):
                    if kp + 1 < NP2:
                        pend.append(qk(kp + 1))
                    ps, pskey = pend.pop(0)
                    ptb, ptkey = pt.next()
                    P.act(lambda e, ps=ps, ptb=ptb: e.activation(out=ptb[:], in_=ps[:], func=AF.Exp, scale=ATT_SCALE),
                          r=[pskey], w=[ptkey])

                    def pv(e, ptb=ptb, kp=kp, po=po, vab=vab):
                        e.matmul(po[0:65, :], lhsT=vab[:, 2 * kp, :], rhs=ptb[:, 0:T], start=(kp == 0), stop=False)
                        return e.matmul(po[0:65, :], lhsT=vab[:, 2 * kp + 1, :], rhs=ptb[:, T:2 * T], start=False,
                                        stop=(kp == NP2 - 1))

                    P.pe(pv, r=[ptkey] + vkeys, w=[pokey])
                    if kp == 2 and tail2 is not None:
                        tail2()
                        tail2 = None
                rd, rdkey = rden.next()
                rb, rbkey = rbs.next()
                ab, abkey = ast.next()
                P.dve(lambda e, rd=rd, po=po: e.reciprocal(out=rd[64:65, :], in_=po[64:65, :]), r=[pokey], w=[rdkey])

                def tail(rd=rd, rdkey=rdkey, rb=rb, rbkey=rbkey, ab=ab, abkey=abkey, po=po, pokey=pokey, h=h, g=g):
                    P.pe(lambda e: e.matmul(pR[0:64, :], lhsT=ones[64:65, 0:64], rhs=rd[64:65, :], start=True,
                                            stop=True), r=[rdkey, "ones"], w=["pR"])
                    P.dve(lambda e: e.tensor_copy(out=rb[:], in_=pR[0:64, :]), r=["pR"], w=[rbkey])
                    P.dve(lambda e: e.tensor_tensor(out=ab[:], in0=po[0:64, :], in1=rb[:], op=ALU.mult),
                          r=[pokey, rbkey], w=[abkey])
                    P.dma("sp", scr["aT"][h * 64:(h + 1) * 64, g * T:(g + 1) * T], ab[:], r=[abkey])

                tail2 = tail
        tail2()
        P.flush()


def phase_mlstm(P, nc, w, scr, cins):
    name = "ml"
    with ExitStack() as st:
        def A(nm, shape, dt):
            return st.enter_context(nc.sbuf_tensor(f"{name}_{nm}", shape, dt))

        def PS(nm, shape, dt):
            return st.enter_context(nc.psum_tensor(f"{name}_{nm}", shape, dt))

        ident, cf, epsb = load_consts(P, A, cins, need_cf=True)
        identf = cf[:, 0:128]
        triLE = cf[:, 128:256]
        triGE = cf[:, 256:384]
        onesf = cf[:, 384:512]
        G = A("G", [128, NCH, 16], F32)
        bi = A("bi", [128, 8], F32)
        bf_ = A("bf", [128, 8], F32)
        GI = A("GI", [128, 2, NCH, 4], F32)
        GF = A("GF", [128, 2, NCH, 4], F32)
        t1 = A("t1", [128, 512], F32)
        t2 = A("t2", [128, 512], F32)
        LF = A("LF", [128, 512], F32)
        Bc = A("Bc", [128, 512], F32)
        CS = A("CS", [128, 512], F32)
        BL = A("BL", [128, 512], F32)
        MXB = A("MXB", [128, 512], F32)
        mxcol = A("mxcol", [128, 4], F32)
        dg = A("dg", [128, 512], F32)
        M_all = A("M_all", [128, 512], F32)
        DARG = A("DARG", [128, 512], F32)
        DEC = A("DEC", [128, 512], F32)
        A_all = A("A_all", [128, 512], F32)
        THR = A("THR", [128, 512], F32)
        mcur = A("mcur", [128, 8], F32)
        wq = A("wq", [128, 4, 128], BF16)
        wk = A("wk", [128, 4, 128], BF16)
        wvv = A("wv", [128, 4, 128], BF16)
        cw = A("cw", [128, 4, 5], F32)
        cbias = A("cb", [128, 4], F32)
        mnw = A("mnw", [128, 4], F32)
        msk = A("msk", [128, 4], F32)
        p0 = PS("p0", [128, 512], F32)
        p1 = PS("p1", [128, 512], F32)
        pS_ = PS("pS", [128, 512], F32)
        pKV = PS("pKV", [128, 1024], F32)
        pN = PS("pN", [128, 1024], F32)
        pH = PS("pH", [128, 512], F32)

        for q4 in range(4):
            P.dma("sp", G[:, q4 * 16:(q4 + 1) * 16, :],
                  scr["gates"][q4 * 2048:(q4 + 1) * 2048, :].rearrange("(c p) g -> p c g", p=128), w=[("G", q4)])
        gk = [("G", q4) for q4 in range(4)]
        P.dma("sp", bi[:], w["b_igate"].partition_broadcast(128), w=["bi"])
        P.dma("sp", bf_[:], w["b_fgate"].partition_broadcast(128), w=["bf"])
        for (wt, key) in ((wq, "w_mq"), (wk, "w_mk"), (wvv, "w_mv")):
            P.dma("pool", wt[:], w[key].rearrange("h d e -> d h e"), w=[key])
        for jj in range(5):
            P.dma("sp", cw[:, :, jj], w["conv_w"][jj].rearrange("(k p) -> p k", p=128), w=[("cw", jj)],
                  allow_slow_non_contiguous=True)
        P.dma("sp", cbias[:], w["conv_b"].rearrange("(k p) -> p k", p=128), w=["cb"], allow_slow_non_contiguous=True)
        P.dma("sp", mnw[:], w["m_norm_w"].rearrange("(k p) -> p k", p=128), w=["mnw"], allow_slow_non_contiguous=True)
        P.dma("sp", msk[:], w["m_skip"].rearrange("(k p) -> p k", p=128), w=["msk"], allow_slow_non_contiguous=True)

        dgw = A("dgw", [128, 4, 5, 128], BF16)
        cbr = A("cbr", [1, 512], F32)
        cbr2 = A("cbr2", [1, 512], F32)
        brow = A("brow", [1, 2, 512], BF16)
        onesb = A("onesb", [1, 128], BF16)
        P.dma("sp", cbr[:], w["conv_b"].rearrange("(o n) -> o n", o=1), w=["cbr"])
        P.dve(lambda e: e.memset(onesb[:], 1.0), w=["onesb"])
        P.fence()
        for hc in range(4):
            for jj in range(5):
                P.dve(lambda e, hc=hc, jj=jj: e.tensor_scalar(out=dgw[:, hc, jj, :], in0=identf,
                                                              scalar1=cw[:, hc, jj:jj + 1], scalar2=None,
                                                              op0=ALU.mult), w=[("dgw", hc, jj)])
        P.dve(lambda e: e.tensor_copy(out=brow[:, 0, :], in_=cbr[:]), w=["brow0"])
        P.dve(lambda e: e.tensor_copy(out=cbr2[:], in_=brow[:, 0, :]), r=["brow0"], w=["cbr2"])
        P.dve(lambda e: e.tensor_tensor(out=cbr2[:], in0=cbr[:], in1=cbr2[:], op=ALU.subtract), r=["cbr2"], w=["cbr2"])
        P.dve(lambda e: e.tensor_copy(out=brow[:, 1, :], in_=cbr2[:]), r=["cbr2"], w=["brow1"])
        for d in range(2):
            P.dve(lambda e, d=d: e.tensor_tensor(out=GI[:, d], in0=G[:, :, d * 4:d * 4 + 4],
                                                 in1=bc(bi[:, d * 4:d * 4 + 4], [128, NCH, 4], 1), op=ALU.add),
                  r=gk + ["bi"], w=[("GI", d)])
            P.dve(lambda e, d=d: e.tensor_tensor(out=GF[:, d], in0=G[:, :, 8 + d * 4:12 + d * 4],
                                                 in1=bc(bf_[:, d * 4:d * 4 + 4], [128, NCH, 4], 1), op=ALU.add),
                  r=gk + ["bf"], w=[("GF", d)])
        GFf = GF[:].rearrange("p d c h -> p (d c h)")
        GIf = GI[:].rearrange("p d c h -> p (d c h)")
        gfk = [("GF", 0), ("GF", 1)]
        gik = [("GI", 0), ("GI", 1)]
        P.dve(lambda e: e.scalar_tensor_tensor(out=t1[:], in0=GFf, scalar=-1.0, in1=GFf, op0=ALU.mult, op1=ALU.max),
              r=gfk, w=["t1"])
        P.act(lambda e: e.activation(out=t1[:], in_=t1[:], func=AF.Exp, scale=-1.0), r=["t1"], w=["t1"])
        P.act(lambda e: e.activation(out=t1[:], in_=t1[:], func=AF.Ln, bias=1.0), r=["t1"], w=["t1"])
        P.dve(lambda e: e.tensor_scalar(out=t2[:], in0=GFf, scalar1=0.0, scalar2=None, op0=ALU.min), r=gfk, w=["t2"])
        P.dve(lambda e: e.tensor_tensor(out=LF[:], in0=t2[:], in1=t1[:], op=ALU.subtract), r=["t1", "t2"], w=["LF"])
        P.pe(lambda e: e.matmul(p0[:, 0:256], lhsT=triLE, rhs=LF[:, 0:256], start=True, stop=True),
             r=["LF", "cf"], w=["p0a"])
        P.pe(lambda e: e.matmul(p0[:, 256:512], lhsT=triGE, rhs=LF[:, 256:512], start=True, stop=True),
             r=["LF", "cf"], w=["p0b"])
        P.pe(lambda e: e.matmul(p1[:], lhsT=onesf, rhs=LF[:], start=True, stop=True), r=["LF", "cf"], w=["p1"])
        P.dve(lambda e: e.tensor_copy(out=Bc[:], in_=p0[:]), r=["p0a", "p0b"], w=["Bc"])
        P.act(lambda e: e.copy(out=BL[:], in_=p1[:]), r=["p1"], w=["BL"])
        P.dve(lambda e: e.tensor_tensor(out=CS[:], in0=GIf, in1=Bc[:], op=ALU.subtract), r=gik + ["Bc"], w=["CS"])

        def trf(e):
            inst = None
            for k in range(4):
                inst = e.transpose(p0[:, k * 128:(k + 1) * 128], CS[:, k * 128:(k + 1) * 128], identf)
            return inst

        P.pe(trf, r=["CS", "cf", "Bc"], w=["p0a", "p0b"])
        P.dve(lambda e: e.tensor_reduce(out=mxcol[:], in_=p0[:].rearrange("p (k s) -> p k s", k=4), axis=AX.X,
                                        op=ALU.max), r=["p0a", "p0b"], w=["mxcol"])
        for k in range(4):
            P.dve(lambda e, k=k: e.tensor_scalar(out=dg[:, k * 128:(k + 1) * 128], in0=identf,
                                                 scalar1=mxcol[:, k:k + 1], scalar2=None, op0=ALU.mult),
                  r=["mxcol", "cf"], w=[("dg", k)])
        P.pe(lambda e: e.matmul(p1[:], lhsT=onesf, rhs=dg[:], start=True, stop=True),
             r=[("dg", k) for k in range(4)] + ["cf", "BL"], w=["p1"])
        P.dve(lambda e: e.tensor_copy(out=MXB[:], in_=p1[:]), r=["p1"], w=["MXB"])
        P.dve(lambda e: e.memset(mcur[:], 0.0), w=[("mcur", 0), ("mcur", 1)])
        for jstep in range(NCH):
            for d in range(2):
                c = jstep if d == 0 else NCH - 1 - jstep
                sl = slice(d * 256 + c * 4, d * 256 + c * 4 + 4)
                mc = mcur[:, d * 4:d * 4 + 4]
                eng = P.dve
                eng(lambda e, sl=sl, mc=mc: e.tensor_tensor(out=M_all[:, sl], in0=mc, in1=MXB[:, sl], op=ALU.max),
                    r=[("mcur", d), "MXB"], w=[("M", d, c)])
                eng(lambda e, sl=sl, mc=mc: e.tensor_tensor(out=DARG[:, sl], in0=mc, in1=M_all[:, sl],
                                                            op=ALU.subtract),
                    r=[("mcur", d), ("M", d, c)], w=[("DARG", d, c)])
                eng(lambda e, sl=sl, mc=mc: e.tensor_tensor(out=mc, in0=M_all[:, sl], in1=BL[:, sl], op=ALU.add),
                    r=[("M", d, c), "BL", ("DARG", d, c)], w=[("mcur", d)])
        mk = [("M", d, c) for d in range(2) for c in range(NCH)]
        dk = [("DARG", d, c) for d in range(2) for c in range(NCH)]
        P.act(lambda e: e.activation(out=DEC[:], in_=DARG[:], func=AF.Exp), r=dk, w=["DEC"])
        P.dve(lambda e: e.tensor_tensor(out=t1[:], in0=CS[:], in1=M_all[:], op=ALU.subtract), r=["CS"] + mk, w=["t1"])
        P.act(lambda e: e.activation(out=A_all[:], in_=t1[:], func=AF.Exp), r=["t1"], w=["A_all"])
        P.dve(lambda e: e.tensor_tensor(out=t2[:], in0=Bc[:], in1=M_all[:], op=ALU.add), r=["Bc"] + mk, w=["t2"])
        P.act(lambda e: e.activation(out=THR[:], in_=t2[:], func=AF.Exp, scale=-1.0), r=["t2"], w=["THR"])
        P.flush()

        mt = Rot(A, "mt", 5, [128, 4, 132], F32)
        xcT = Rot(A, "xcT", 3, [128, 4, 128], F32)
        xcb = Rot(A, "xcb", 2, [128, 4, 128], BF16)
        mb = Rot(A, "mb", 2, [128, 4, 132], BF16)
        qTb = Rot(A, "qTb", 2, [128, 4, 128], BF16)
        kTb = Rot(A, "kTb", 2, [128, 4, 128], BF16)
        kb = Rot(A, "kb", 2, [128, 4, 128], BF16)
        vab = Rot(A, "vab", 2, [128, 4, 129], BF16)
        scm = Rot(A, "scm", 2, [128, 4, 128], BF16)
        Cd = Rot(A, "Cd", 2, [128, 4, 129], F32)
        KVs = Rot(A, "KVs", 2, [128, 4, 129], F32)
        Cdb = Rot(A, "Cdb", 2, [128, 4, 129], BF16)
        Call = A("Call", [128, 4, 129], F32)
        sm = Rot(A, "sm", 2, [128, 4, 4], F32)
        hout = Rot(A, "hout", 2, [128, 4, 128], F32)
        hbt = Rot(A, "hbt", 5, [128, 4, 128], F32)
        sopt = Rot(A, "sopt", 5, [128, 4, 128], F32)
        hsq = A("hsq", [128, 4, 128], F32)
        hss = Rot(A, "hss", 2, [128, 2, 4], F32)
        ut = Rot(A, "ut", 2, [128, 4, 128], F32)
        bmst = Rot(A, "bmst", 2, [128, 4, T], BF16)
        mask = {0: triLE, 1: triGE}

        for d in (1, 0):
            P.dve(lambda e: e.memset(Call[:], 0.0), w=["Call"])
            order = range(NCH) if d == 0 else range(NCH - 1, -1, -1)
            bst = {"bms": None, "key": None}

            def chunk_body(c, d=d, bst=bst):
                t0 = c * 128
                sl = slice(d * 256 + c * 4, d * 256 + c * 4 + 4)
                m_, mkey = mt.next()
                lo, hi = max(t0 - 2, 0), min(t0 + 130, S)
                o0 = lo - (t0 - 2)
                wl = [mkey]
                if c == 0:
                    P.pool(lambda e, m_=m_: e.memset(m_[:, :, 0:2], 0.0), w=[mkey + ("z",)])
                    wl.append(mkey + ("z",))
                if c == NCH - 1:
                    P.pool(lambda e, m_=m_: e.memset(m_[:, :, 130:132], 0.0), w=[mkey + ("z",)])
                    wl.append(mkey + ("z",))
                P.dma("sp", m_[:, :, o0:o0 + (hi - lo)], scr["minT"][:, lo:hi].rearrange("(k p) t -> p k t", p=128),
                      w=[mkey], r=[mkey + ("z",)])
                mkeys = [mkey]
                if d == 0:
                    hb_, hbkey = hbt.next()
                    so_, sokey = sopt.next()
                    P.dma("sp", hb_[:].rearrange("p h e -> p (h e)"), scr["hb"][t0:t0 + 128, :], w=[hbkey])
                    P.dma("sp", so_[:], scr["sopT"][:, t0:t0 + 128].rearrange("(k p) t -> p k t", p=128), w=[sokey])
                yield
                mbb, mbkey = mb.next()
                P.pool(lambda e, mbb=mbb, m_=m_: e.tensor_copy(out=mbb[:], in_=m_[:]), r=mkeys, w=[mbkey])

                def mm_conv(e, mbb=mbb):
                    inst = None
                    for hc in range(4):
                        o_ = p0[:, hc * 128:(hc + 1) * 128]
                        for jj in range(5):
                            e.matmul(o_, lhsT=dgw[:, hc, jj, :], rhs=mbb[:, hc, jj:jj + 128], start=(jj == 0), stop=False)
                        e.matmul(o_, lhsT=brow[0:1, 0, hc * 128:(hc + 1) * 128], rhs=onesb[0:1, :], start=False, stop=False)
                        inst = e.matmul(o_, lhsT=brow[0:1, 1, hc * 128:(hc + 1) * 128], rhs=onesb[0:1, :], start=False,
                                        stop=True)
                    return inst

                P.pe(mm_conv, r=[mbkey, "dgw", "brow"], w=["p0"])
                xb, xbkey = xcb.next()
                P.act(lambda e, xb=xb: e.activation(out=xb[:].rearrange("p h t -> p (h t)"), in_=p0[:], func=AF.Silu),
                      r=["p0"], w=[xbkey])
                if d == 0:
                    xf, xfkey = xcT.next()
                    P.act(lambda e, xf=xf: e.activation(out=xf[:].rearrange("p h t -> p (h t)"), in_=p0[:],
                                                        func=AF.Silu), r=["p0"], w=[xfkey])
                qT_, qTkey = qTb.next()
                kT_, kTkey = kTb.next()
                k_, kkey = kb.next()
                va_, vakey = vab.next()

                def proj(e, wt, x, out_p, tmaj):
                    inst = None
                    for h in range(4):
                        if tmaj:
                            inst = e.matmul(out_p[:, h * 128:(h + 1) * 128], lhsT=x[:, h, :], rhs=wt[:, h, :],
                                            start=True, stop=True)
                        else:
                            inst = e.matmul(out_p[:, h * 128:(h + 1) * 128], lhsT=wt[:, h, :], rhs=x[:, h, :],
                                            start=True, stop=True)
                    return inst

                P.pe(lambda e, xb=xb: proj(e, wq, xb, p1, False), r=[xbkey, "w_mq"], w=["p1"])
                P.act(lambda e, qT_=qT_: e.copy(out=qT_[:].rearrange("p h t -> p (h t)"), in_=p1[:]), r=["p1"],
                      w=[qTkey])
                P.pe(lambda e, xb=xb: proj(e, wk, xb, p0, False), r=[xbkey, "w_mk"], w=["p0"])
                P.act(lambda e, kT_=kT_: e.mul(out=kT_[:].rearrange("p h t -> p (h t)"), in_=p0[:], mul=128 ** -0.5),
                      r=["p0"], w=[kTkey])
                P.pe(lambda e, xb=xb: proj(e, wk, xb, p1, True), r=[xbkey, "w_mk"], w=["p1"])
                P.act(lambda e, k_=k_: e.mul(out=k_[:].rearrange("p h t -> p (h t)"), in_=p1[:], mul=128 ** -0.5),
                      r=["p1"], w=[kkey])
                P.pe(lambda e, mbb=mbb: proj(e, wvv, mbb[:, :, 2:130], p0, True), r=[mbkey, "w_mv"], w=["p0"])
                P.dve(lambda e, va_=va_, sl=sl: e.tensor_tensor(
                    out=va_[:, :, 0:128], in0=p0[:].rearrange("p (h e) -> p h e", h=4),
                    in1=bc(A_all[:, sl], [128, 4, 128], 2), op=ALU.mult), r=["p0", "A_all"], w=[vakey + (0,)])
                P.dve(lambda e, va_=va_, sl=sl: e.tensor_copy(out=va_[:, :, 128:129], in_=A_all[:, sl].unsqueeze(2)),
                      r=["A_all"], w=[vakey + (1,)])
                vakeys = [vakey + (0,), vakey + (1,)]

                def mm_s(e, kT_=kT_, qT_=qT_):
                    inst = None
                    for h in range(4):
                        inst = e.matmul(pS_[:, h * 128:(h + 1) * 128], lhsT=kT_[:, h, :], rhs=qT_[:, h, :],
                                        start=True, stop=True)
                    return inst

                P.pe(mm_s, r=[kTkey, qTkey], w=["pS"])
                sc_, sckey = scm.next()
                P.dve(lambda e, sc_=sc_, d=d: e.tensor_tensor(out=sc_[:], in0=pS_[:].rearrange("p (h t) -> p h t", h=4),
                                                              in1=bc(mask[d], [128, 4, 128], 1), op=ALU.mult),
                      r=["pS", "cf"], w=[sckey])

                def mm_kv(e, k_=k_, va_=va_):
                    inst = None
                    for h in range(4):
                        inst = e.matmul(pKV[:, h * 256:h * 256 + 129], lhsT=k_[:, h, :], rhs=va_[:, h, :],
                                        start=True, stop=True)
                    return inst

                P.pe(mm_kv, r=[kkey] + vakeys, w=["pKV"])
                kv3 = pKV[:].rearrange("p (h x) -> p h x", h=4)[:, :, 0:129]
                kvs_, kvskey = KVs.next()
                P.dve(lambda e, kvs_=kvs_: e.tensor_copy(out=kvs_[:], in_=kv3), r=["pKV"], w=[kvskey])
                yield
                cd_, cdkey = Cd.next()
                cdb_, cdbkey = Cdb.next()
                P.dve(lambda e, cd_=cd_, sl=sl: e.tensor_tensor(out=cd_[:], in0=Call[:],
                                                                in1=bc(DEC[:, sl], [128, 4, 129], 2), op=ALU.mult),
                      r=["Call", "DEC"], w=[cdkey])
                P.dve(lambda e, cd_=cd_, kvs_=kvs_: e.tensor_tensor(out=Call[:], in0=cd_[:], in1=kvs_[:], op=ALU.add),
                      r=[cdkey, kvskey], w=["Call"])
                P.act(lambda e, cd_=cd_, cdb_=cdb_: e.copy(out=cdb_[:], in_=cd_[:]), r=[cdkey], w=[cdbkey])
                yield

                def mm_n(e, sc_=sc_, va_=va_, qT_=qT_, cdb_=cdb_):
                    inst = None
                    for h in range(4):
                        e.matmul(pN[:, h * 256:h * 256 + 129], lhsT=sc_[:, h, :], rhs=va_[:, h, :], start=True,
                                 stop=False)
                        inst = e.matmul(pN[:, h * 256:h * 256 + 129], lhsT=qT_[:, h, :], rhs=cdb_[:, h, :],
                                        start=False, stop=True)
                    return inst

                P.pe(mm_n, r=[sckey, qTkey, cdbkey] + vakeys, w=["pN"])
                n3 = pN[:].rearrange("p (h x) -> p h x", h=4)
                yield
                s_, skey = sm.next()
                den = n3[:, :, 128]
                P.dve(lambda e, s_=s_: e.tensor_copy(out=s_[:, 3, :], in_=den), r=["pN"], w=[skey + (3,)])
                P.dve(lambda e, s_=s_: e.scalar_tensor_tensor(out=s_[:, 0, :], in0=s_[:, 3, :], scalar=-1.0,
                                                              in1=s_[:, 3, :], op0=ALU.mult, op1=ALU.max),
                      r=[skey + (3,)], w=[skey + (0,)])
                P.dve(lambda e, s_=s_, sl=sl: e.tensor_tensor(out=s_[:, 1, :], in0=s_[:, 0, :], in1=THR[:, sl],
                                                              op=ALU.max), r=[skey + (0,), "THR"], w=[skey + (1,)])
                P.dve(lambda e, s_=s_: e.reciprocal(out=s_[:, 2, :], in_=s_[:, 1, :]), r=[skey + (1,)],
                      w=[skey + (2,)])
                ho, hokey = hout.next()
                P.dve(lambda e, ho=ho, s_=s_: e.tensor_tensor(out=ho[:], in0=n3[:, :, 0:128],
                                                              in1=bc(s_[:, 2, :], [128, 4, 128], 2), op=ALU.mult),
                      r=["pN", skey + (2,)], w=[hokey])
                if d == 1:
                    P.dma("sp", scr["hb"][t0:t0 + 128, :], ho[:].rearrange("p h e -> p (h e)"), r=[hokey])
                    return
                P.pool(lambda e, ho=ho, hb_=hb_: e.tensor_tensor(out=hb_[:], in0=ho[:], in1=hb_[:], op=ALU.add),
                       r=[hokey, hbkey], w=[hbkey])
                P.pool(lambda e, hb_=hb_: e.tensor_tensor(out=hsq[:], in0=hb_[:], in1=hb_[:], op=ALU.mult),
                       r=[hbkey], w=["hsq"])
                hs_, hskey = hss.next()
                P.dve(lambda e, hs_=hs_: e.tensor_reduce(out=hs_[:, 0, :], in_=hsq[:], axis=AX.X, op=ALU.add),
                      r=["hsq"], w=[hskey + (0,)])
                rstd_pool(P, hs_[:, 1, :], hs_[:, 0, :], 128, epsb[:, 0:4], [hskey + (0,)], [hskey + (1,)])
                P.pool(lambda e, hb_=hb_, hs_=hs_: e.tensor_tensor(out=hb_[:], in0=hb_[:],
                                                                   in1=bc(hs_[:, 1, :], [128, 4, 128], 2),
                                                                   op=ALU.mult), r=[hbkey, hskey + (1,)], w=[hbkey])

                def trh(e, hb_=hb_):
                    inst = None
                    for h in range(4):
                        inst = e.transpose(pH[:, h * 128:(h + 1) * 128], hb_[:, h, :], identf)
                    return inst

                P.pe(trh, r=[hbkey, "cf"], w=["pH"])
                u_, ukey = ut.next()
                for h in range(4):
                    P.act(lambda e, u_=u_, h=h: e.activation(out=u_[:, h, :], in_=pH[:, h * 128:(h + 1) * 128],
                                                             func=AF.Identity, scale=mnw[:, h:h + 1]),
                          r=["pH", "mnw"], w=[ukey + (h,)])
                    P.dve(lambda e, u_=u_, h=h, xf=xf: e.scalar_tensor_tensor(
                        out=u_[:, h, :], in0=xf[:, h, :], scalar=msk[:, h:h + 1], in1=u_[:, h, :], op0=ALU.mult,
                        op1=ALU.add), r=[ukey + (h,), xfkey, "msk"], w=[ukey + (h,)])
                if c % 4 == 0:
                    bst["bms"], bst["key"] = bmst.next()
                bms, bmskey = bst["bms"], bst["key"]
                cj = c % 4
                P.dve(lambda e, u_=u_, so_=so_, bms=bms, cj=cj: e.tensor_tensor(
                    out=bms[:, :, cj * 128:(cj + 1) * 128], in0=u_[:], in1=so_[:], op=ALU.mult),
                    r=[ukey + (h,) for h in range(4)] + [sokey], w=[bmskey + (cj,)])
                if cj == 3:
                    tt0 = (c - 3) * 128
                    P.dma("sp", scr["bmT"][:, tt0:tt0 + T].rearrange("(k p) t -> p k t", p=128), bms[:],
                          r=[bmskey + (q,) for q in range(4)])

            order = list(order)
            n_ = len(order)
            gens = {c: chunk_body(c) for c in order}
            LA = 3
            for k in range(min(LA, n_)):
                next(gens[order[k]])
            next(gens[order[0]])
            for t in range(n_ + 1):
                if t < n_:
                    next(gens[order[t]])
                if t >= 1:
                    for _ in gens.pop(order[t - 1]):
                        pass
                if t < n_:
                    next(gens[order[t]])
                if t + 1 < n_:
                    next(gens[order[t + 1]])
                if t + LA < n_:
                    next(gens[order[t + LA]])
            P.flush()


def phase_merge(P, nc, x1s, w, scr, cins):
    name = "mg"
    with ExitStack() as st:
        def A(nm, shape, dt):
            return st.enter_context(nc.sbuf_tensor(f"{name}_{nm}", shape, dt))

        def PS(nm, shape, dt):
            return st.enter_context(nc.psum_tensor(f"{name}_{nm}", shape, dt))

        wa = A("wa", [128, 4, D], BF16)
        wb = A("wb", [128, 4, D], BF16)
        wo = A("wo", [128, 8, D], BF16)
        xt = Rot(A, "xt", 2, [128, 4, D], F32)
        at = Rot(A, "at", 2, [128, 4, T], BF16)
        bt = Rot(A, "bt", 2, [128, 4, T], BF16)
        sga = Rot(A, "sga", 2, [128, 8, T], F32)
        sgb = Rot(A, "sgb", 2, [128, 8, T], F32)
        mT = Rot(A, "mT", 2, [128, 8, T], BF16)
        ta = Rot(A, "ta", 2, [128, T], F32)
        tb = Rot(A, "tb", 2, [128, T], F32)
        pA_ = [PS(f"pA{b}", [128, T], F32) for b in range(2)]
        pB_ = [PS(f"pB{b}", [128, T], F32) for b in range(2)]
        pD = [PS(f"pD{b}", [128, T], F32) for b in range(2)]
        P.dma("pool", wa[:], w["w_branch_a"].rearrange("(k p) n -> p k n", p=128), w=["wa"])
        P.dma("pool", wb[:], w["w_branch_b"].rearrange("(k p) n -> p k n", p=128), w=["wb"])
        P.dma("pool", wo[:], w["w_out"].rearrange("(k p) n -> p k n", p=128), w=["wo"])

        def xview(ap, i):
            return ap[i * T:(i + 1) * T, :].rearrange("(j p) d -> p j d", p=128)

        bufs = {}

        def load(i):
            t0 = i * T
            x_, xk = xt.next()
            a_, ak = at.next()
            b_, bk = bt.next()
            ga_, gak = sga.next()
            gb_, gbk = sgb.next()
            P.dma("sp", a_[:], scr["aT"][:, t0:t0 + T].rearrange("(k p) t -> p k t", p=128), w=[ak])
            P.dma("sp", b_[:], scr["bmT"][:, t0:t0 + T].rearrange("(k p) t -> p k t", p=128), w=[bk])
            P.dma("sp", ga_[:], scr["sgT"][0:1024, t0:t0 + T].rearrange("(k p) t -> p k t", p=128), w=[gak])
            P.dma("sp", gb_[:], scr["sgT"][1024:2048, t0:t0 + T].rearrange("(k p) t -> p k t", p=128), w=[gbk])
            P.dma("sp", x_[:], xview(x1s, i), w=[xk + (j,) for j in range(4)])
            bufs[i] = (x_, xk, a_, ak, b_, bk, ga_, gak, gb_, gbk)

        load(0)
        na = 0
        nd = 0
        for i in range(NT):
            if i + 1 < NT:
                load(i + 1)
            x_, xk, a_, ak, b_, bk, ga_, gak, gb_, gbk = bufs.pop(i)
            m_, mk = mT.next()
            for cc in range(8):
                pa, pb = pA_[na % 2], pB_[na % 2]
                pak, pbk = ("pA", na % 2), ("pB", na % 2)
                na += 1

                def mm_a(e, pa=pa, cc=cc, a_=a_):
                    inst = None
                    for k in range(4):
                        inst = e.matmul(pa[:], lhsT=wa[:, k, cc * 128:(cc + 1) * 128], rhs=a_[:, k, :],
                                        start=(k == 0), stop=(k == 3))
                    return inst

                def mm_b(e, pb=pb, cc=cc, b_=b_):
                    inst = None
                    for k in range(4):
                        inst = e.matmul(pb[:], lhsT=wb[:, k, cc * 128:(cc + 1) * 128], rhs=b_[:, k, :],
                                        start=(k == 0), stop=(k == 3))
                    return inst

                P.pe(mm_a, r=[ak, "wa"], w=[pak])
                P.pe(mm_b, r=[bk, "wb"], w=[pbk])
                ta_, tak = ta.next()
                tb_, tbk = tb.next()
                P.dve(lambda e, ta_=ta_, pa=pa, ga_=ga_, cc=cc: e.tensor_tensor(out=ta_[:], in0=pa[:],
                                                                                in1=ga_[:, cc, :], op=ALU.mult),
                      r=[pak, gak], w=[tak])
                P.dve(lambda e, tb_=tb_, pb=pb, gb_=gb_, cc=cc: e.tensor_tensor(out=tb_[:], in0=pb[:],
                                                                                in1=gb_[:, cc, :], op=ALU.mult),
                      r=[pbk, gbk], w=[tbk])
                P.pool(lambda e, ta_=ta_, tb_=tb_, m_=m_, cc=cc: e.tensor_tensor(out=m_[:, cc, :], in0=ta_[:],
                                                                                 in1=tb_[:], op=ALU.add),
                       r=[tak, tbk], w=[mk + (cc,)])
            mkeys = [mk + (cc,) for cc in range(8)]
            for j in range(4):
                for half in range(2):
                    d_ = pD[nd % 2]
                    dk_ = ("pD", nd % 2)
                    nd += 1

                    def mm_o(e, d_=d_, j=j, half=half, m_=m_):
                        inst = None
                        for k in range(8):
                            inst = e.matmul(d_[:], lhsT=m_[:, k, j * 128:(j + 1) * 128],
                                            rhs=wo[:, k, half * 512:(half + 1) * 512], start=(k == 0), stop=(k == 7))
                        return inst

                    P.pe(mm_o, r=mkeys + ["wo"], w=[dk_])
                    xs = x_[:, j, half * 512:(half + 1) * 512]
                    P.dve(lambda e, d_=d_, xs=xs: e.tensor_tensor(out=xs, in0=d_[:], in1=xs, op=ALU.add),
                          r=[dk_, xk + (j,)], w=[xk + (j,)])
            P.dma("sp", xview(x1s, i), x_[:], r=[xk + (j,) for j in range(4)])
        P.flush()


W_NAMES = ["ffn1_norm_w", "ffn1_w_gate", "ffn1_w_up", "ffn1_w_down", "mix_norm_w", "w_in", "q_a_norm_w", "w_uq",
           "kv_a_norm_w", "w_uk", "w_uv", "q_norm_w", "k_norm_w", "w_branch_a", "conv_w", "conv_b", "w_mq", "w_mk",
           "w_mv", "b_igate", "b_fgate", "m_norm_w", "m_skip", "w_branch_b", "w_out", "ffn2_norm_w", "ffn2_w_gate",
           "ffn2_w_up", "ffn2_w_down", "final_norm_w"]
W_SHAPES = {
    "ffn1_norm_w": [D], "ffn1_w_gate": [D, FF], "ffn1_w_up": [D, FF], "ffn1_w_down": [FF, D], "mix_norm_w": [D],
    "w_in": [D, IN_DIM], "q_a_norm_w": [256], "w_uq": [256, 768], "kv_a_norm_w": [128], "w_uk": [128, 512],
    "w_uv": [128, 512], "q_norm_w": [96], "k_norm_w": [96], "w_branch_a": [512, D], "conv_w": [5, 512],
    "conv_b": [512], "w_mq": [4, 128, 128], "w_mk": [4, 128, 128], "w_mv": [4, 128, 128], "b_igate": [8],
    "b_fgate": [8], "m_norm_w": [512], "m_skip": [512], "w_branch_b": [512, D], "w_out": [D, D],
    "ffn2_norm_w": [D], "ffn2_w_gate": [D, FF], "ffn2_w_up": [D, FF], "ffn2_w_down": [FF, D], "final_norm_w": [D],
}


def build_program(phases=("ffn1", "inproj", "attn", "mlstm", "merge", "ffn2"), debug=()):
    nc = bass.Bass("TRN2", target_bir_lowering=False)
    x = nc.dram_tensor("x", [S, D], F32, kind="ExternalInput").ap()
    pos = nc.dram_tensor("positions", [S], I32, kind="ExternalInput").ap()
    w = {k: nc.dram_tensor(k, W_SHAPES[k], F32, kind="ExternalInput").ap() for k in W_NAMES}
    cins = {
        "c_ident": nc.dram_tensor("c_ident", [128, 128], BF16, kind="ExternalInput").ap(),
        "c_f32": nc.dram_tensor("c_f32", [128, 512], F32, kind="ExternalInput").ap(),
        "c_inv": nc.dram_tensor("c_inv", [16], F32, kind="ExternalInput").ap(),
    }
    out = nc.dram_tensor("out", [S, D], F32, kind="ExternalOutput").ap()

    def scratch(nm, shape, dt):
        kind = "ExternalOutput" if nm in debug else "Internal"
        return nc.dram_tensor("s_" + nm, shape, dt, kind=kind).ap()

    x1s = scratch("x1", [S, D], F32)
    scr = {
        "sgT": scratch("sgT", [2048, S], F32),
        "minT": scratch("minT", [512, S], F32),
        "sopT": scratch("sopT", [512, S], F32),
        "gates": scratch("gates", [S, 16], F32),
        "qkT": scratch("qkT", [16, 96, S], BF16),
        "vS": scratch("vS", [S, 512], BF16),
        "aT": scratch("aT", [512, S], BF16),
        "bmT": scratch("bmT", [512, S], BF16),
        "hb": scratch("hb", [S, 512], F32),
    }
    with ExitStack() as top:
        sems = [top.enter_context(nc.semaphore(f"sem{i}")) for i in range(96)]
        P = Prog(nc, sems)
        if "ffn1" in phases:
            phase_ffn(P, nc, "f1", x, x1s, w["ffn1_norm_w"], w["ffn1_w_gate"], w["ffn1_w_up"], w["ffn1_w_down"], cins)
        if "inproj" in phases:
            phase_inproj(P, nc, x1s, pos, w, scr, cins)
        if "attn" in phases:
            phase_attn(P, nc, scr, cins)
        if "mlstm" in phases:
            phase_mlstm(P, nc, w, scr, cins)
        if "merge" in phases:
            phase_merge(P, nc, x1s, w, scr, cins)
        if "ffn2" in phases:
            phase_ffn(P, nc, "f2", x1s, out, w["ffn2_norm_w"], w["ffn2_w_gate"], w["ffn2_w_up"], w["ffn2_w_down"],
                      cins, final_w=w["final_norm_w"])
    return nc


def make_consts():
    ident = np.eye(128, dtype=np.float32)
    s_idx = np.arange(128)[:, None]
    t_idx = np.arange(128)[None, :]
    cf = np.concatenate([ident, (s_idx <= t_idx).astype(np.float32), (s_idx >= t_idx).astype(np.float32),
                         np.ones((128, 128), np.float32)], axis=1)
    half = 16
    inv = (np.float32(10000.0) ** (-np.arange(half, dtype=np.float32) / np.float32(half))).astype(np.float32)
    return {"c_ident": ident.astype(ml_dtypes.bfloat16), "c_f32": np.ascontiguousarray(cf), "c_inv": inv}


def make_in_maps(inputs, n_cores=8):
    consts = make_consts()
    maps = []
    for b in range(n_cores):
        m = {"x": np.ascontiguousarray(inputs["x"][b]), "positions": np.ascontiguousarray(inputs["positions"][b])}
        for k in W_NAMES:
            m[k] = np.ascontiguousarray(np.asarray(inputs[k])[0])
        m.update(consts)
        maps.append(m)
    return maps


def kernel(**inputs):
    inputs = {k: np.asarray(v) for k, v in inputs.items()}
    nc = build_program()
    in_maps = make_in_maps(inputs, 8)
    res = run_bass_kernel_spmd(nc, in_maps, core_ids=list(range(8)))
    return np.stack([np.asarray(r["out"]) for r in res.results], axis=0).astype(np.float32)
```

```python
import math
from contextlib import ExitStack
from collections import defaultdict

import numpy as np
import ml_dtypes
import concourse.bass as bass
import concourse.mybir as mybir
from concourse.bass_utils import run_bass_kernel_spmd

F32 = mybir.dt.float32
BF16 = mybir.dt.bfloat16
I32 = mybir.dt.int32
AF = mybir.ActivationFunctionType
ALU = mybir.AluOpType
AX = mybir.AxisListType

S = 8192
D = 1024
FF = 2816
NHC = FF // 128
T = 512
NT = S // T
NCH = S // 128
IN_DIM = 3504
EPS = 1e-6
ATT_SCALE = 96 ** -0.5


class _Op:
    __slots__ = ("eng", "fn", "deps", "dma", "prev")

    def __init__(self, eng, fn, deps, dma):
        self.eng, self.fn, self.deps, self.dma, self.prev = eng, fn, deps, dma, None


class Prog:
    DMAQ = {"sp": 8, "pool": 4}

    def __init__(self, nc, sems):
        self.nc = nc
        self.sems = sems
        self.next_sem = 0
        self.dma_sems = {q: [self._alloc() for _ in range(k)] for q, k in self.DMAQ.items()}
        self.dma_cnt = {q: [0] * k for q, k in self.DMAQ.items()}
        self.dma_last = {q: [None] * k for q, k in self.DMAQ.items()}
        self.dma_n = {q: 0 for q in self.DMAQ}
        self._reset()

    def _alloc(self):
        i = self.next_sem
        self.next_sem += 1
        assert i < len(self.sems), "out of semaphores"
        return i

    def _reset(self):
        self.ops = []
        self.lastw = {}
        self.readers = defaultdict(list)
        self.fence_deps = set()
        self.fenced = set()

    def fence(self):
        self.fence_deps = set(range(len(self.ops)))
        self.fenced = set()

    def op(self, eng, fn, r=(), w=(), dma=False):
        idx = len(self.ops)
        deps = set()
        for k in r:
            if k in self.lastw:
                deps.add(self.lastw[k])
        for k in w:
            if k in self.lastw:
                deps.add(self.lastw[k])
            deps.update(self.readers.get(k, ()))
        for k in w:
            self.lastw[k] = idx
            self.readers[k] = []
        for k in r:
            self.readers[k].append(idx)
        if self.fence_deps and eng not in self.fenced:
            deps |= self.fence_deps
            self.fenced.add(eng)
        deps.discard(idx)
        self.ops.append(_Op(eng, fn, deps, dma))
        return idx

    def pe(self, fn, r=(), w=()):
        return self.op("pe", fn, r, w)

    def act(self, fn, r=(), w=()):
        return self.op("act", fn, r, w)

    def dve(self, fn, r=(), w=()):
        return self.op("dve", fn, r, w)

    def pool(self, fn, r=(), w=()):
        return self.op("pool", fn, r, w)

    def dma(self, q, out, in_, r=(), w=(), **kw):
        return self.op(q, lambda e, out=out, in_=in_, kw=kw: e.dma_start(out=out, in_=in_, **kw), r, w, dma=True)

    def flush(self):
        nc = self.nc
        ops = self.ops
        n = len(ops)
        signal = [False] * n

        def skip(p, c):
            return p.eng == "pe" and c.eng == "pe" and not p.dma and not c.dma

        for o in ops:
            for d in o.deps:
                if not skip(ops[d], o):
                    signal[d] = True
        csem = {}
        ccnt = {}
        ticket = [None] * n
        per_eng = {e: [] for e in ("pe", "act", "dve", "pool", "sp")}
        for i, o in enumerate(ops):
            per_eng[o.eng].append(i)
            if o.dma:
                q = o.eng
                K = len(self.dma_sems[q])
                j = self.dma_n[q] % K
                self.dma_n[q] += 1
                o.prev = self.dma_last[q][j]
                self.dma_cnt[q][j] += 1
                ticket[i] = (self.dma_sems[q][j], 16 * self.dma_cnt[q][j])
                self.dma_last[q][j] = ticket[i]
            elif signal[i]:
                e = o.eng
                if e not in csem or ccnt[e] >= 30000:
                    csem[e] = self._alloc()
                    ccnt[e] = 0
                ccnt[e] += 1
                ticket[i] = (csem[e], ccnt[e])
        sems = self.sems

        def emit(engname, eng):
            waited = {}
            for i in per_eng[engname]:
                o = ops[i]
                need = {}
                for d in o.deps:
                    if skip(ops[d], o):
                        continue
                    s, v = ticket[d]
                    if need.get(s, 0) < v:
                        need[s] = v
                if o.prev is not None:
                    s, v = o.prev
                    if need.get(s, 0) < v:
                        need[s] = v
                for s, v in need.items():
                    if waited.get(s, 0) < v:
                        eng.wait_ge(sems[s], v)
                        waited[s] = v
                inst = o.fn(eng)
                if ticket[i] is not None:
                    inst.then_inc(sems[ticket[i][0]], 16 if o.dma else 1)
            if engname in self.dma_last:
                for t in self.dma_last[engname]:
                    if t is not None and waited.get(t[0], 0) < t[1]:
                        eng.wait_ge(sems[t[0]], t[1])

        with nc.Block() as blk:
            if per_eng["pe"]:
                @blk.tensor
                def _(e):
                    emit("pe", e)
            if per_eng["act"]:
                @blk.scalar
                def _(e):
                    emit("act", e)
            if per_eng["dve"]:
                @blk.vector
                def _(e):
                    emit("dve", e)
            if per_eng["pool"]:
                @blk.gpsimd
                def _(e):
                    emit("pool", e)
            if per_eng["sp"]:
                @blk.sync
                def _(e):
                    emit("sp", e)
        self._reset()


class Rot:
    def __init__(self, alloc, name, n, shape, dt):
        self.t = [alloc(f"{name}{i}", shape, dt) for i in range(n)]
        self.name = name
        self.i = -1

    def next(self):
        self.i += 1
        j = self.i % len(self.t)
        return self.t[j], (self.name, j)


USE_POOL_POW = False


def rstd_pool(P, out_ap, in_ap, n, mh_ap, r, w):
    if not USE_POOL_POW:
        P.act(lambda e: e.activation(out=out_ap, in_=in_ap, func=AF.Sqrt, scale=1.0 / n, bias=EPS), r=r, w=w)
        P.dve(lambda e: e.reciprocal(out=out_ap, in_=out_ap), r=w, w=w)
        return
    P.pool(lambda e: e.tensor_scalar(out=out_ap, in0=in_ap, scalar1=1.0 / n, scalar2=EPS, op0=ALU.mult, op1=ALU.add),
           r=r, w=w)
    P.pool(lambda e: e.tensor_tensor(out=out_ap, in0=out_ap, in1=mh_ap, op=ALU.pow), r=w, w=w)


def bc(ap2d, shape, axis):
    return ap2d.unsqueeze(axis).to_broadcast(list(shape))


def emit_norm_T(P, x_ap, x_key, nw, hn, hn_key, ss, rs, col, epsb, ident, pT, pT_key, hT_view, hT_key,
                evac="act", n=D):
    nk = n // 128
    P.act(lambda e: e.activation(out=hn[:, 0:n], in_=x_ap, func=AF.Square, accum_out=ss[:, col:col + 1]),
          r=[x_key], w=[hn_key, ("ss", col)])
    P.act(lambda e: e.activation(out=rs[:, col:col + 1], in_=ss[:, col:col + 1], func=AF.Sqrt,
                                 scale=1.0 / n, bias=epsb[:, 0:1]),
          r=[("ss", col)], w=[("rs", col)])
    P.dve(lambda e: e.reciprocal(out=rs[:, col:col + 1], in_=rs[:, col:col + 1]),
          r=[("rs", col)], w=[("rs", col)])
    P.dve(lambda e: e.scalar_tensor_tensor(out=hn[:, 0:n], in0=x_ap, scalar=rs[:, col:col + 1], in1=nw,
                                           op0=ALU.mult, op1=ALU.mult),
          r=[x_key, ("rs", col)], w=[hn_key])

    def tr(e):
        inst = None
        for k in range(nk):
            inst = e.transpose(pT[:, k * 128:(k + 1) * 128], hn[:, k * 128:(k + 1) * 128], ident[:])
        return inst

    P.pe(tr, r=[hn_key], w=[pT_key])
    src = pT[:, 0:n].rearrange("p (k t) -> p k t", k=nk)
    if evac == "act":
        P.act(lambda e: e.copy(out=hT_view, in_=src), r=[pT_key], w=[hT_key])
    else:
        P.dve(lambda e: e.tensor_copy(out=hT_view, in_=src), r=[pT_key], w=[hT_key])


def load_consts(P, A, cins, need_cf=False):
    ident = A("ident", [128, 128], BF16)
    cf = A("cf32", [128, 512], F32) if need_cf else None
    epsb = A("mhalf", [128, 16], F32)
    P.dma("sp", ident[:], cins["c_ident"], w=["ident"])
    if need_cf:
        P.dma("sp", cf[:], cins["c_f32"], w=["cf"])
    P.dve(lambda e: e.memset(epsb[:], -0.5), w=["epsb"])
    return ident, cf, epsb


def phase_ffn(P, nc, name, src, dst, norm_w, wg, wu, wd, cins, final_w=None):
    with ExitStack() as st:
        def A(nm, shape, dt):
            return st.enter_context(nc.sbuf_tensor(f"{name}_{nm}", shape, dt))

        def PS(nm, shape, dt):
            return st.enter_context(nc.psum_tensor(f"{name}_{nm}", shape, dt))

        wgt = A("wg", [128, 8, FF], BF16)
        wut = A("wu", [128, 8, FF], BF16)
        wdt = A("wd", [128, NHC, D], BF16)
        xt = [A(f"xt{b}", [128, 4, D], F32) for b in range(2)]
        hT = A("hT", [128, 8, T], BF16)
        actb = A("act", [128, NHC, T], BF16)
        hn = A("hn", [128, D], BF16)
        sg = [A(f"sg{b}", [128, T], BF16) for b in range(2)]
        nw = A("nw", [128, D], F32)
        fw = A("fw", [128, D], F32) if final_w is not None else None
        ncol = NT * 4 * (2 if final_w is not None else 1)
        ss = A("ss", [128, ncol], F32)
        rs = A("rs", [128, ncol], F32)
        ident, cf, epsb = load_consts(P, A, cins)
        P.dve(lambda e: e.memset(epsb[:], EPS), r=["epsb"], w=["epsb"])
        pT = PS("pT", [128, D], BF16)
        pG = [PS(f"pG{b}", [128, T], F32) for b in range(2)]
        pU = [PS(f"pU{b}", [128, T], F32) for b in range(2)]
        pD = [PS(f"pD{b}", [128, T], F32) for b in range(2)]

        P.dve(lambda e: e.memset(ss[:], 0.0), w=[("ss", c) for c in range(ncol)])
        P.dma("sp", nw[:], norm_w.partition_broadcast(128), w=["nw"])
        if fw is not None:
            P.dma("sp", fw[:], final_w.partition_broadcast(128), w=["fw"])
        P.fence()
        for (wt, wsrc, key) in ((wgt, wg, "wg"), (wut, wu, "wu")):
            v = wsrc.rearrange("(k p) n -> p k n", p=128)
            for q in range(4):
                c0, c1 = q * 704, (q + 1) * 704
                P.dma("pool", wt[:, :, c0:c1], v[:, :, c0:c1], w=[(key, q)])
        vd = wd.rearrange("(k p) n -> p k n", p=128)
        for q in range(2):
            P.dma("pool", wdt[:, q * 11:(q + 1) * 11, :], vd[:, q * 11:(q + 1) * 11, :], w=[("wd", q)])

        def xview(ap, i):
            return ap[i * T:(i + 1) * T, :].rearrange("(j p) d -> p j d", p=128)

        def load(i):
            b = i % 2
            P.dma("sp", xt[b][:], xview(src, i), w=[("xt", b, j) for j in range(4)])

        def norm_sub(i, j):
            b = i % 2
            col = i * 4 + j
            emit_norm_T(P, xt[b][:, j, :], ("xt", b, j), nw[:], hn, "hn", ss, rs, col, epsb, ident, pT, "pT",
                        hT[:, :, j * 128:(j + 1) * 128], ("hT", j))

        load(0)
        for j in range(4):
            norm_sub(0, j)
        for i in range(NT):
            b = i % 2
            if i + 1 < NT:
                load(i + 1)
            for hc in range(NHC):
                q = hc * 128 // 704
                q2 = (hc * 128 + 127) // 704
                g, u = pG[hc % 2], pU[hc % 2]

                def mm_g(e, hc=hc, g=g):
                    inst = None
                    for k in range(8):
                        inst = e.matmul(g[:], lhsT=wgt[:, k, hc * 128:(hc + 1) * 128], rhs=hT[:, k, :],
                                        start=(k == 0), stop=(k == 7))
                    return inst

                def mm_u(e, hc=hc, u=u):
                    inst = None
                    for k in range(8):
                        inst = e.matmul(u[:], lhsT=wut[:, k, hc * 128:(hc + 1) * 128], rhs=hT[:, k, :],
                                        start=(k == 0), stop=(k == 7))
                    return inst

                hkeys = [("hT", j) for j in range(4)]
                P.pe(mm_g, r=hkeys + [("wg", q), ("wg", q2)], w=[("pG", hc % 2)])
                P.pe(mm_u, r=hkeys + [("wu", q), ("wu", q2)], w=[("pU", hc % 2)])
                sgb = sg[hc % 2]
                P.act(lambda e, g=g, sgb=sgb: e.activation(out=sgb[:], in_=g[:], func=AF.Silu),
                      r=[("pG", hc % 2)], w=[("sg", hc % 2)])
                P.dve(lambda e, u=u, sgb=sgb, hc=hc: e.tensor_tensor(out=actb[:, hc, :], in0=u[:], in1=sgb[:],
                                                                     op=ALU.mult),
                      r=[("pU", hc % 2), ("sg", hc % 2)], w=[("act", hc)])
            nd = 0
            for j in range(4):
                for half in range(2):
                    d_ = pD[nd % 2]
                    nd += 1

                    def mm_d(e, j=j, half=half, d_=d_):
                        inst = None
                        for hc in range(NHC):
                            inst = e.matmul(d_[:], lhsT=actb[:, hc, j * 128:(j + 1) * 128],
                                            rhs=wdt[:, hc, half * 512:(half + 1) * 512],
                                            start=(hc == 0), stop=(hc == NHC - 1))
                        return inst

                    P.pe(mm_d, r=[("act", hc) for hc in range(NHC)] + [("wd", 0), ("wd", 1)],
                         w=[("pD", (nd - 1) % 2)])
                    xs = xt[b][:, j, half * 512:(half + 1) * 512]
                    P.dve(lambda e, d_=d_, xs=xs: e.scalar_tensor_tensor(out=xs, in0=d_[:], scalar=0.5, in1=xs,
                                                                         op0=ALU.mult, op1=ALU.add),
                          r=[("pD", (nd - 1) % 2), ("xt", b, j)], w=[("xt", b, j)])
                if final_w is not None:
                    col = NT * 4 + i * 4 + j
                    xj = xt[b][:, j, :]
                    P.act(lambda e, xj=xj, col=col: e.activation(out=hn[:], in_=xj, func=AF.Square,
                                                                 accum_out=ss[:, col:col + 1]),
                          r=[("xt", b, j)], w=["hn", ("ss", col)])
                    P.act(lambda e, col=col: e.activation(out=rs[:, col:col + 1], in_=ss[:, col:col + 1],
                                                          func=AF.Sqrt, scale=1.0 / D, bias=epsb[:, 0:1]),
                          r=[("ss", col)], w=[("rs", col)])
                    P.dve(lambda e, col=col: e.reciprocal(out=rs[:, col:col + 1], in_=rs[:, col:col + 1]),
                          r=[("rs", col)], w=[("rs", col)])
                    P.dve(lambda e, xj=xj, col=col: e.scalar_tensor_tensor(out=xj, in0=xj, scalar=rs[:, col:col + 1],
                                                                           in1=fw[:], op0=ALU.mult, op1=ALU.mult),
                          r=[("xt", b, j), ("rs", col), "fw"], w=[("xt", b, j)])
                if i + 1 < NT and j >= 1:
                    for jj in ((0, 1) if j == 1 else (2,) if j == 2 else (3,)):
                        norm_sub(i + 1, jj)
            P.dma("sp", xview(dst, i), xt[b][:], r=[("xt", b, j) for j in range(4)])
        P.flush()


def phase_inproj(P, nc, x1s, pos, w, scr, cins):
    name = "ip"
    with ExitStack() as st:
        def A(nm, shape, dt):
            return st.enter_context(nc.sbuf_tensor(f"{name}_{nm}", shape, dt))

        def PS(nm, shape, dt):
            return st.enter_context(nc.psum_tensor(f"{name}_{nm}", shape, dt))

        win = A("win", [128, 8, IN_DIM], BF16)
        wuq = A("wuq", [128, 2, 768], BF16)
        wuk = A("wuk", [128, 512], BF16)
        wuv = A("wuv", [128, 512], BF16)
        xt = [A(f"xt{b}", [128, 4, D], F32) for b in range(2)]
        hT = A("hT", [128, 8, T], BF16)
        hn = A("hn", [128, D], BF16)
        nw = A("nw", [128, D], F32)
        qaw = A("qaw", [128, 384], F32)
        qkw = A("qkw", [128, 2, 96], F32)
        ss = A("ss", [128, NT * 4], F32)
        rs = A("rs", [128, NT * 4], F32)
        ss2 = A("ss2", [128, NT * 4, 2], F32)
        rs2 = A("rs2", [128, NT * 4, 2], F32)
        ss16 = A("ss16", [128, 4, 16], F32)
        rs16 = A("rs16", [128, 4, 16], F32)
        stage = Rot(A, "stg", 2, [128, 4, T], F32)
        cn = Rot(A, "cn", 2, [128, 384], BF16)
        cnT = Rot(A, "cnT", 2, [128, 3, 128], BF16)
        kpe = Rot(A, "kpe", 2, [128, 32], F32)
        gst = A("gst", [128, 4, 16], F32)
        vst = Rot(A, "vst", 2, [128, 512], BF16)
        qkf = Rot(A, "qkf", 2, [128, 16, 96], F32)
        sq = A("sq", [128, 16, 96], F32)
        rtmp = Rot(A, "rtmp", 1, [128, 4, 16, 16], F32)
        qkb = Rot(A, "qkb", 2, [128, 16, 96], BF16)
        qkTs = Rot(A, "qkTs", 1, [128, 16, T], BF16)
        posi = A("posi", [128, NCH], I32)
        posf = A("posf", [128, NCH], F32)
        inv = A("inv", [128, 16], F32)
        ang = qkf.t[0][:].rearrange("p h d -> p (h d)")[:, 0:NCH * 16].rearrange("p (c f) -> p c f", f=16)
        kf = qkf.t[1][:].rearrange("p h d -> p (h d)")[:, 0:NCH * 16].rearrange("p (c f) -> p c f", f=16)
        ki_t = A("ki", [128, NCH, 16], I32)
        ki = ki_t[:]
        cosT = A("cos", [128, NCH, 16], F32)
        sinT = A("sin", [128, NCH, 16], F32)
        hpi = A("hpi", [128, 1], F32)
        ident, cf, _mh = load_consts(P, A, cins)
        epsb = A("epsv", [128, 1], F32)
        P.dve(lambda e: e.memset(epsb[:], EPS), w=["epsv"])
        pT = PS("pT", [128, D], BF16)
        pF = [PS(f"pF{b}", [128, T], F32) for b in range(2)]
        pA = PS("pA", [128, T], F32)
        pQ = PS("pQ", [128, 1024], F32)
        pK, pV = pF[0], pF[1]
        pX = PS("pX", [128, 8 * 128], BF16)

        P.dve(lambda e: e.memset(ss[:], 0.0), w=[("ss", c) for c in range(NT * 4)])
        P.dve(lambda e: e.memset(ss2[:], 0.0), w=[("ss2", c) for c in range(NT * 4)])
        P.dve(lambda e: e.memset(hpi[:], math.pi / 2), w=["hpi"])
        P.dma("sp", nw[:], w["mix_norm_w"].partition_broadcast(128), w=["nw"])
        P.dma("sp", qaw[:, 0:256], w["q_a_norm_w"].partition_broadcast(128), w=["qaw"])
        P.dma("sp", qaw[:, 256:384], w["kv_a_norm_w"].partition_broadcast(128), w=["qaw2"])
        P.dma("sp", qkw[:, 0, :], w["q_norm_w"].partition_broadcast(128), w=["qkw0"])
        P.dma("sp", qkw[:, 1, :], w["k_norm_w"].partition_broadcast(128), w=["qkw1"])
        P.dma("sp", inv[:], cins["c_inv"].partition_broadcast(128), w=["inv"])
        P.dma("sp", posi[:], pos.rearrange("(c p) -> p c", p=128), w=["posi"], allow_slow_non_contiguous=True)
        wv = w["w_in"].rearrange("(k p) n -> p k n", p=128)
        P.dma("pool", win[:, :, 0:416], wv[:, :, 0:416], w=["winA"])
        P.dma("pool", win[:, :, 416:432], wv[:, :, 1440:1456], w=["winA2"])
        P.dma("pool", win[:, :, 432:1456], wv[:, :, 416:1440], w=[("winF", 0)])
        P.dma("pool", win[:, :, 1456:2480], wv[:, :, 1456:2480], w=[("winF", 1)])
        P.dma("pool", win[:, :, 2480:3504], wv[:, :, 2480:3504], w=[("winF", 2)])
        P.dma("pool", wuq[:], w["w_uq"].rearrange("(k p) n -> p k n", p=128), w=["wuq"])
        P.dma("pool", wuk[:], w["w_uk"], w=["wuk"])
        P.dma("pool", wuv[:], w["w_uv"], w=["wuv"])

        P.dve(lambda e: e.tensor_copy(out=posf[:], in_=posi[:]), r=["posi"], w=["posf"])
        P.dve(lambda e: e.tensor_tensor(out=ang, in0=bc(posf[:], [128, NCH, 16], 2),
                                        in1=bc(inv[:], [128, NCH, 16], 1), op=ALU.mult),
              r=["posf", "inv"], w=["ang"])
        P.dve(lambda e: e.tensor_scalar(out=kf, in0=ang, scalar1=1.0 / (2 * math.pi), scalar2=None,
                                        op0=ALU.mult), r=["ang"], w=["kf"])
        P.dve(lambda e: e.tensor_copy(out=ki, in_=kf), r=["kf"], w=["ki"])
        P.dve(lambda e: e.tensor_copy(out=kf, in_=ki), r=["ki"], w=["kf"])
        C1 = 6.28125
        C2 = 2 * math.pi - C1
        P.dve(lambda e: e.scalar_tensor_tensor(out=ang, in0=kf, scalar=-C1, in1=ang, op0=ALU.mult,
                                               op1=ALU.add), r=["kf", "ang"], w=["ang"])
        P.dve(lambda e: e.scalar_tensor_tensor(out=ang, in0=kf, scalar=-C2, in1=ang, op0=ALU.mult,
                                               op1=ALU.add), r=["kf", "ang"], w=["ang"])
        P.dve(lambda e: e.tensor_scalar(out=ang, in0=ang, scalar1=-3.1415925, scalar2=3.1415925,
                                        op0=ALU.max, op1=ALU.min), r=["ang"], w=["ang"])
        P.act(lambda e: e.activation(out=sinT[:], in_=ang, func=AF.Sin), r=["ang"], w=["sin"])
        P.dve(lambda e: e.scalar_tensor_tensor(out=kf, in0=ang, scalar=-1.0, in1=ang, op0=ALU.mult,
                                               op1=ALU.max), r=["ang"], w=["kf"])
        P.act(lambda e: e.activation(out=cosT[:], in_=kf, func=AF.Sin, scale=-1.0, bias=hpi[:]),
              r=["kf", "hpi"], w=["cos"])

        P.fence()

        def xview(ap, i):
            return ap[i * T:(i + 1) * T, :].rearrange("(j p) d -> p j d", p=128)

        def load(i):
            b = i % 2
            P.dma("sp", xt[b][:], xview(x1s, i), w=[("xt", b, j) for j in range(4)])

        def norm_sub(i, j):
            b = i % 2
            col = i * 4 + j
            emit_norm_T(P, xt[b][:, j, :], ("xt", b, j), nw[:], hn, "hn", ss, rs, col, epsb, ident, pT, "pT",
                        hT[:, :, j * 128:(j + 1) * 128], ("hT", j), evac="dve")

        fm_groups = [
            (scr["minT"], 432, None), (scr["sopT"], 944, AF.Sigmoid),
            (scr["sgT"][0:512, :], 1456, AF.Sigmoid), (scr["sgT"][512:1024, :], 1968, AF.Sigmoid),
            (scr["sgT"][1024:1536, :], 2480, AF.Sigmoid), (scr["sgT"][1536:2048, :], 2992, AF.Sigmoid),
        ]
        hkeys = [("hT", j) for j in range(4)]
        wkeys = ["winA", "winA2"] + [("winF", q) for q in range(3)]
        nf = 0
        load(0)
        for j in range(4):
            norm_sub(0, j)
        for i in range(NT):
            b = i % 2
            t0 = i * T
            if i + 1 < NT:
                load(i + 1)
            for (dram, col0, func) in fm_groups:
                stg, skey = stage.next()
                for cc in range(4):
                    pf = pF[nf % 2]
                    pkey = ("pF", nf % 2)
                    nf += 1

                    def mm_f(e, pf=pf, c0=col0 + cc * 128):
                        inst = None
                        for k in range(8):
                            inst = e.matmul(pf[:], lhsT=win[:, k, c0:c0 + 128], rhs=hT[:, k, :],
                                            start=(k == 0), stop=(k == 7))
                        return inst

                    P.pe(mm_f, r=hkeys + wkeys, w=[pkey])
                    if func is None:
                        P.dve(lambda e, pf=pf, stg=stg, cc=cc: e.tensor_copy(out=stg[:, cc, :], in_=pf[:]),
                              r=[pkey], w=[skey + (cc,)])
                    else:
                        P.act(lambda e, pf=pf, stg=stg, cc=cc, func=func: e.activation(out=stg[:, cc, :], in_=pf[:],
                                                                                      func=func),
                              r=[pkey], w=[skey + (cc,)])
                P.dma("sp", dram[:, t0:t0 + T].rearrange("(c p) t -> p c t", p=128), stg[:],
                      r=[skey + (cc,) for cc in range(4)])
            qs, qskey = qkTs.next()
            def sub_body(j, i=i, t0=t0, qs=qs, qskey=qskey):
                c = i * 4 + j
                tok0 = t0 + j * 128

                def mm_a(e, j=j):
                    inst = None
                    for k in range(8):
                        inst = e.matmul(pA[:, 0:432], lhsT=hT[:, k, j * 128:(j + 1) * 128], rhs=win[:, k, 0:432],
                                        start=(k == 0), stop=(k == 7))
                    return inst

                P.pe(mm_a, r=hkeys + wkeys, w=["pA"])
                cnb, cnkey = cn.next()
                kp, kpkey = kpe.next()
                P.act(lambda e, c=c: e.activation(out=hn[:, 0:256], in_=pA[:, 0:256], func=AF.Square,
                                                  accum_out=ss2[:, c, 0:1]), r=["pA"], w=["hn", ("ss2", c)])
                P.act(lambda e, c=c: e.activation(out=hn[:, 256:384], in_=pA[:, 256:384], func=AF.Square,
                                                  accum_out=ss2[:, c, 1:2]), r=["pA"], w=["hn", ("ss2", c)])
                P.act(lambda e, c=c: e.activation(out=rs2[:, c, 0:1], in_=ss2[:, c, 0:1], func=AF.Sqrt,
                                                  scale=1.0 / 256, bias=epsb[:]), r=[("ss2", c)], w=[("rs2", c)])
                P.act(lambda e, c=c: e.activation(out=rs2[:, c, 1:2], in_=ss2[:, c, 1:2], func=AF.Sqrt,
                                                  scale=1.0 / 128, bias=epsb[:]), r=[("rs2", c)], w=[("rs2", c)])
                P.dve(lambda e, c=c: e.reciprocal(out=rs2[:, c, :], in_=rs2[:, c, :]), r=[("rs2", c)],
                      w=[("rs2", c)])
                P.dve(lambda e, c=c, cnb=cnb: e.scalar_tensor_tensor(out=cnb[:, 0:256], in0=pA[:, 0:256],
                                                                     scalar=rs2[:, c, 0:1], in1=qaw[:, 0:256],
                                                                     op0=ALU.mult, op1=ALU.mult),
                      r=["pA", ("rs2", c), "qaw"], w=[cnkey + (0,)])
                P.dve(lambda e, c=c, cnb=cnb: e.scalar_tensor_tensor(out=cnb[:, 256:384], in0=pA[:, 256:384],
                                                                     scalar=rs2[:, c, 1:2], in1=qaw[:, 256:384],
                                                                     op0=ALU.mult, op1=ALU.mult),
                      r=["pA", ("rs2", c), "qaw2"], w=[cnkey + (1,)])
                P.dve(lambda e, kp=kp: e.tensor_copy(out=kp[:], in_=pA[:, 384:416]), r=["pA"], w=[kpkey])
                P.dve(lambda e, j=j: e.tensor_copy(out=gst[:, j, :], in_=pA[:, 416:432]), r=["pA"], w=[("gst", j)])
                yield
                cT, cTkey = cnT.next()

                def tr3(e, cnb=cnb):
                    inst = None
                    for k in range(3):
                        inst = e.transpose(pT[:, k * 128:(k + 1) * 128], cnb[:, k * 128:(k + 1) * 128], ident[:])
                    return inst

                P.pe(tr3, r=[cnkey + (0,), cnkey + (1,)], w=["pT"])
                P.dve(lambda e, cT=cT: e.tensor_copy(out=cT[:], in_=pT[:, 0:384].rearrange("p (k t) -> p k t", k=3)),
                      r=["pT"], w=[cTkey])

                yield
                def mm_q(e, cT=cT):
                    e.matmul(pQ[:, 0:512], lhsT=cT[:, 0, :], rhs=wuq[:, 0, 0:512], start=True, stop=False)
                    e.matmul(pQ[:, 0:512], lhsT=cT[:, 1, :], rhs=wuq[:, 1, 0:512], start=False, stop=True)
                    e.matmul(pQ[:, 512:768], lhsT=cT[:, 0, :], rhs=wuq[:, 0, 512:768], start=True, stop=False)
                    return e.matmul(pQ[:, 512:768], lhsT=cT[:, 1, :], rhs=wuq[:, 1, 512:768], start=False, stop=True)

                P.pe(mm_q, r=[cTkey, "wuq"], w=["pQ"])
                P.pe(lambda e, cT=cT: e.matmul(pK[:], lhsT=cT[:, 2, :], rhs=wuk[:], start=True, stop=True),
                     r=[cTkey, "wuk"], w=[("pF", 0)])
                P.pe(lambda e, cT=cT: e.matmul(pV[:], lhsT=cT[:, 2, :], rhs=wuv[:], start=True, stop=True),
                     r=[cTkey, "wuv"], w=[("pF", 1)])
                vs, vskey = vst.next()
                P.act(lambda e, vs=vs: e.copy(out=vs[:], in_=pV[:]), r=[("pF", 1)], w=[vskey])
                P.dma("sp", scr["vS"][tok0:tok0 + 128, :], vs[:], r=[vskey])
                qf, qfkey = qkf.next()
                P.act(lambda e, qf=qf: e.copy(out=qf[:, 0:8, :].rearrange("p h d -> p (h d)"), in_=pQ[:, 0:768]),
                      r=["pQ"], w=[qfkey + ("q",)])
                P.dve(lambda e, qf=qf: e.tensor_copy(out=qf[:, 8:16, 0:64],
                                                     in_=pK[:].rearrange("p (h d) -> p h d", h=8)),
                      r=[("pF", 0)], w=[qfkey + ("k",)])
                P.pool(lambda e, qf=qf, kp=kp: e.tensor_copy(out=qf[:, 8:16, 64:96], in_=bc(kp[:], [128, 8, 32], 1)),
                       r=[kpkey], w=[qfkey + ("kp",)])
                qkeys = [qfkey + (s_,) for s_ in ("q", "k", "kp")]
                yield
                P.pool(lambda e, qf=qf: e.tensor_tensor(out=sq[:], in0=qf[:], in1=qf[:], op=ALU.mult),
                       r=qkeys, w=["sq"])
                P.dve(lambda e, j=j: e.tensor_reduce(out=ss16[:, j, :], in_=sq[:], axis=AX.X, op=ALU.add),
                      r=["sq"], w=[("ss16", j)])
                yield
                P.act(lambda e, j=j: e.activation(out=rs16[:, j, :], in_=ss16[:, j, :], func=AF.Sqrt,
                                                  scale=1.0 / 96, bias=epsb[:]), r=[("ss16", j)], w=[("rs16", j)])
                P.dve(lambda e, j=j: e.reciprocal(out=rs16[:, j, :], in_=rs16[:, j, :]), r=[("rs16", j)],
                      w=[("rs16", j)])
                P.dve(lambda e, qf=qf, j=j: e.tensor_tensor(out=qf[:], in0=qf[:],
                                                            in1=bc(rs16[:, j, :], [128, 16, 96], 2), op=ALU.mult),
                      r=qkeys + [("rs16", j)], w=qkeys)
                yield
                qb, qbkey = qkb.next()
                for hh in range(2):
                    eng = P.pool if hh == 0 else P.dve
                    eng(lambda e, qf=qf, qb=qb, hh=hh: e.tensor_tensor(
                        out=qb[:, hh * 8:(hh + 1) * 8, 0:64], in0=qf[:, hh * 8:(hh + 1) * 8, 0:64],
                        in1=bc(qkw[:, hh, 0:64], [128, 8, 64], 1), op=ALU.mult),
                        r=qkeys + ["qkw0", "qkw1"], w=[qbkey + ("n", hh)])
                    eng(lambda e, qf=qf, hh=hh: e.tensor_tensor(
                        out=qf[:, hh * 8:(hh + 1) * 8, 64:96], in0=qf[:, hh * 8:(hh + 1) * 8, 64:96],
                        in1=bc(qkw[:, hh, 64:96], [128, 8, 32], 1), op=ALU.mult),
                        r=qkeys + ["qkw0", "qkw1"], w=qkeys)
                yield
                rt, rtkey = rtmp.next()
                cb_ = bc(cosT[:, c, :], [128, 16, 16], 1)
                sb_ = bc(sinT[:, c, :], [128, 16, 16], 1)
                x1_ = lambda qf: qf[:, :, 64:80]
                x2_ = lambda qf: qf[:, :, 80:96]
                P.pool(lambda e, qf=qf, rt=rt, cb_=cb_: e.tensor_tensor(out=rt[:, 0], in0=x1_(qf), in1=cb_, op=ALU.mult),
                       r=qkeys + ["cos"], w=[rtkey + (0,)])
                P.pool(lambda e, qf=qf, rt=rt, sb_=sb_: e.tensor_tensor(out=rt[:, 1], in0=x2_(qf), in1=sb_, op=ALU.mult),
                       r=qkeys + ["sin"], w=[rtkey + (1,)])
                P.dve(lambda e, qf=qf, rt=rt, cb_=cb_: e.tensor_tensor(out=rt[:, 2], in0=x2_(qf), in1=cb_, op=ALU.mult),
                      r=qkeys + ["cos"], w=[rtkey + (2,)])
                P.dve(lambda e, qf=qf, rt=rt, sb_=sb_: e.tensor_tensor(out=rt[:, 3], in0=x1_(qf), in1=sb_, op=ALU.mult),
                      r=qkeys + ["sin"], w=[rtkey + (3,)])
                P.pool(lambda e, qb=qb, rt=rt: e.tensor_tensor(out=qb[:, :, 64:80], in0=rt[:, 0], in1=rt[:, 1],
                                                               op=ALU.subtract),
                       r=[rtkey + (0,), rtkey + (1,)], w=[qbkey + ("r1",)])
                P.dve(lambda e, qb=qb, rt=rt: e.tensor_tensor(out=qb[:, :, 80:96], in0=rt[:, 2], in1=rt[:, 3],
                                                              op=ALU.add),
                      r=[rtkey + (2,), rtkey + (3,)], w=[qbkey + ("r2",)])
                qbkeys = [qbkey + ("n", 0), qbkey + ("n", 1), qbkey + ("r1",), qbkey + ("r2",)]

                yield
                for h0 in range(2):
                    def tr8(e, qb=qb, h0=h0):
                        inst = None
                        for hh in range(8):
                            inst = e.transpose(pX[0:96, hh * 128:(hh + 1) * 128], qb[:, h0 * 8 + hh, :], ident[:])
                        return inst

                    P.pe(tr8, r=qbkeys, w=["pX"])
                    P.act(lambda e, qs=qs, j=j, h0=h0: e.copy(
                        out=qs[0:96, h0 * 8:(h0 + 1) * 8, j * 128:(j + 1) * 128],
                        in_=pX[0:96, :].rearrange("p (h t) -> p h t", h=8)), r=["pX"], w=[qskey + (j, h0)])
            for j0 in (0, 2):
                gl = [sub_body(j0), sub_body(j0 + 1)]
                while gl:
                    for g_ in list(gl):
                        try:
                            next(g_)
                        except StopIteration:
                            gl.remove(g_)
                if i + 1 < NT:
                    norm_sub(i + 1, j0)
                    norm_sub(i + 1, j0 + 1)
            P.dma("sp", scr["gates"][t0:t0 + T, :].rearrange("(j p) g -> p j g", p=128), gst[:],
                  r=[("gst", j) for j in range(4)])
            P.dma("sp", scr["qkT"][:, :, t0:t0 + T].rearrange("h d t -> d h t"), qs[0:96, :, :],
                  r=[qskey + (j, h0) for j in range(4) for h0 in range(2)])
        P.flush()


def phase_attn(P, nc, scr, cins):
    name = "at"
    with ExitStack() as st:
        def A(nm, shape, dt):
            return st.enter_context(nc.sbuf_tensor(f"{name}_{nm}", shape, dt))

        def PS(nm, shape, dt):
            return st.enter_context(nc.psum_tensor(f"{name}_{nm}", shape, dt))

        kt = Rot(A, "kt", 2, [128, S], BF16)
        qt = Rot(A, "qt", 2, [128, S], BF16)
        va = Rot(A, "va", 2, [128, NCH, 65], BF16)
        pt = Rot(A, "pt", 3, [128, 2 * T], BF16)
        rden = Rot(A, "rden", 2, [128, T], F32)
        rbs = Rot(A, "rbs", 2, [64, T], F32)
        ast = Rot(A, "ast", 2, [64, T], BF16)
        ones = A("ones", [128, 64], F32)
        pS = [PS(f"pS{b}", [128, 2 * T], F32) for b in range(2)]
        pO = [PS(f"pO{b}", [128, T], F32) for b in range(2)]
        pR = PS("pR", [128, T], F32)
        P.dve(lambda e: e.memset(ones[:], 1.0), w=["ones"])
        for b in range(2):
            P.dve(lambda e, b=b: e.memset(va.t[b][:, :, 64:65], 1.0), w=[("va1", b)])
        ns = 0
        no = 0
        NP2 = NCH // 2
        tail2 = None
        def head_loads(h):
            ktb, ktkey = kt.next()
            qtb, qtkey = qt.next()
            vab, vakey = va.next()
            P.dma("sp", ktb[0:96, :], scr["qkT"][8 + h], w=[ktkey])
            P.dma("sp", qtb[0:96, :], scr["qkT"][h], w=[qtkey])
            for q4 in range(4):
                P.dma("sp", vab[:, q4 * 16:(q4 + 1) * 16, 0:64],
                      scr["vS"][q4 * 2048:(q4 + 1) * 2048, h * 64:(h + 1) * 64].rearrange("(c p) d -> p c d", p=128),
                      w=[vakey + (q4,)])
            vkeys = [vakey + (q4,) for q4 in range(4)] + [("va1", va.i % 2)]
            return ktb, ktkey, qtb, qtkey, vab, vkeys

        hl = {0: head_loads(0)}
        for h in range(8):
            ktb, ktkey, qtb, qtkey, vab, vkeys = hl.pop(h)
            if h + 1 < 8:
                hl[h + 1] = head_loads(h + 1)
            for g in range(NT):
                po = pO[no % 2]
                pokey = ("pO", no % 2)
                no += 1
                qsl = qtb[0:96, g * T:(g + 1) * T]
                pend = []

                def qk(kp):
                    nonlocal ns
                    ps = pS[ns % 2]
                    pskey = ("pS", ns % 2)
                    ns += 1

                    def mm(e, ps=ps, kp=kp, ktb=ktb, qsl=qsl):
                        e.matmul(ps[:, 0:T], lhsT=ktb[0:96, (2 * kp) * 128:(2 * kp + 1) * 128], rhs=qsl, start=True,
                                 stop=True)
                        return e.matmul(ps[:, T:2 * T], lhsT=ktb[0:96, (2 * kp + 1) * 128:(2 * kp + 2) * 128], rhs=qsl,
                                        start=True, stop=True)

                    P.pe(mm, r=[ktkey, qtkey], w=[pskey])
                    return ps, pskey

                pend.append(qk(0))
                for kp in range(NP2):
                    if kp + 1 < NP2:
                        pend.append(qk(kp + 1))
                    ps, pskey = pend.pop(0)
                    ptb, ptkey = pt.next()
                    P.act(lambda e, ps=ps, ptb=ptb: e.activation(out=ptb[:], in_=ps[:], func=AF.Exp, scale=ATT_SCALE),
                          r=[pskey], w=[ptkey])

                    def pv(e, ptb=ptb, kp=kp, po=po, vab=vab):
                        e.matmul(po[0:65, :], lhsT=vab[:, 2 * kp, :], rhs=ptb[:, 0:T], start=(kp == 0), stop=False)
                        return e.matmul(po[0:65, :], lhsT=vab[:, 2 * kp + 1, :], rhs=ptb[:, T:2 * T], start=False,
                                        stop=(kp == NP2 - 1))

                    P.pe(pv, r=[ptkey] + vkeys, w=[pokey])
                    if kp == 2 and tail2 is not None:
                        tail2()
                        tail2 = None
                rd, rdkey = rden.next()
                rb, rbkey = rbs.next()
                ab, abkey = ast.next()
                P.dve(lambda e, rd=rd, po=po: e.reciprocal(out=rd[64:65, :], in_=po[64:65, :]), r=[pokey], w=[rdkey])

                def tail(rd=rd, rdkey=rdkey, rb=rb, rbkey=rbkey, ab=ab, abkey=abkey, po=po, pokey=pokey, h=h, g=g):
                    P.pe(lambda e: e.matmul(pR[0:64, :], lhsT=ones[64:65, 0:64], rhs=rd[64:65, :], start=True,
                                            stop=True), r=[rdkey, "ones"], w=["pR"])
                    P.dve(lambda e: e.tensor_copy(out=rb[:], in_=pR[0:64, :]), r=["pR"], w=[rbkey])
                    P.dve(lambda e: e.tensor_tensor(out=ab[:], in0=po[0:64, :], in1=rb[:], op=ALU.mult),
                          r=[pokey, rbkey], w=[abkey])
                    P.dma("sp", scr["aT"][h * 64:(h + 1) * 64, g * T:(g + 1) * T], ab[:], r=[abkey])

                tail2 = tail
        tail2()
        P.flush()


def phase_mlstm(P, nc, w, scr, cins):
    name = "ml"
    with ExitStack() as st:
        def A(nm, shape, dt):
            return st.enter_context(nc.sbuf_tensor(f"{name}_{nm}", shape, dt))

        def PS(nm, shape, dt):
            return st.enter_context(nc.psum_tensor(f"{name}_{nm}", shape, dt))

        ident, cf, epsb = load_consts(P, A, cins, need_cf=True)
        identf = cf[:, 0:128]
        triLE = cf[:, 128:256]
        triGE = cf[:, 256:384]
        onesf = cf[:, 384:512]
        G = A("G", [128, NCH, 16], F32)
        bi = A("bi", [128, 8], F32)
        bf_ = A("bf", [128, 8], F32)
        GI = A("GI", [128, 2, NCH, 4], F32)
        GF = A("GF", [128, 2, NCH, 4], F32)
        t1 = A("t1", [128, 512], F32)
        t2 = A("t2", [128, 512], F32)
        LF = A("LF", [128, 512], F32)
        Bc = A("Bc", [128, 512], F32)
        CS = A("CS", [128, 512], F32)
        BL = A("BL", [128, 512], F32)
        MXB = A("MXB", [128, 512], F32)
        mxcol = A("mxcol", [128, 4], F32)
        dg = A("dg", [128, 512], F32)
        M_all = A("M_all", [128, 512], F32)
        DARG = A("DARG", [128, 512], F32)
        DEC = A("DEC", [128, 512], F32)
        A_all = A("A_all", [128, 512], F32)
        THR = A("THR", [128, 512], F32)
        mcur = A("mcur", [128, 8], F32)
        wq = A("wq", [128, 4, 128], BF16)
        wk = A("wk", [128, 4, 128], BF16)
        wvv = A("wv", [128, 4, 128], BF16)
        cw = A("cw", [128, 4, 5], F32)
        cbias = A("cb", [128, 4], F32)
        mnw = A("mnw", [128, 4], F32)
        msk = A("msk", [128, 4], F32)
        p0 = PS("p0", [128, 512], F32)
        p1 = PS("p1", [128, 512], F32)
        pS_ = PS("pS", [128, 512], F32)
        pKV = PS("pKV", [128, 1024], F32)
        pN = PS("pN", [128, 1024], F32)
        pH = PS("pH", [128, 512], F32)

        for q4 in range(4):
            P.dma("sp", G[:, q4 * 16:(q4 + 1) * 16, :],
                  scr["gates"][q4 * 2048:(q4 + 1) * 2048, :].rearrange("(c p) g -> p c g", p=128), w=[("G", q4)])
        gk = [("G", q4) for q4 in range(4)]
        P.dma("sp", bi[:], w["b_igate"].partition_broadcast(128), w=["bi"])
        P.dma("sp", bf_[:], w["b_fgate"].partition_broadcast(128), w=["bf"])
        for (wt, key) in ((wq, "w_mq"), (wk, "w_mk"), (wvv, "w_mv")):
            P.dma("pool", wt[:], w[key].rearrange("h d e -> d h e"), w=[key])
        for jj in range(5):
            P.dma("sp", cw[:, :, jj], w["conv_w"][jj].rearrange("(k p) -> p k", p=128), w=[("cw", jj)],
                  allow_slow_non_contiguous=True)
        P.dma("sp", cbias[:], w["conv_b"].rearrange("(k p) -> p k", p=128), w=["cb"], allow_slow_non_contiguous=True)
        P.dma("sp", mnw[:], w["m_norm_w"].rearrange("(k p) -> p k", p=128), w=["mnw"], allow_slow_non_contiguous=True)
        P.dma("sp", msk[:], w["m_skip"].rearrange("(k p) -> p k", p=128), w=["msk"], allow_slow_non_contiguous=True)

        dgw = A("dgw", [128, 4, 5, 128], BF16)
        cbr = A("cbr", [1, 512], F32)
        cbr2 = A("cbr2", [1, 512], F32)
        brow = A("brow", [1, 2, 512], BF16)
        onesb = A("onesb", [1, 128], BF16)
        P.dma("sp", cbr[:], w["conv_b"].rearrange("(o n) -> o n", o=1), w=["cbr"])
        P.dve(lambda e: e.memset(onesb[:], 1.0), w=["onesb"])
        P.fence()
        for hc in range(4):
            for jj in range(5):
                P.dve(lambda e, hc=hc, jj=jj: e.tensor_scalar(out=dgw[:, hc, jj, :], in0=identf,
                                                              scalar1=cw[:, hc, jj:jj + 1], scalar2=None,
                                                              op0=ALU.mult), w=[("dgw", hc, jj)])
        P.dve(lambda e: e.tensor_copy(out=brow[:, 0, :], in_=cbr[:]), w=["brow0"])
        P.dve(lambda e: e.tensor_copy(out=cbr2[:], in_=brow[:, 0, :]), r=["brow0"], w=["cbr2"])
        P.dve(lambda e: e.tensor_tensor(out=cbr2[:], in0=cbr[:], in1=cbr2[:], op=ALU.subtract), r=["cbr2"], w=["cbr2"])
        P.dve(lambda e: e.tensor_copy(out=brow[:, 1, :], in_=cbr2[:]), r=["cbr2"], w=["brow1"])
        for d in range(2):
            P.dve(lambda e, d=d: e.tensor_tensor(out=GI[:, d], in0=G[:, :, d * 4:d * 4 + 4],
                                                 in1=bc(bi[:, d * 4:d * 4 + 4], [128, NCH, 4], 1), op=ALU.add),
                  r=gk + ["bi"], w=[("GI", d)])
            P.dve(lambda e, d=d: e.tensor_tensor(out=GF[:, d], in0=G[:, :, 8 + d * 4:12 + d * 4],
                                                 in1=bc(bf_[:, d * 4:d * 4 + 4], [128, NCH, 4], 1), op=ALU.add),
                  r=gk + ["bf"], w=[("GF", d)])
        GFf = GF[:].rearrange("p d c h -> p (d c h)")
        GIf = GI[:].rearrange("p d c h -> p (d c h)")
        gfk = [("GF", 0), ("GF", 1)]
        gik = [("GI", 0), ("GI", 1)]
        P.dve(lambda e: e.scalar_tensor_tensor(out=t1[:], in0=GFf, scalar=-1.0, in1=GFf, op0=ALU.mult, op1=ALU.max),
              r=gfk, w=["t1"])
        P.act(lambda e: e.activation(out=t1[:], in_=t1[:], func=AF.Exp, scale=-1.0), r=["t1"], w=["t1"])
        P.act(lambda e: e.activation(out=t1[:], in_=t1[:], func=AF.Ln, bias=1.0), r=["t1"], w=["t1"])
        P.dve(lambda e: e.tensor_scalar(out=t2[:], in0=GFf, scalar1=0.0, scalar2=None, op0=ALU.min), r=gfk, w=["t2"])
        P.dve(lambda e: e.tensor_tensor(out=LF[:], in0=t2[:], in1=t1[:], op=ALU.subtract), r=["t1", "t2"], w=["LF"])
        P.pe(lambda e: e.matmul(p0[:, 0:256], lhsT=triLE, rhs=LF[:, 0:256], start=True, stop=True),
             r=["LF", "cf"], w=["p0a"])
        P.pe(lambda e: e.matmul(p0[:, 256:512], lhsT=triGE, rhs=LF[:, 256:512], start=True, stop=True),
             r=["LF", "cf"], w=["p0b"])
        P.pe(lambda e: e.matmul(p1[:], lhsT=onesf, rhs=LF[:], start=True, stop=True), r=["LF", "cf"], w=["p1"])
        P.dve(lambda e: e.tensor_copy(out=Bc[:], in_=p0[:]), r=["p0a", "p0b"], w=["Bc"])
        P.act(lambda e: e.copy(out=BL[:], in_=p1[:]), r=["p1"], w=["BL"])
        P.dve(lambda e: e.tensor_tensor(out=CS[:], in0=GIf, in1=Bc[:], op=ALU.subtract), r=gik + ["Bc"], w=["CS"])

        def trf(e):
            inst = None
            for k in range(4):
                inst = e.transpose(p0[:, k * 128:(k + 1) * 128], CS[:, k * 128:(k + 1) * 128], identf)
            return inst

        P.pe(trf, r=["CS", "cf", "Bc"], w=["p0a", "p0b"])
        P.dve(lambda e: e.tensor_reduce(out=mxcol[:], in_=p0[:].rearrange("p (k s) -> p k s", k=4), axis=AX.X,
                                        op=ALU.max), r=["p0a", "p0b"], w=["mxcol"])
        for k in range(4):
            P.dve(lambda e, k=k: e.tensor_scalar(out=dg[:, k * 128:(k + 1) * 128], in0=identf,
                                                 scalar1=mxcol[:, k:k + 1], scalar2=None, op0=ALU.mult),
                  r=["mxcol", "cf"], w=[("dg", k)])
        P.pe(lambda e: e.matmul(p1[:], lhsT=onesf, rhs=dg[:], start=True, stop=True),
             r=[("dg", k) for k in range(4)] + ["cf", "BL"], w=["p1"])
        P.dve(lambda e: e.tensor_copy(out=MXB[:], in_=p1[:]), r=["p1"], w=["MXB"])
        P.dve(lambda e: e.memset(mcur[:], 0.0), w=[("mcur", 0), ("mcur", 1)])
        for jstep in range(NCH):
            for d in range(2):
                c = jstep if d == 0 else NCH - 1 - jstep
                sl = slice(d * 256 + c * 4, d * 256 + c * 4 + 4)
                mc = mcur[:, d * 4:d * 4 + 4]
                eng = P.dve
                eng(lambda e, sl=sl, mc=mc: e.tensor_tensor(out=M_all[:, sl], in0=mc, in1=MXB[:, sl], op=ALU.max),
                    r=[("mcur", d), "MXB"], w=[("M", d, c)])
                eng(lambda e, sl=sl, mc=mc: e.tensor_tensor(out=DARG[:, sl], in0=mc, in1=M_all[:, sl],
                                                            op=ALU.subtract),
                    r=[("mcur", d), ("M", d, c)], w=[("DARG", d, c)])
                eng(lambda e, sl=sl, mc=mc: e.tensor_tensor(out=mc, in0=M_all[:, sl], in1=BL[:, sl], op=ALU.add),
                    r=[("M", d, c), "BL", ("DARG", d, c)], w=[("mcur", d)])
        mk = [("M", d, c) for d in range(2) for c in range(NCH)]
        dk = [("DARG", d, c) for d in range(2) for c in range(NCH)]
        P.act(lambda e: e.activation(out=DEC[:], in_=DARG[:], func=AF.Exp), r=dk, w=["DEC"])
        P.dve(lambda e: e.tensor_tensor(out=t1[:], in0=CS[:], in1=M_all[:], op=ALU.subtract), r=["CS"] + mk, w=["t1"])
        P.act(lambda e: e.activation(out=A_all[:], in_=t1[:], func=AF.Exp), r=["t1"], w=["A_all"])
        P.dve(lambda e: e.tensor_tensor(out=t2[:], in0=Bc[:], in1=M_all[:], op=ALU.add), r=["Bc"] + mk, w=["t2"])
        P.act(lambda e: e.activation(out=THR[:], in_=t2[:], func=AF.Exp, scale=-1.0), r=["t2"], w=["THR"])
        P.flush()

        mt = Rot(A, "mt", 5, [128, 4, 132], F32)
        xcT = Rot(A, "xcT", 3, [128, 4, 128], F32)
        xcb = Rot(A, "xcb", 2, [128, 4, 128], BF16)
        mb = Rot(A, "mb", 2, [128, 4, 132], BF16)
        qTb = Rot(A, "qTb", 2, [128, 4, 128], BF16)
        kTb = Rot(A, "kTb", 2, [128, 4, 128], BF16)
        kb = Rot(A, "kb", 2, [128, 4, 128], BF16)
        vab = Rot(A, "vab", 2, [128, 4, 129], BF16)
        scm = Rot(A, "scm", 2, [128, 4, 128], BF16)
        Cd = Rot(A, "Cd", 2, [128, 4, 129], F32)
        KVs = Rot(A, "KVs", 2, [128, 4, 129], F32)
        Cdb = Rot(A, "Cdb", 2, [128, 4, 129], BF16)
        Call = A("Call", [128, 4, 129], F32)
        sm = Rot(A, "sm", 2, [128, 4, 4], F32)
        hout = Rot(A, "hout", 2, [128, 4, 128], F32)
        hbt = Rot(A, "hbt", 5, [128, 4, 128], F32)
        sopt = Rot(A, "sopt", 5, [128, 4, 128], F32)
        hsq = A("hsq", [128, 4, 128], F32)
        hss = Rot(A, "hss", 2, [128, 2, 4], F32)
        ut = Rot(A, "ut", 2, [128, 4, 128], F32)
        bmst = Rot(A, "bmst", 2, [128, 4, T], BF16)
        mask = {0: triLE, 1: triGE}

        for d in (1, 0):
            P.dve(lambda e: e.memset(Call[:], 0.0), w=["Call"])
            order = range(NCH) if d == 0 else range(NCH - 1, -1, -1)
            bst = {"bms": None, "key": None}

            def chunk_body(c, d=d, bst=bst):
                t0 = c * 128
                sl = slice(d * 256 + c * 4, d * 256 + c * 4 + 4)
                m_, mkey = mt.next()
                lo, hi = max(t0 - 2, 0), min(t0 + 130, S)
                o0 = lo - (t0 - 2)
                wl = [mkey]
                if c == 0:
                    P.pool(lambda e, m_=m_: e.memset(m_[:, :, 0:2], 0.0), w=[mkey + ("z",)])
                    wl.append(mkey + ("z",))
                if c == NCH - 1:
                    P.pool(lambda e, m_=m_: e.memset(m_[:, :, 130:132], 0.0), w=[mkey + ("z",)])
                    wl.append(mkey + ("z",))
                P.dma("sp", m_[:, :, o0:o0 + (hi - lo)], scr["minT"][:, lo:hi].rearrange("(k p) t -> p k t", p=128),
                      w=[mkey], r=[mkey + ("z",)])
                mkeys = [mkey]
                if d == 0:
                    hb_, hbkey = hbt.next()
                    so_, sokey = sopt.next()
                    P.dma("sp", hb_[:].rearrange("p h e -> p (h e)"), scr["hb"][t0:t0 + 128, :], w=[hbkey])
                    P.dma("sp", so_[:], scr["sopT"][:, t0:t0 + 128].rearrange("(k p) t -> p k t", p=128), w=[sokey])
                yield
                mbb, mbkey = mb.next()
                P.pool(lambda e, mbb=mbb, m_=m_: e.tensor_copy(out=mbb[:], in_=m_[:]), r=mkeys, w=[mbkey])

                def mm_conv(e, mbb=mbb):
                    inst = None
                    for hc in range(4):
                        o_ = p0[:, hc * 128:(hc + 1) * 128]
                        for jj in range(5):
                            e.matmul(o_, lhsT=dgw[:, hc, jj, :], rhs=mbb[:, hc, jj:jj + 128], start=(jj == 0), stop=False)
                        e.matmul(o_, lhsT=brow[0:1, 0, hc * 128:(hc + 1) * 128], rhs=onesb[0:1, :], start=False, stop=False)
                        inst = e.matmul(o_, lhsT=brow[0:1, 1, hc * 128:(hc + 1) * 128], rhs=onesb[0:1, :], start=False,
                                        stop=True)
                    return inst

                P.pe(mm_conv, r=[mbkey, "dgw", "brow"], w=["p0"])
                xb, xbkey = xcb.next()
                P.act(lambda e, xb=xb: e.activation(out=xb[:].rearrange("p h t -> p (h t)"), in_=p0[:], func=AF.Silu),
                      r=["p0"], w=[xbkey])
                if d == 0:
                    xf, xfkey = xcT.next()
                    P.act(lambda e, xf=xf: e.activation(out=xf[:].rearrange("p h t -> p (h t)"), in_=p0[:],
                                                        func=AF.Silu), r=["p0"], w=[xfkey])
                qT_, qTkey = qTb.next()
                kT_, kTkey = kTb.next()
                k_, kkey = kb.next()
                va_, vakey = vab.next()

                def proj(e, wt, x, out_p, tmaj):
                    inst = None
                    for h in range(4):
                        if tmaj:
                            inst = e.matmul(out_p[:, h * 128:(h + 1) * 128], lhsT=x[:, h, :], rhs=wt[:, h, :],
                                            start=True, stop=True)
                        else:
                            inst = e.matmul(out_p[:, h * 128:(h + 1) * 128], lhsT=wt[:, h, :], rhs=x[:, h, :],
                                            start=True, stop=True)
                    return inst

                P.pe(lambda e, xb=xb: proj(e, wq, xb, p1, False), r=[xbkey, "w_mq"], w=["p1"])
                P.act(lambda e, qT_=qT_: e.copy(out=qT_[:].rearrange("p h t -> p (h t)"), in_=p1[:]), r=["p1"],
                      w=[qTkey])
                P.pe(lambda e, xb=xb: proj(e, wk, xb, p0, False), r=[xbkey, "w_mk"], w=["p0"])
                P.act(lambda e, kT_=kT_: e.mul(out=kT_[:].rearrange("p h t -> p (h t)"), in_=p0[:], mul=128 ** -0.5),
                      r=["p0"], w=[kTkey])
                P.pe(lambda e, xb=xb: proj(e, wk, xb, p1, True), r=[xbkey, "w_mk"], w=["p1"])
                P.act(lambda e, k_=k_: e.mul(out=k_[:].rearrange("p h t -> p (h t)"), in_=p1[:], mul=128 ** -0.5),
                      r=["p1"], w=[kkey])
                P.pe(lambda e, mbb=mbb: proj(e, wvv, mbb[:, :, 2:130], p0, True), r=[mbkey, "w_mv"], w=["p0"])
                P.dve(lambda e, va_=va_, sl=sl: e.tensor_tensor(
                    out=va_[:, :, 0:128], in0=p0[:].rearrange("p (h e) -> p h e", h=4),
                    in1=bc(A_all[:, sl], [128, 4, 128], 2), op=ALU.mult), r=["p0", "A_all"], w=[vakey + (0,)])
                P.dve(lambda e, va_=va_, sl=sl: e.tensor_copy(out=va_[:, :, 128:129], in_=A_all[:, sl].unsqueeze(2)),
                      r=["A_all"], w=[vakey + (1,)])
                vakeys = [vakey + (0,), vakey + (1,)]

                def mm_s(e, kT_=kT_, qT_=qT_):
                    inst = None
                    for h in range(4):
                        inst = e.matmul(pS_[:, h * 128:(h + 1) * 128], lhsT=kT_[:, h, :], rhs=qT_[:, h, :],
                                        start=True, stop=True)
                    return inst

                P.pe(mm_s, r=[kTkey, qTkey], w=["pS"])
                sc_, sckey = scm.next()
                P.dve(lambda e, sc_=sc_, d=d: e.tensor_tensor(out=sc_[:], in0=pS_[:].rearrange("p (h t) -> p h t", h=4),
                                                              in1=bc(mask[d], [128, 4, 128], 1), op=ALU.mult),
                      r=["pS", "cf"], w=[sckey])

                def mm_kv(e, k_=k_, va_=va_):
                    inst = None
                    for h in range(4):
                        inst = e.matmul(pKV[:, h * 256:h * 256 + 129], lhsT=k_[:, h, :], rhs=va_[:, h, :],
                                        start=True, stop=True)
                    return inst

                P.pe(mm_kv, r=[kkey] + vakeys, w=["pKV"])
                kv3 = pKV[:].rearrange("p (h x) -> p h x", h=4)[:, :, 0:129]
                kvs_, kvskey = KVs.next()
                P.dve(lambda e, kvs_=kvs_: e.tensor_copy(out=kvs_[:], in_=kv3), r=["pKV"], w=[kvskey])
                yield
                cd_, cdkey = Cd.next()
                cdb_, cdbkey = Cdb.next()
                P.dve(lambda e, cd_=cd_, sl=sl: e.tensor_tensor(out=cd_[:], in0=Call[:],
                                                                in1=bc(DEC[:, sl], [128, 4, 129], 2), op=ALU.mult),
                      r=["Call", "DEC"], w=[cdkey])
                P.dve(lambda e, cd_=cd_, kvs_=kvs_: e.tensor_tensor(out=Call[:], in0=cd_[:], in1=kvs_[:], op=ALU.add),
                      r=[cdkey, kvskey], w=["Call"])
                P.act(lambda e, cd_=cd_, cdb_=cdb_: e.copy(out=cdb_[:], in_=cd_[:]), r=[cdkey], w=[cdbkey])
                yield

                def mm_n(e, sc_=sc_, va_=va_, qT_=qT_, cdb_=cdb_):
                    inst = None
                    for h in range(4):
                        e.matmul(pN[:, h * 256:h * 256 + 129], lhsT=sc_[:, h, :], rhs=va_[:, h, :], start=True,
                                 stop=False)
                        inst = e.matmul(pN[:, h * 256:h * 256 + 129], lhsT=qT_[:, h, :], rhs=cdb_[:, h, :],
                                        start=False, stop=True)
                    return inst

                P.pe(mm_n, r=[sckey, qTkey, cdbkey] + vakeys, w=["pN"])
                n3 = pN[:].rearrange("p (h x) -> p h x", h=4)
                yield
                s_, skey = sm.next()
                den = n3[:, :, 128]
                P.dve(lambda e, s_=s_: e.tensor_copy(out=s_[:, 3, :], in_=den), r=["pN"], w=[skey + (3,)])
                P.dve(lambda e, s_=s_: e.scalar_tensor_tensor(out=s_[:, 0, :], in0=s_[:, 3, :], scalar=-1.0,
                                                              in1=s_[:, 3, :], op0=ALU.mult, op1=ALU.max),
                      r=[skey + (3,)], w=[skey + (0,)])
                P.dve(lambda e, s_=s_, sl=sl: e.tensor_tensor(out=s_[:, 1, :], in0=s_[:, 0, :], in1=THR[:, sl],
                                                              op=ALU.max), r=[skey + (0,), "THR"], w=[skey + (1,)])
                P.dve(lambda e, s_=s_: e.reciprocal(out=s_[:, 2, :], in_=s_[:, 1, :]), r=[skey + (1,)],
                      w=[skey + (2,)])
                ho, hokey = hout.next()
                P.dve(lambda e, ho=ho, s_=s_: e.tensor_tensor(out=ho[:], in0=n3[:, :, 0:128],
                                                              in1=bc(s_[:, 2, :], [128, 4, 128], 2), op=ALU.mult),
                      r=["pN", skey + (2,)], w=[hokey])
                if d == 1:
                    P.dma("sp", scr["hb"][t0:t0 + 128, :], ho[:].rearrange("p h e -> p (h e)"), r=[hokey])
                    return
                P.pool(lambda e, ho=ho, hb_=hb_: e.tensor_tensor(out=hb_[:], in0=ho[:], in1=hb_[:], op=ALU.add),
                       r=[hokey, hbkey], w=[hbkey])
                P.pool(lambda e, hb_=hb_: e.tensor_tensor(out=hsq[:], in0=hb_[:], in1=hb_[:], op=ALU.mult),
                       r=[hbkey], w=["hsq"])
                hs_, hskey = hss.next()
                P.dve(lambda e, hs_=hs_: e.tensor_reduce(out=hs_[:, 0, :], in_=hsq[:], axis=AX.X, op=ALU.add),
                      r=["hsq"], w=[hskey + (0,)])
                rstd_pool(P, hs_[:, 1, :], hs_[:, 0, :], 128, epsb[:, 0:4], [hskey + (0,)], [hskey + (1,)])
                P.pool(lambda e, hb_=hb_, hs_=hs_: e.tensor_tensor(out=hb_[:], in0=hb_[:],
                                                                   in1=bc(hs_[:, 1, :], [128, 4, 128], 2),
                                                                   op=ALU.mult), r=[hbkey, hskey + (1,)], w=[hbkey])

                def trh(e, hb_=hb_):
                    inst = None
                    for h in range(4):
                        inst = e.transpose(pH[:, h * 128:(h + 1) * 128], hb_[:, h, :], identf)
                    return inst

                P.pe(trh, r=[hbkey, "cf"], w=["pH"])
                u_, ukey = ut.next()
                for h in range(4):
                    P.act(lambda e, u_=u_, h=h: e.activation(out=u_[:, h, :], in_=pH[:, h * 128:(h + 1) * 128],
                                                             func=AF.Identity, scale=mnw[:, h:h + 1]),
                          r=["pH", "mnw"], w=[ukey + (h,)])
                    P.dve(lambda e, u_=u_, h=h, xf=xf: e.scalar_tensor_tensor(
                        out=u_[:, h, :], in0=xf[:, h, :], scalar=msk[:, h:h + 1], in1=u_[:, h, :], op0=ALU.mult,
                        op1=ALU.add), r=[ukey + (h,), xfkey, "msk"], w=[ukey + (h,)])
                if c % 4 == 0:
                    bst["bms"], bst["key"] = bmst.next()
                bms, bmskey = bst["bms"], bst["key"]
                cj = c % 4
                P.dve(lambda e, u_=u_, so_=so_, bms=bms, cj=cj: e.tensor_tensor(
                    out=bms[:, :, cj * 128:(cj + 1) * 128], in0=u_[:], in1=so_[:], op=ALU.mult),
                    r=[ukey + (h,) for h in range(4)] + [sokey], w=[bmskey + (cj,)])
                if cj == 3:
                    tt0 = (c - 3) * 128
                    P.dma("sp", scr["bmT"][:, tt0:tt0 + T].rearrange("(k p) t -> p k t", p=128), bms[:],
                          r=[bmskey + (q,) for q in range(4)])

            order = list(order)
            n_ = len(order)
            gens = {c: chunk_body(c) for c in order}
            LA = 3
            for k in range(min(LA, n_)):
                next(gens[order[k]])
            next(gens[order[0]])
            for t in range(n_ + 1):
                if t < n_:
                    next(gens[order[t]])
                if t >= 1:
                    for _ in gens.pop(order[t - 1]):
                        pass
                if t < n_:
                    next(gens[order[t]])
                if t + 1 < n_:
                    next(gens[order[t + 1]])
                if t + LA < n_:
                    next(gens[order[t + LA]])
            P.flush()


def phase_merge(P, nc, x1s, w, scr, cins):
    name = "mg"
    with ExitStack() as st:
        def A(nm, shape, dt):
            return st.enter_context(nc.sbuf_tensor(f"{name}_{nm}", shape, dt))

        def PS(nm, shape, dt):
            return st.enter_context(nc.psum_tensor(f"{name}_{nm}", shape, dt))

        wa = A("wa", [128, 4, D], BF16)
        wb = A("wb", [128, 4, D], BF16)
        wo = A("wo", [128, 8, D], BF16)
        xt = Rot(A, "xt", 2, [128, 4, D], F32)
        at = Rot(A, "at", 2, [128, 4, T], BF16)
        bt = Rot(A, "bt", 2, [128, 4, T], BF16)
        sga = Rot(A, "sga", 2, [128, 8, T], F32)
        sgb = Rot(A, "sgb", 2, [128, 8, T], F32)
        mT = Rot(A, "mT", 2, [128, 8, T], BF16)
        ta = Rot(A, "ta", 2, [128, T], F32)
        tb = Rot(A, "tb", 2, [128, T], F32)
        pA_ = [PS(f"pA{b}", [128, T], F32) for b in range(2)]
        pB_ = [PS(f"pB{b}", [128, T], F32) for b in range(2)]
        pD = [PS(f"pD{b}", [128, T], F32) for b in range(2)]
        P.dma("pool", wa[:], w["w_branch_a"].rearrange("(k p) n -> p k n", p=128), w=["wa"])
        P.dma("pool", wb[:], w["w_branch_b"].rearrange("(k p) n -> p k n", p=128), w=["wb"])
        P.dma("pool", wo[:], w["w_out"].rearrange("(k p) n -> p k n", p=128), w=["wo"])

        def xview(ap, i):
            return ap[i * T:(i + 1) * T, :].rearrange("(j p) d -> p j d", p=128)

        bufs = {}

        def load(i):
            t0 = i * T
            x_, xk = xt.next()
            a_, ak = at.next()
            b_, bk = bt.next()
            ga_, gak = sga.next()
            gb_, gbk = sgb.next()
            P.dma("sp", a_[:], scr["aT"][:, t0:t0 + T].rearrange("(k p) t -> p k t", p=128), w=[ak])
            P.dma("sp", b_[:], scr["bmT"][:, t0:t0 + T].rearrange("(k p) t -> p k t", p=128), w=[bk])
            P.dma("sp", ga_[:], scr["sgT"][0:1024, t0:t0 + T].rearrange("(k p) t -> p k t", p=128), w=[gak])
            P.dma("sp", gb_[:], scr["sgT"][1024:2048, t0:t0 + T].rearrange("(k p) t -> p k t", p=128), w=[gbk])
            P.dma("sp", x_[:], xview(x1s, i), w=[xk + (j,) for j in range(4)])
            bufs[i] = (x_, xk, a_, ak, b_, bk, ga_, gak, gb_, gbk)

        load(0)
        na = 0
        nd = 0
        for i in range(NT):
            if i + 1 < NT:
                load(i + 1)
            x_, xk, a_, ak, b_, bk, ga_, gak, gb_, gbk = bufs.pop(i)
            m_, mk = mT.next()
            for cc in range(8):
                pa, pb = pA_[na % 2], pB_[na % 2]
                pak, pbk = ("pA", na % 2), ("pB", na % 2)
                na += 1

                def mm_a(e, pa=pa, cc=cc, a_=a_):
                    inst = None
                    for k in range(4):
                        inst = e.matmul(pa[:], lhsT=wa[:, k, cc * 128:(cc + 1) * 128], rhs=a_[:, k, :],
                                        start=(k == 0), stop=(k == 3))
                    return inst

                def mm_b(e, pb=pb, cc=cc, b_=b_):
                    inst = None
                    for k in range(4):
                        inst = e.matmul(pb[:], lhsT=wb[:, k, cc * 128:(cc + 1) * 128], rhs=b_[:, k, :],
                                        start=(k == 0), stop=(k == 3))
                    return inst

                P.pe(mm_a, r=[ak, "wa"], w=[pak])
                P.pe(mm_b, r=[bk, "wb"], w=[pbk])
                ta_, tak = ta.next()
                tb_, tbk = tb.next()
                P.dve(lambda e, ta_=ta_, pa=pa, ga_=ga_, cc=cc: e.tensor_tensor(out=ta_[:], in0=pa[:],
                                                                                in1=ga_[:, cc, :], op=ALU.mult),
                      r=[pak, gak], w=[tak])
                P.dve(lambda e, tb_=tb_, pb=pb, gb_=gb_, cc=cc: e.tensor_tensor(out=tb_[:], in0=pb[:],
                                                                                in1=gb_[:, cc, :], op=ALU.mult),
                      r=[pbk, gbk], w=[tbk])
                P.pool(lambda e, ta_=ta_, tb_=tb_, m_=m_, cc=cc: e.tensor_tensor(out=m_[:, cc, :], in0=ta_[:],
                                                                                 in1=tb_[:], op=ALU.add),
                       r=[tak, tbk], w=[mk + (cc,)])
            mkeys = [mk + (cc,) for cc in range(8)]
            for j in range(4):
                for half in range(2):
                    d_ = pD[nd % 2]
                    dk_ = ("pD", nd % 2)
                    nd += 1

                    def mm_o(e, d_=d_, j=j, half=half, m_=m_):
                        inst = None
                        for k in range(8):
                            inst = e.matmul(d_[:], lhsT=m_[:, k, j * 128:(j + 1) * 128],
                                            rhs=wo[:, k, half * 512:(half + 1) * 512], start=(k == 0), stop=(k == 7))
                        return inst

                    P.pe(mm_o, r=mkeys + ["wo"], w=[dk_])
                    xs = x_[:, j, half * 512:(half + 1) * 512]
                    P.dve(lambda e, d_=d_, xs=xs: e.tensor_tensor(out=xs, in0=d_[:], in1=xs, op=ALU.add),
                          r=[dk_, xk + (j,)], w=[xk + (j,)])
            P.dma("sp", xview(x1s, i), x_[:], r=[xk + (j,) for j in range(4)])
        P.flush()


W_NAMES = ["ffn1_norm_w", "ffn1_w_gate", "ffn1_w_up", "ffn1_w_down", "mix_norm_w", "w_in", "q_a_norm_w", "w_uq",
           "kv_a_norm_w", "w_uk", "w_uv", "q_norm_w", "k_norm_w", "w_branch_a", "conv_w", "conv_b", "w_mq", "w_mk",
           "w_mv", "b_igate", "b_fgate", "m_norm_w", "m_skip", "w_branch_b", "w_out", "ffn2_norm_w", "ffn2_w_gate",
           "ffn2_w_up", "ffn2_w_down", "final_norm_w"]
W_SHAPES = {
    "ffn1_norm_w": [D], "ffn1_w_gate": [D, FF], "ffn1_w_up": [D, FF], "ffn1_w_down": [FF, D], "mix_norm_w": [D],
    "w_in": [D, IN_DIM], "q_a_norm_w": [256], "w_uq": [256, 768], "kv_a_norm_w": [128], "w_uk": [128, 512],
    "w_uv": [128, 512], "q_norm_w": [96], "k_norm_w": [96], "w_branch_a": [512, D], "conv_w": [5, 512],
    "conv_b": [512], "w_mq": [4, 128, 128], "w_mk": [4, 128, 128], "w_mv": [4, 128, 128], "b_igate": [8],
    "b_fgate": [8], "m_norm_w": [512], "m_skip": [512], "w_branch_b": [512, D], "w_out": [D, D],
    "ffn2_norm_w": [D], "ffn2_w_gate": [D, FF], "ffn2_w_up": [D, FF], "ffn2_w_down": [FF, D], "final_norm_w": [D],
}


def build_program(phases=("ffn1", "inproj", "attn", "mlstm", "merge", "ffn2"), debug=()):
    nc = bass.Bass("TRN2", target_bir_lowering=False)
    x = nc.dram_tensor("x", [S, D], F32, kind="ExternalInput").ap()
    pos = nc.dram_tensor("positions", [S], I32, kind="ExternalInput").ap()
    w = {k: nc.dram_tensor(k, W_SHAPES[k], F32, kind="ExternalInput").ap() for k in W_NAMES}
    cins = {
        "c_ident": nc.dram_tensor("c_ident", [128, 128], BF16, kind="ExternalInput").ap(),
        "c_f32": nc.dram_tensor("c_f32", [128, 512], F32, kind="ExternalInput").ap(),
        "c_inv": nc.dram_tensor("c_inv", [16], F32, kind="ExternalInput").ap(),
    }
    out = nc.dram_tensor("out", [S, D], F32, kind="ExternalOutput").ap()

    def scratch(nm, shape, dt):
        kind = "ExternalOutput" if nm in debug else "Internal"
        return nc.dram_tensor("s_" + nm, shape, dt, kind=kind).ap()

    x1s = scratch("x1", [S, D], F32)
    scr = {
        "sgT": scratch("sgT", [2048, S], F32),
        "minT": scratch("minT", [512, S], F32),
        "sopT": scratch("sopT", [512, S], F32),
        "gates": scratch("gates", [S, 16], F32),
        "qkT": scratch("qkT", [16, 96, S], BF16),
        "vS": scratch("vS", [S, 512], BF16),
        "aT": scratch("aT", [512, S], BF16),
        "bmT": scratch("bmT", [512, S], BF16),
        "hb": scratch("hb", [S, 512], F32),
    }
    with ExitStack() as top:
        sems = [top.enter_context(nc.semaphore(f"sem{i}")) for i in range(96)]
        P = Prog(nc, sems)
        if "ffn1" in phases:
            phase_ffn(P, nc, "f1", x, x1s, w["ffn1_norm_w"], w["ffn1_w_gate"], w["ffn1_w_up"], w["ffn1_w_down"], cins)
        if "inproj" in phases:
            phase_inproj(P, nc, x1s, pos, w, scr, cins)
        if "attn" in phases:
            phase_attn(P, nc, scr, cins)
        if "mlstm" in phases:
            phase_mlstm(P, nc, w, scr, cins)
        if "merge" in phases:
            phase_merge(P, nc, x1s, w, scr, cins)
        if "ffn2" in phases:
            phase_ffn(P, nc, "f2", x1s, out, w["ffn2_norm_w"], w["ffn2_w_gate"], w["ffn2_w_up"], w["ffn2_w_down"],
                      cins, final_w=w["final_norm_w"])
    return nc


def make_consts():
    ident = np.eye(128, dtype=np.float32)
    s_idx = np.arange(128)[:, None]
    t_idx = np.arange(128)[None, :]
    cf = np.concatenate([ident, (s_idx <= t_idx).astype(np.float32), (s_idx >= t_idx).astype(np.float32),
                         np.ones((128, 128), np.float32)], axis=1)
    half = 16
    inv = (np.float32(10000.0) ** (-np.arange(half, dtype=np.float32) / np.float32(half))).astype(np.float32)
    return {"c_ident": ident.astype(ml_dtypes.bfloat16), "c_f32": np.ascontiguousarray(cf), "c_inv": inv}


def make_in_maps(inputs, n_cores=8):
    consts = make_consts()
    maps = []
    for b in range(n_cores):
        m = {"x": np.ascontiguousarray(inputs["x"][b]), "positions": np.ascontiguousarray(inputs["positions"][b])}
        for k in W_NAMES:
            m[k] = np.ascontiguousarray(np.asarray(inputs[k])[0])
        m.update(consts)
        maps.append(m)
    return maps


def kernel(**inputs):
    inputs = {k: np.asarray(v) for k, v in inputs.items()}
    nc = build_program()
    in_maps = make_in_maps(inputs, 8)
    res = run_bass_kernel_spmd(nc, in_maps, core_ids=list(range(8)))
    return np.stack([np.asarray(r["out"]) for r in res.results], axis=0).astype(np.float32)
```

```python
import math
from contextlib import ExitStack
from collections import defaultdict

import numpy as np
import ml_dtypes
import concourse.bass as bass
import concourse.mybir as mybir
from concourse.bass_utils import run_bass_kernel_spmd

F32 = mybir.dt.float32
BF16 = mybir.dt.bfloat16
I32 = mybir.dt.int32
AF = mybir.ActivationFunctionType
ALU = mybir.AluOpType
AX = mybir.AxisListType

S = 8192
D = 1024
FF = 2816
NHC = FF // 128
T = 512
NT = S // T
NCH = S // 128
IN_DIM = 3504
EPS = 1e-6
ATT_SCALE = 96 ** -0.5


class _Op:
    __slots__ = ("eng", "fn", "deps", "dma", "prev")

    def __init__(self, eng, fn, deps, dma):
        self.eng, self.fn, self.deps, self.dma, self.prev = eng, fn, deps, dma, None


class Prog:
    DMAQ = {"sp": 8, "pool": 4}

    def __init__(self, nc, sems):
        self.nc = nc
        self.sems = sems
        self.next_sem = 0
        self.dma_sems = {q: [self._alloc() for _ in range(k)] for q, k in self.DMAQ.items()}
        self.dma_cnt = {q: [0] * k for q, k in self.DMAQ.items()}
        self.dma_last = {q: [None] * k for q, k in self.DMAQ.items()}
        self.dma_n = {q: 0 for q in self.DMAQ}
        self._reset()

    def _alloc(self):
        i = self.next_sem
        self.next_sem += 1
        assert i < len(self.sems), "out of semaphores"
        return i

    def _reset(self):
        self.ops = []
        self.lastw = {}
        self.readers = defaultdict(list)
        self.fence_deps = set()
        self.fenced = set()

    def fence(self):
        self.fence_deps = set(range(len(self.ops)))
        self.fenced = set()

    def op(self, eng, fn, r=(), w=(), dma=False):
        idx = len(self.ops)
        deps = set()
        for k in r:
            if k in self.lastw:
                deps.add(self.lastw[k])
        for k in w:
            if k in self.lastw:
                deps.add(self.lastw[k])
            deps.update(self.readers.get(k, ()))
        for k in w:
            self.lastw[k] = idx
            self.readers[k] = []
        for k in r:
            self.readers[k].append(idx)
        if self.fence_deps and eng not in self.fenced:
            deps |= self.fence_deps
            self.fenced.add(eng)
        deps.discard(idx)
        self.ops.append(_Op(eng, fn, deps, dma))
        return idx

    def pe(self, fn, r=(), w=()):
        return self.op("pe", fn, r, w)

    def act(self, fn, r=(), w=()):
        return self.op("act", fn, r, w)

    def dve(self, fn, r=(), w=()):
        return self.op("dve", fn, r, w)

    def pool(self, fn, r=(), w=()):
        return self.op("pool", fn, r, w)

    def dma(self, q, out, in_, r=(), w=(), **kw):
        return self.op(q, lambda e, out=out, in_=in_, kw=kw: e.dma_start(out=out, in_=in_, **kw), r, w, dma=True)

    def flush(self):
        nc = self.nc
        ops = self.ops
        n = len(ops)
        signal = [False] * n

        def skip(p, c):
            return p.eng == "pe" and c.eng == "pe" and not p.dma and not c.dma

        for o in ops:
            for d in o.deps:
                if not skip(ops[d], o):
                    signal[d] = True
        csem = {}
        ccnt = {}
        ticket = [None] * n
        per_eng = {e: [] for e in ("pe", "act", "dve", "pool", "sp")}
        for i, o in enumerate(ops):
            per_eng[o.eng].append(i)
            if o.dma:
                q = o.eng
                K = len(self.dma_sems[q])
                j = self.dma_n[q] % K
                self.dma_n[q] += 1
                o.prev = self.dma_last[q][j]
                self.dma_cnt[q][j] += 1
                ticket[i] = (self.dma_sems[q][j], 16 * self.dma_cnt[q][j])
                self.dma_last[q][j] = ticket[i]
            elif signal[i]:
                e = o.eng
                if e not in csem or ccnt[e] >= 30000:
                    csem[e] = self._alloc()
                    ccnt[e] = 0
                ccnt[e] += 1
                ticket[i] = (csem[e], ccnt[e])
        sems = self.sems

        def emit(engname, eng):
            waited = {}
            for i in per_eng[engname]:
                o = ops[i]
                need = {}
                for d in o.deps:
                    if skip(ops[d], o):
                        continue
                    s, v = ticket[d]
                    if need.get(s, 0) < v:
                        need[s] = v
                if o.prev is not None:
                    s, v = o.prev
                    if need.get(s, 0) < v:
                        need[s] = v
                for s, v in need.items():
                    if waited.get(s, 0) < v:
                        eng.wait_ge(sems[s], v)
                        waited[s] = v
                inst = o.fn(eng)
                if ticket[i] is not None:
                    inst.then_inc(sems[ticket[i][0]], 16 if o.dma else 1)
            if engname in self.dma_last:
                for t in self.dma_last[engname]:
                    if t is not None and waited.get(t[0], 0) < t[1]:
                        eng.wait_ge(sems[t[0]], t[1])

        with nc.Block() as blk:
            if per_eng["pe"]:
                @blk.tensor
                def _(e):
                    emit("pe", e)
            if per_eng["act"]:
                @blk.scalar
                def _(e):
                    emit("act", e)
            if per_eng["dve"]:
                @blk.vector
                def _(e):
                    emit("dve", e)
            if per_eng["pool"]:
                @blk.gpsimd
                def _(e):
                    emit("pool", e)
            if per_eng["sp"]:
                @blk.sync
                def _(e):
                    emit("sp", e)
        self._reset()


class Rot:
    def __init__(self, alloc, name, n, shape, dt):
        self.t = [alloc(f"{name}{i}", shape, dt) for i in range(n)]
        self.name = name
        self.i = -1

    def next(self):
        self.i += 1
        j = self.i % len(self.t)
        return self.t[j], (self.name, j)


USE_POOL_POW = False


def rstd_pool(P, out_ap, in_ap, n, mh_ap, r, w):
    if not USE_POOL_POW:
        P.act(lambda e: e.activation(out=out_ap, in_=in_ap, func=AF.Sqrt, scale=1.0 / n, bias=EPS), r=r, w=w)
        P.dve(lambda e: e.reciprocal(out=out_ap, in_=out_ap), r=w, w=w)
        return
    P.pool(lambda e: e.tensor_scalar(out=out_ap, in0=in_ap, scalar1=1.0 / n, scalar2=EPS, op0=ALU.mult, op1=ALU.add),
           r=r, w=w)
    P.pool(lambda e: e.tensor_tensor(out=out_ap, in0=out_ap, in1=mh_ap, op=ALU.pow), r=w, w=w)


def bc(ap2d, shape, axis):
    return ap2d.unsqueeze(axis).to_broadcast(list(shape))


def emit_norm_T(P, x_ap, x_key, nw, hn, hn_key, ss, rs, col, epsb, ident, pT, pT_key, hT_view, hT_key,
                evac="act", n=D):
    nk = n // 128
    P.act(lambda e: e.activation(out=hn[:, 0:n], in_=x_ap, func=AF.Square, accum_out=ss[:, col:col + 1]),
          r=[x_key], w=[hn_key, ("ss", col)])
    P.act(lambda e: e.activation(out=rs[:, col:col + 1], in_=ss[:, col:col + 1], func=AF.Sqrt,
                                 scale=1.0 / n, bias=epsb[:, 0:1]),
          r=[("ss", col)], w=[("rs", col)])
    P.dve(lambda e: e.reciprocal(out=rs[:, col:col + 1], in_=rs[:, col:col + 1]),
          r=[("rs", col)], w=[("rs", col)])
    P.dve(lambda e: e.scalar_tensor_tensor(out=hn[:, 0:n], in0=x_ap, scalar=rs[:, col:col + 1], in1=nw,
                                           op0=ALU.mult, op1=ALU.mult),
          r=[x_key, ("rs", col)], w=[hn_key])

    def tr(e):
        inst = None
        for k in range(nk):
            inst = e.transpose(pT[:, k * 128:(k + 1) * 128], hn[:, k * 128:(k + 1) * 128], ident[:])
        return inst

    P.pe(tr, r=[hn_key], w=[pT_key])
    src = pT[:, 0:n].rearrange("p (k t) -> p k t", k=nk)
    if evac == "act":
        P.act(lambda e: e.copy(out=hT_view, in_=src), r=[pT_key], w=[hT_key])
    else:
        P.dve(lambda e: e.tensor_copy(out=hT_view, in_=src), r=[pT_key], w=[hT_key])


def load_consts(P, A, cins, need_cf=False):
    ident = A("ident", [128, 128], BF16)
    cf = A("cf32", [128, 512], F32) if need_cf else None
    epsb = A("mhalf", [128, 16], F32)
    P.dma("sp", ident[:], cins["c_ident"], w=["ident"])
    if need_cf:
        P.dma("sp", cf[:], cins["c_f32"], w=["cf"])
    P.dve(lambda e: e.memset(epsb[:], -0.5), w=["epsb"])
    return ident, cf, epsb


def phase_ffn(P, nc, name, src, dst, norm_w, wg, wu, wd, cins, final_w=None):
    with ExitStack() as st:
        def A(nm, shape, dt):
            return st.enter_context(nc.sbuf_tensor(f"{name}_{nm}", shape, dt))

        def PS(nm, shape, dt):
            return st.enter_context(nc.psum_tensor(f"{name}_{nm}", shape, dt))

        wgt = A("wg", [128, 8, FF], BF16)
        wut = A("wu", [128, 8, FF], BF16)
        wdt = A("wd", [128, NHC, D], BF16)
        xt = [A(f"xt{b}", [128, 4, D], F32) for b in range(2)]
        hT = A("hT", [128, 8, T], BF16)
        actb = A("act", [128, NHC, T], BF16)
        hn = A("hn", [128, D], BF16)
        sg = [A(f"sg{b}", [128, T], BF16) for b in range(2)]
        nw = A("nw", [128, D], F32)
        fw = A("fw", [128, D], F32) if final_w is not None else None
        ncol = NT * 4 * (2 if final_w is not None else 1)
        ss = A("ss", [128, ncol], F32)
        rs = A("rs", [128, ncol], F32)
        ident, cf, epsb = load_consts(P, A, cins)
        P.dve(lambda e: e.memset(epsb[:], EPS), r=["epsb"], w=["epsb"])
        pT = PS("pT", [128, D], BF16)
        pG = [PS(f"pG{b}", [128, T], F32) for b in range(2)]
        pU = [PS(f"pU{b}", [128, T], F32) for b in range(2)]
        pD = [PS(f"pD{b}", [128, T], F32) for b in range(2)]

        P.dve(lambda e: e.memset(ss[:], 0.0), w=[("ss", c) for c in range(ncol)])
        P.dma("sp", nw[:], norm_w.partition_broadcast(128), w=["nw"])
        if fw is not None:
            P.dma("sp", fw[:], final_w.partition_broadcast(128), w=["fw"])
        P.fence()
        for (wt, wsrc, key) in ((wgt, wg, "wg"), (wut, wu, "wu")):
            v = wsrc.rearrange("(k p) n -> p k n", p=128)
            for q in range(4):
                c0, c1 = q * 704, (q + 1) * 704
                P.dma("pool", wt[:, :, c0:c1], v[:, :, c0:c1], w=[(key, q)])
        vd = wd.rearrange("(k p) n -> p k n", p=128)
        for q in range(2):
            P.dma("pool", wdt[:, q * 11:(q + 1) * 11, :], vd[:, q * 11:(q + 1) * 11, :], w=[("wd", q)])

        def xview(ap, i):
            return ap[i * T:(i + 1) * T, :].rearrange("(j p) d -> p j d", p=128)

        def load(i):
            b = i % 2
            P.dma("sp", xt[b][:], xview(src, i), w=[("xt", b, j) for j in range(4)])

        def norm_sub(i, j):
            b = i % 2
            col = i * 4 + j
            emit_norm_T(P, xt[b][:, j, :], ("xt", b, j), nw[:], hn, "hn", ss, rs, col, epsb, ident, pT, "pT",
                        hT[:, :, j * 128:(j + 1) * 128], ("hT", j))

        load(0)
        for j in range(4):
            norm_sub(0, j)
        for i in range(NT):
            b = i % 2
            if i + 1 < NT:
                load(i + 1)
            for hc in range(NHC):
                q = hc * 128 // 704
                q2 = (hc * 128 + 127) // 704
                g, u = pG[hc % 2], pU[hc % 2]

                def mm_g(e, hc=hc, g=g):
                    inst = None
                    for k in range(8):
                        inst = e.matmul(g[:], lhsT=wgt[:, k, hc * 128:(hc + 1) * 128], rhs=hT[:, k, :],
                                        start=(k == 0), stop=(k == 7))
                    return inst

                def mm_u(e, hc=hc, u=u):
                    inst = None
                    for k in range(8):
                        inst = e.matmul(u[:], lhsT=wut[:, k, hc * 128:(hc + 1) * 128], rhs=hT[:, k, :],
                                        start=(k == 0), stop=(k == 7))
                    return inst

                hkeys = [("hT", j) for j in range(4)]
                P.pe(mm_g, r=hkeys + [("wg", q), ("wg", q2)], w=[("pG", hc % 2)])
                P.pe(mm_u, r=hkeys + [("wu", q), ("wu", q2)], w=[("pU", hc % 2)])
                sgb = sg[hc % 2]
                P.act(lambda e, g=g, sgb=sgb: e.activation(out=sgb[:], in_=g[:], func=AF.Silu),
                      r=[("pG", hc % 2)], w=[("sg", hc % 2)])
                P.dve(lambda e, u=u, sgb=sgb, hc=hc: e.tensor_tensor(out=actb[:, hc, :], in0=u[:], in1=sgb[:],
                                                                     op=ALU.mult),
                      r=[("pU", hc % 2), ("sg", hc % 2)], w=[("act", hc)])
            nd = 0
            for j in range(4):
                for half in range(2):
                    d_ = pD[nd % 2]
                    nd += 1

                    def mm_d(e, j=j, half=half, d_=d_):
                        inst = None
                        for hc in range(NHC):
                            inst = e.matmul(d_[:], lhsT=actb[:, hc, j * 128:(j + 1) * 128],
                                            rhs=wdt[:, hc, half * 512:(half + 1) * 512],
                                            start=(hc == 0), stop=(hc == NHC - 1))
                        return inst

                    P.pe(mm_d, r=[("act", hc) for hc in range(NHC)] + [("wd", 0), ("wd", 1)],
                         w=[("pD", (nd - 1) % 2)])
                    xs = xt[b][:, j, half * 512:(half + 1) * 512]
                    P.dve(lambda e, d_=d_, xs=xs: e.scalar_tensor_tensor(out=xs, in0=d_[:], scalar=0.5, in1=xs,
                                                                         op0=ALU.mult, op1=ALU.add),
                          r=[("pD", (nd - 1) % 2), ("xt", b, j)], w=[("xt", b, j)])
                if final_w is not None:
                    col = NT * 4 + i * 4 + j
                    xj = xt[b][:, j, :]
                    P.act(lambda e, xj=xj, col=col: e.activation(out=hn[:], in_=xj, func=AF.Square,
                                                                 accum_out=ss[:, col:col + 1]),
                          r=[("xt", b, j)], w=["hn", ("ss", col)])
                    P.act(lambda e, col=col: e.activation(out=rs[:, col:col + 1], in_=ss[:, col:col + 1],
                                                          func=AF.Sqrt, scale=1.0 / D, bias=epsb[:, 0:1]),
                          r=[("ss", col)], w=[("rs", col)])
                    P.dve(lambda e, col=col: e.reciprocal(out=rs[:, col:col + 1], in_=rs[:, col:col + 1]),
                          r=[("rs", col)], w=[("rs", col)])
                    P.dve(lambda e, xj=xj, col=col: e.scalar_tensor_tensor(out=xj, in0=xj, scalar=rs[:, col:col + 1],
                                                                           in1=fw[:], op0=ALU.mult, op1=ALU.mult),
                          r=[("xt", b, j), ("rs", col), "fw"], w=[("xt", b, j)])
                if i + 1 < NT and j >= 1:
                    for jj in ((0, 1) if j == 1 else (2,) if j == 2 else (3,)):
                        norm_sub(i + 1, jj)
            P.dma("sp", xview(dst, i), xt[b][:], r=[("xt", b, j) for j in range(4)])
        P.flush()


def phase_inproj(P, nc, x1s, pos, w, scr, cins):
    name = "ip"
    with ExitStack() as st:
        def A(nm, shape, dt):
            return st.enter_context(nc.sbuf_tensor(f"{name}_{nm}", shape, dt))

        def PS(nm, shape, dt):
            return st.enter_context(nc.psum_tensor(f"{name}_{nm}", shape, dt))

        win = A("win", [128, 8, IN_DIM], BF16)
        wuq = A("wuq", [128, 2, 768], BF16)
        wuk = A("wuk", [128, 512], BF16)
        wuv = A("wuv", [128, 512], BF16)
        xt = [A(f"xt{b}", [128, 4, D], F32) for b in range(2)]
        hT = A("hT", [128, 8, T], BF16)
        hn = A("hn", [128, D], BF16)
        nw = A("nw", [128, D], F32)
        qaw = A("qaw", [128, 384], F32)
        qkw = A("qkw", [128, 2, 96], F32)
        ss = A("ss", [128, NT * 4], F32)
        rs = A("rs", [128, NT * 4], F32)
        ss2 = A("ss2", [128, NT * 4, 2], F32)
        rs2 = A("rs2", [128, NT * 4, 2], F32)
        ss16 = A("ss16", [128, 4, 16], F32)
        rs16 = A("rs16", [128, 4, 16], F32)
        stage = Rot(A, "stg", 2, [128, 4, T], F32)
        cn = Rot(A, "cn", 2, [128, 384], BF16)
        cnT = Rot(A, "cnT", 2, [128, 3, 128], BF16)
        kpe = Rot(A, "kpe", 2, [128, 32], F32)
        gst = A("gst", [128, 4, 16], F32)
        vst = Rot(A, "vst", 2, [128, 512], BF16)
        qkf = Rot(A, "qkf", 2, [128, 16, 96], F32)
        sq = A("sq", [128, 16, 96], F32)
        rtmp = Rot(A, "rtmp", 1, [128, 4, 16, 16], F32)
        qkb = Rot(A, "qkb", 2, [128, 16, 96], BF16)
        qkTs = Rot(A, "qkTs", 1, [128, 16, T], BF16)
        posi = A("posi", [128, NCH], I32)
        posf = A("posf", [128, NCH], F32)
        inv = A("inv", [128, 16], F32)
        ang = qkf.t[0][:].rearrange("p h d -> p (h d)")[:, 0:NCH * 16].rearrange("p (c f) -> p c f", f=16)
        kf = qkf.t[1][:].rearrange("p h d -> p (h d)")[:, 0:NCH * 16].rearrange("p (c f) -> p c f", f=16)
        ki_t = A("ki", [128, NCH, 16], I32)
        ki = ki_t[:]
        cosT = A("cos", [128, NCH, 16], F32)
        sinT = A("sin", [128, NCH, 16], F32)
        hpi = A("hpi", [128, 1], F32)
        ident, cf, _mh = load_consts(P, A, cins)
        epsb = A("epsv", [128, 1], F32)
        P.dve(lambda e: e.memset(epsb[:], EPS), w=["epsv"])
        pT = PS("pT", [128, D], BF16)
        pF = [PS(f"pF{b}", [128, T], F32) for b in range(2)]
        pA = PS("pA", [128, T], F32)
        pQ = PS("pQ", [128, 1024], F32)
        pK, pV = pF[0], pF[1]
        pX = PS("pX", [128, 8 * 128], BF16)

        P.dve(lambda e: e.memset(ss[:], 0.0), w=[("ss", c) for c in range(NT * 4)])
        P.dve(lambda e: e.memset(ss2[:], 0.0), w=[("ss2", c) for c in range(NT * 4)])
        P.dve(lambda e: e.memset(hpi[:], math.pi / 2), w=["hpi"])
        P.dma("sp", nw[:], w["mix_norm_w"].partition_broadcast(128), w=["nw"])
        P.dma("sp", qaw[:, 0:256], w["q_a_norm_w"].partition_broadcast(128), w=["qaw"])
        P.dma("sp", qaw[:, 256:384], w["kv_a_norm_w"].partition_broadcast(128), w=["qaw2"])
        P.dma("sp", qkw[:, 0, :], w["q_norm_w"].partition_broadcast(128), w=["qkw0"])
        P.dma("sp", qkw[:, 1, :], w["k_norm_w"].partition_broadcast(128), w=["qkw1"])
        P.dma("sp", inv[:], cins["c_inv"].partition_broadcast(128), w=["inv"])
        P.dma("sp", posi[:], pos.rearrange("(c p) -> p c", p=128), w=["posi"], allow_slow_non_contiguous=True)
        wv = w["w_in"].rearrange("(k p) n -> p k n", p=128)
        P.dma("pool", win[:, :, 0:416], wv[:, :, 0:416], w=["winA"])
        P.dma("pool", win[:, :, 416:432], wv[:, :, 1440:1456], w=["winA2"])
        P.dma("pool", win[:, :, 432:1456], wv[:, :, 416:1440], w=[("winF", 0)])
        P.dma("pool", win[:, :, 1456:2480], wv[:, :, 1456:2480], w=[("winF", 1)])
        P.dma("pool", win[:, :, 2480:3504], wv[:, :, 2480:3504], w=[("winF", 2)])
        P.dma("pool", wuq[:], w["w_uq"].rearrange("(k p) n -> p k n", p=128), w=["wuq"])
        P.dma("pool", wuk[:], w["w_uk"], w=["wuk"])
        P.dma("pool", wuv[:], w["w_uv"], w=["wuv"])

        P.dve(lambda e: e.tensor_copy(out=posf[:], in_=posi[:]), r=["posi"], w=["posf"])
        P.dve(lambda e: e.tensor_tensor(out=ang, in0=bc(posf[:], [128, NCH, 16], 2),
                                        in1=bc(inv[:], [128, NCH, 16], 1), op=ALU.mult),
              r=["posf", "inv"], w=["ang"])
        P.dve(lambda e: e.tensor_scalar(out=kf, in0=ang, scalar1=1.0 / (2 * math.pi), scalar2=None,
                                        op0=ALU.mult), r=["ang"], w=["kf"])
        P.dve(lambda e: e.tensor_copy(out=ki, in_=kf), r=["kf"], w=["ki"])
        P.dve(lambda e: e.tensor_copy(out=kf, in_=ki), r=["ki"], w=["kf"])
        C1 = 6.28125
        C2 = 2 * math.pi - C1
        P.dve(lambda e: e.scalar_tensor_tensor(out=ang, in0=kf, scalar=-C1, in1=ang, op0=ALU.mult,
                                               op1=ALU.add), r=["kf", "ang"], w=["ang"])
        P.dve(lambda e: e.scalar_tensor_tensor(out=ang, in0=kf, scalar=-C2, in1=ang, op0=ALU.mult,
                                               op1=ALU.add), r=["kf", "ang"], w=["ang"])
        P.dve(lambda e: e.tensor_scalar(out=ang, in0=ang, scalar1=-3.1415925, scalar2=3.1415925,
                                        op0=ALU.max, op1=ALU.min), r=["ang"], w=["ang"])
        P.act(lambda e: e.activation(out=sinT[:], in_=ang, func=AF.Sin), r=["ang"], w=["sin"])
        P.dve(lambda e: e.scalar_tensor_tensor(out=kf, in0=ang, scalar=-1.0, in1=ang, op0=ALU.mult,
                                               op1=ALU.max), r=["ang"], w=["kf"])
        P.act(lambda e: e.activation(out=cosT[:], in_=kf, func=AF.Sin, scale=-1.0, bias=hpi[:]),
              r=["kf", "hpi"], w=["cos"])

        P.fence()

        def xview(ap, i):
            return ap[i * T:(i + 1) * T, :].rearrange("(j p) d -> p j d", p=128)

        def load(i):
            b = i % 2
            P.dma("sp", xt[b][:], xview(x1s, i), w=[("xt", b, j) for j in range(4)])

        def norm_sub(i, j):
            b = i % 2
            col = i * 4 + j
            emit_norm_T(P, xt[b][:, j, :], ("xt", b, j), nw[:], hn, "hn", ss, rs, col, epsb, ident, pT, "pT",
                        hT[:, :, j * 128:(j + 1) * 128], ("hT", j), evac="dve")

        fm_groups = [
            (scr["minT"], 432, None), (scr["sopT"], 944, AF.Sigmoid),
            (scr["sgT"][0:512, :], 1456, AF.Sigmoid), (scr["sgT"][512:1024, :], 1968, AF.Sigmoid),
            (scr["sgT"][1024:1536, :], 2480, AF.Sigmoid), (scr["sgT"][1536:2048, :], 2992, AF.Sigmoid),
        ]
        hkeys = [("hT", j) for j in range(4)]
        wkeys = ["winA", "winA2"] + [("winF", q) for q in range(3)]
        nf = 0
        load(0)
        for j in range(4):
            norm_sub(0, j)
        for i in range(NT):
            b = i % 2
            t0 = i * T
            if i + 1 < NT:
                load(i + 1)
            def fm_gen(i=i, t0=t0):
              nonlocal nf
              for (dram, col0, func) in fm_groups:
                stg, skey = stage.next()
                for cc in range(4):
                    pf = pF[nf % 2]
                    pkey = ("pF", nf % 2)
                    nf += 1

                    def mm_f(e, pf=pf, c0=col0 + cc * 128):
                        inst = None
                        for k in range(8):
                            inst = e.matmul(pf[:], lhsT=win[:, k, c0:c0 + 128], rhs=hT[:, k, :],
                                            start=(k == 0), stop=(k == 7))
                        return inst

                    P.pe(mm_f, r=hkeys + wkeys, w=[pkey])
                    if func is None:
                        P.dve(lambda e, pf=pf, stg=stg, cc=cc: e.tensor_copy(out=stg[:, cc, :], in_=pf[:]),
                              r=[pkey], w=[skey + (cc,)])
                    else:
                        P.act(lambda e, pf=pf, stg=stg, cc=cc, func=func: e.activation(out=stg[:, cc, :], in_=pf[:],
                                                                                      func=func),
                              r=[pkey], w=[skey + (cc,)])
                    if cc == 3:
                        P.dma("sp", dram[:, t0:t0 + T].rearrange("(c p) t -> p c t", p=128), stg[:],
                              r=[skey + (q_,) for q_ in range(4)])
                    yield
            fmg = fm_gen()
            for _ in range(4):
                next(fmg, None)
            qs, qskey = qkTs.next()
            def sub_body(j, i=i, t0=t0, qs=qs, qskey=qskey):
                c = i * 4 + j
                tok0 = t0 + j * 128

                def mm_a(e, j=j):
                    inst = None
                    for k in range(8):
                        inst = e.matmul(pA[:, 0:432], lhsT=hT[:, k, j * 128:(j + 1) * 128], rhs=win[:, k, 0:432],
                                        start=(k == 0), stop=(k == 7))
                    return inst

                P.pe(mm_a, r=hkeys + wkeys, w=["pA"])
                cnb, cnkey = cn.next()
                kp, kpkey = kpe.next()
                P.act(lambda e, c=c: e.activation(out=hn[:, 0:256], in_=pA[:, 0:256], func=AF.Square,
                                                  accum_out=ss2[:, c, 0:1]), r=["pA"], w=["hn", ("ss2", c)])
                P.act(lambda e, c=c: e.activation(out=hn[:, 256:384], in_=pA[:, 256:384], func=AF.Square,
                                                  accum_out=ss2[:, c, 1:2]), r=["pA"], w=["hn", ("ss2", c)])
                P.act(lambda e, c=c: e.activation(out=rs2[:, c, 0:1], in_=ss2[:, c, 0:1], func=AF.Sqrt,
                                                  scale=1.0 / 256, bias=epsb[:]), r=[("ss2", c)], w=[("rs2", c)])
                P.act(lambda e, c=c: e.activation(out=rs2[:, c, 1:2], in_=ss2[:, c, 1:2], func=AF.Sqrt,
                                                  scale=1.0 / 128, bias=epsb[:]), r=[("rs2", c)], w=[("rs2", c)])
                P.dve(lambda e, c=c: e.reciprocal(out=rs2[:, c, :], in_=rs2[:, c, :]), r=[("rs2", c)],
                      w=[("rs2", c)])
                P.dve(lambda e, c=c, cnb=cnb: e.scalar_tensor_tensor(out=cnb[:, 0:256], in0=pA[:, 0:256],
                                                                     scalar=rs2[:, c, 0:1], in1=qaw[:, 0:256],
                                                                     op0=ALU.mult, op1=ALU.mult),
                      r=["pA", ("rs2", c), "qaw"], w=[cnkey + (0,)])
                P.dve(lambda e, c=c, cnb=cnb: e.scalar_tensor_tensor(out=cnb[:, 256:384], in0=pA[:, 256:384],
                                                                     scalar=rs2[:, c, 1:2], in1=qaw[:, 256:384],
                                                                     op0=ALU.mult, op1=ALU.mult),
                      r=["pA", ("rs2", c), "qaw2"], w=[cnkey + (1,)])
                P.dve(lambda e, kp=kp: e.tensor_copy(out=kp[:], in_=pA[:, 384:416]), r=["pA"], w=[kpkey])
                P.dve(lambda e, j=j: e.tensor_copy(out=gst[:, j, :], in_=pA[:, 416:432]), r=["pA"], w=[("gst", j)])
                yield
                cT, cTkey = cnT.next()

                def tr3(e, cnb=cnb):
                    inst = None
                    for k in range(3):
                        inst = e.transpose(pT[:, k * 128:(k + 1) * 128], cnb[:, k * 128:(k + 1) * 128], ident[:])
                    return inst

                P.pe(tr3, r=[cnkey + (0,), cnkey + (1,)], w=["pT"])
                P.dve(lambda e, cT=cT: e.tensor_copy(out=cT[:], in_=pT[:, 0:384].rearrange("p (k t) -> p k t", k=3)),
                      r=["pT"], w=[cTkey])

                yield
                def mm_q(e, cT=cT):
                    e.matmul(pQ[:, 0:512], lhsT=cT[:, 0, :], rhs=wuq[:, 0, 0:512], start=True, stop=False)
                    e.matmul(pQ[:, 0:512], lhsT=cT[:, 1, :], rhs=wuq[:, 1, 0:512], start=False, stop=True)
                    e.matmul(pQ[:, 512:768], lhsT=cT[:, 0, :], rhs=wuq[:, 0, 512:768], start=True, stop=False)
                    return e.matmul(pQ[:, 512:768], lhsT=cT[:, 1, :], rhs=wuq[:, 1, 512:768], start=False, stop=True)

                P.pe(mm_q, r=[cTkey, "wuq"], w=["pQ"])
                P.pe(lambda e, cT=cT: e.matmul(pK[:], lhsT=cT[:, 2, :], rhs=wuk[:], start=True, stop=True),
                     r=[cTkey, "wuk"], w=[("pF", 0)])
                P.pe(lambda e, cT=cT: e.matmul(pV[:], lhsT=cT[:, 2, :], rhs=wuv[:], start=True, stop=True),
                     r=[cTkey, "wuv"], w=[("pF", 1)])
                vs, vskey = vst.next()
                P.act(lambda e, vs=vs: e.copy(out=vs[:], in_=pV[:]), r=[("pF", 1)], w=[vskey])
                P.dma("sp", scr["vS"][tok0:tok0 + 128, :], vs[:], r=[vskey])
                qf, qfkey = qkf.next()
                P.act(lambda e, qf=qf: e.copy(out=qf[:, 0:8, :].rearrange("p h d -> p (h d)"), in_=pQ[:, 0:768]),
                      r=["pQ"], w=[qfkey + ("q",)])
                P.dve(lambda e, qf=qf: e.tensor_copy(out=qf[:, 8:16, 0:64],
                                                     in_=pK[:].rearrange("p (h d) -> p h d", h=8)),
                      r=[("pF", 0)], w=[qfkey + ("k",)])
                P.pool(lambda e, qf=qf, kp=kp: e.tensor_copy(out=qf[:, 8:16, 64:96], in_=bc(kp[:], [128, 8, 32], 1)),
                       r=[kpkey], w=[qfkey + ("kp",)])
                qkeys = [qfkey + (s_,) for s_ in ("q", "k", "kp")]
                yield
                P.pool(lambda e, qf=qf: e.tensor_tensor(out=sq[:], in0=qf[:], in1=qf[:], op=ALU.mult),
                       r=qkeys, w=["sq"])
                P.dve(lambda e, j=j: e.tensor_reduce(out=ss16[:, j, :], in_=sq[:], axis=AX.X, op=ALU.add),
                      r=["sq"], w=[("ss16", j)])
                yield
                P.act(lambda e, j=j: e.activation(out=rs16[:, j, :], in_=ss16[:, j, :], func=AF.Sqrt,
                                                  scale=1.0 / 96, bias=epsb[:]), r=[("ss16", j)], w=[("rs16", j)])
                P.dve(lambda e, j=j: e.reciprocal(out=rs16[:, j, :], in_=rs16[:, j, :]), r=[("rs16", j)],
                      w=[("rs16", j)])
                P.dve(lambda e, qf=qf, j=j: e.tensor_tensor(out=qf[:], in0=qf[:],
                                                            in1=bc(rs16[:, j, :], [128, 16, 96], 2), op=ALU.mult),
                      r=qkeys + [("rs16", j)], w=qkeys)
                yield
                qb, qbkey = qkb.next()
                for hh in range(2):
                    eng = P.pool if hh == 0 else P.dve
                    eng(lambda e, qf=qf, qb=qb, hh=hh: e.tensor_tensor(
                        out=qb[:, hh * 8:(hh + 1) * 8, 0:64], in0=qf[:, hh * 8:(hh + 1) * 8, 0:64],
                        in1=bc(qkw[:, hh, 0:64], [128, 8, 64], 1), op=ALU.mult),
                        r=qkeys + ["qkw0", "qkw1"], w=[qbkey + ("n", hh)])
                    eng(lambda e, qf=qf, hh=hh: e.tensor_tensor(
                        out=qf[:, hh * 8:(hh + 1) * 8, 64:96], in0=qf[:, hh * 8:(hh + 1) * 8, 64:96],
                        in1=bc(qkw[:, hh, 64:96], [128, 8, 32], 1), op=ALU.mult),
                        r=qkeys + ["qkw0", "qkw1"], w=qkeys)
                yield
                rt, rtkey = rtmp.next()
                cb_ = bc(cosT[:, c, :], [128, 16, 16], 1)
                sb_ = bc(sinT[:, c, :], [128, 16, 16], 1)
                x1_ = lambda qf: qf[:, :, 64:80]
                x2_ = lambda qf: qf[:, :, 80:96]
                P.pool(lambda e, qf=qf, rt=rt, cb_=cb_: e.tensor_tensor(out=rt[:, 0], in0=x1_(qf), in1=cb_, op=ALU.mult),
                       r=qkeys + ["cos"], w=[rtkey + (0,)])
                P.pool(lambda e, qf=qf, rt=rt, sb_=sb_: e.tensor_tensor(out=rt[:, 1], in0=x2_(qf), in1=sb_, op=ALU.mult),
                       r=qkeys + ["sin"], w=[rtkey + (1,)])
                P.dve(lambda e, qf=qf, rt=rt, cb_=cb_: e.tensor_tensor(out=rt[:, 2], in0=x2_(qf), in1=cb_, op=ALU.mult),
                      r=qkeys + ["cos"], w=[rtkey + (2,)])
                P.dve(lambda e, qf=qf, rt=rt, sb_=sb_: e.tensor_tensor(out=rt[:, 3], in0=x1_(qf), in1=sb_, op=ALU.mult),
                      r=qkeys + ["sin"], w=[rtkey + (3,)])
                P.pool(lambda e, qb=qb, rt=rt: e.tensor_tensor(out=qb[:, :, 64:80], in0=rt[:, 0], in1=rt[:, 1],
                                                               op=ALU.subtract),
                       r=[rtkey + (0,), rtkey + (1,)], w=[qbkey + ("r1",)])
                P.dve(lambda e, qb=qb, rt=rt: e.tensor_tensor(out=qb[:, :, 80:96], in0=rt[:, 2], in1=rt[:, 3],
                                                              op=ALU.add),
                      r=[rtkey + (2,), rtkey + (3,)], w=[qbkey + ("r2",)])
                qbkeys = [qbkey + ("n", 0), qbkey + ("n", 1), qbkey + ("r1",), qbkey + ("r2",)]

                yield
                for h0 in range(2):
                    def tr8(e, qb=qb, h0=h0):
                        inst = None
                        for hh in range(8):
                            inst = e.transpose(pX[0:96, hh * 128:(hh + 1) * 128], qb[:, h0 * 8 + hh, :], ident[:])
                        return inst

                    P.pe(tr8, r=qbkeys, w=["pX"])
                    P.act(lambda e, qs=qs, j=j, h0=h0: e.copy(
                        out=qs[0:96, h0 * 8:(h0 + 1) * 8, j * 128:(j + 1) * 128],
                        in_=pX[0:96, :].rearrange("p (h t) -> p h t", h=8)), r=["pX"], w=[qskey + (j, h0)])
            for j0 in (0, 2):
                gl = [sub_body(j0), sub_body(j0 + 1)]
                while gl:
                    for g_ in list(gl):
                        try:
                            next(g_)
                        except StopIteration:
                            gl.remove(g_)
                        if j0 == 0:
                            next(fmg, None)
                            next(fmg, None)
                if j0 == 0:
                    for _ in fmg:
                        pass
                if i + 1 < NT:
                    norm_sub(i + 1, j0)
                    norm_sub(i + 1, j0 + 1)
            P.dma("sp", scr["gates"][t0:t0 + T, :].rearrange("(j p) g -> p j g", p=128), gst[:],
                  r=[("gst", j) for j in range(4)])
            P.dma("sp", scr["qkT"][:, :, t0:t0 + T].rearrange("h d t -> d h t"), qs[0:96, :, :],
                  r=[qskey + (j, h0) for j in range(4) for h0 in range(2)])
        P.flush()


def phase_attn(P, nc, scr, cins):
    name = "at"
    with ExitStack() as st:
        def A(nm, shape, dt):
            return st.enter_context(nc.sbuf_tensor(f"{name}_{nm}", shape, dt))

        def PS(nm, shape, dt):
            return st.enter_context(nc.psum_tensor(f"{name}_{nm}", shape, dt))

        kt = Rot(A, "kt", 2, [128, S], BF16)
        qt = Rot(A, "qt", 2, [128, S], BF16)
        va = Rot(A, "va", 2, [128, NCH, 65], BF16)
        pt = Rot(A, "pt", 3, [128, 2 * T], BF16)
        rden = Rot(A, "rden", 2, [128, T], F32)
        rbs = Rot(A, "rbs", 2, [64, T], F32)
        ast = Rot(A, "ast", 2, [64, T], BF16)
        ones = A("ones", [128, 64], F32)
        pS = [PS(f"pS{b}", [128, 2 * T], F32) for b in range(2)]
        pO = [PS(f"pO{b}", [128, T], F32) for b in range(2)]
        pR = PS("pR", [128, T], F32)
        P.dve(lambda e: e.memset(ones[:], 1.0), w=["ones"])
        for b in range(2):
            P.dve(lambda e, b=b: e.memset(va.t[b][:, :, 64:65], 1.0), w=[("va1", b)])
        ns = 0
        no = 0
        NP2 = NCH // 2
        tail2 = None
        def head_loads(h):
            ktb, ktkey = kt.next()
            qtb, qtkey = qt.next()
            vab, vakey = va.next()
            P.dma("sp", ktb[0:96, :], scr["qkT"][8 + h], w=[ktkey])
            P.dma("sp", qtb[0:96, :], scr["qkT"][h], w=[qtkey])
            for q4 in range(4):
                P.dma("sp", vab[:, q4 * 16:(q4 + 1) * 16, 0:64],
                      scr["vS"][q4 * 2048:(q4 + 1) * 2048, h * 64:(h + 1) * 64].rearrange("(c p) d -> p c d", p=128),
                      w=[vakey + (q4,)])
            vkeys = [vakey + (q4,) for q4 in range(4)] + [("va1", va.i % 2)]
            return ktb, ktkey, qtb, qtkey, vab, vkeys

        hl = {0: head_loads(0)}
        for h in range(8):
            ktb, ktkey, qtb, qtkey, vab, vkeys = hl.pop(h)
            if h + 1 < 8:
                hl[h + 1] = head_loads(h + 1)
            for g in range(NT):
                po = pO[no % 2]
                pokey = ("pO", no % 2)
                no += 1
                qsl = qtb[0:96, g * T:(g + 1) * T]
                pend = []

                def qk(kp):
                    nonlocal ns
                    ps = pS[ns % 2]
                    pskey = ("pS", ns % 2)
                    ns += 1

                    def mm(e, ps=ps, kp=kp, ktb=ktb, qsl=qsl):
                        e.matmul(ps[:, 0:T], lhsT=ktb[0:96, (2 * kp) * 128:(2 * kp + 1) * 128], rhs=qsl, start=True,
                                 stop=True)
                        return e.matmul(ps[:, T:2 * T], lhsT=ktb[0:96, (2 * kp + 1) * 128:(2 * kp + 2) * 128], rhs=qsl,
                                        start=True, stop=True)

                    P.pe(mm, r=[ktkey, qtkey], w=[pskey])
                    return ps, pskey

                pend.append(qk(0))
                for kp in range(NP2):
                    if kp + 1 < NP2:
                        pend.append(qk(kp + 1))
                    ps, pskey = pend.pop(0)
                    ptb, ptkey = pt.next()
                    P.act(lambda e, ps=ps, ptb=ptb: e.activation(out=ptb[:], in_=ps[:], func=AF.Exp, scale=ATT_SCALE),
                          r=[pskey], w=[ptkey])

                    def pv(e, ptb=ptb, kp=kp, po=po, vab=vab):
                        e.matmul(po[0:65, :], lhsT=vab[:, 2 * kp, :], rhs=ptb[:, 0:T], start=(kp == 0), stop=False)
                        return e.matmul(po[0:65, :], lhsT=vab[:, 2 * kp + 1, :], rhs=ptb[:, T:2 * T], start=False,
                                        stop=(kp == NP2 - 1))

                    P.pe(pv, r=[ptkey] + vkeys, w=[pokey])
                    if kp == 2 and tail2 is not None:
                        tail2()
                        tail2 = None
                rd, rdkey = rden.next()
                rb, rbkey = rbs.next()
                ab, abkey = ast.next()
                P.dve(lambda e, rd=rd, po=po: e.reciprocal(out=rd[64:65, :], in_=po[64:65, :]), r=[pokey], w=[rdkey])

                def tail(rd=rd, rdkey=rdkey, rb=rb, rbkey=rbkey, ab=ab, abkey=abkey, po=po, pokey=pokey, h=h, g=g):
                    P.pe(lambda e: e.matmul(pR[0:64, :], lhsT=ones[64:65, 0:64], rhs=rd[64:65, :], start=True,
                                            stop=True), r=[rdkey, "ones"], w=["pR"])
                    P.dve(lambda e: e.tensor_copy(out=rb[:], in_=pR[0:64, :]), r=["pR"], w=[rbkey])
                    P.dve(lambda e: e.tensor_tensor(out=ab[:], in0=po[0:64, :], in1=rb[:], op=ALU.mult),
                          r=[pokey, rbkey], w=[abkey])
                    P.dma("sp", scr["aT"][h * 64:(h + 1) * 64, g * T:(g + 1) * T], ab[:], r=[abkey])

                tail2 = tail
        tail2()
        P.flush()


def phase_mlstm(P, nc, w, scr, cins):
    name = "ml"
    with ExitStack() as st:
        def A(nm, shape, dt):
            return st.enter_context(nc.sbuf_tensor(f"{name}_{nm}", shape, dt))

        def PS(nm, shape, dt):
            return st.enter_context(nc.psum_tensor(f"{name}_{nm}", shape, dt))

        ident, cf, epsb = load_consts(P, A, cins, need_cf=True)
        identf = cf[:, 0:128]
        triLE = cf[:, 128:256]
        triGE = cf[:, 256:384]
        onesf = cf[:, 384:512]
        G = A("G", [128, NCH, 16], F32)
        bi = A("bi", [128, 8], F32)
        bf_ = A("bf", [128, 8], F32)
        GI = A("GI", [128, 2, NCH, 4], F32)
        GF = A("GF", [128, 2, NCH, 4], F32)
        t1 = A("t1", [128, 512], F32)
        t2 = A("t2", [128, 512], F32)
        LF = A("LF", [128, 512], F32)
        Bc = A("Bc", [128, 512], F32)
        CS = A("CS", [128, 512], F32)
        BL = A("BL", [128, 512], F32)
        MXB = A("MXB", [128, 512], F32)
        mxcol = A("mxcol", [128, 4], F32)
        dg = A("dg", [128, 512], F32)
        M_all = A("M_all", [128, 512], F32)
        DARG = A("DARG", [128, 512], F32)
        DEC = A("DEC", [128, 512], F32)
        A_all = A("A_all", [128, 512], F32)
        THR = A("THR", [128, 512], F32)
        mcur = A("mcur", [128, 8], F32)
        wq = A("wq", [128, 4, 128], BF16)
        wk = A("wk", [128, 4, 128], BF16)
        wvv = A("wv", [128, 4, 128], BF16)
        cw = A("cw", [128, 4, 5], F32)
        cbias = A("cb", [128, 4], F32)
        mnw = A("mnw", [128, 4], F32)
        msk = A("msk", [128, 4], F32)
        p0 = PS("p0", [128, 512], F32)
        p1 = PS("p1", [128, 512], F32)
        pS_ = PS("pS", [128, 512], F32)
        pKV = PS("pKV", [128, 1024], F32)
        pN = PS("pN", [128, 1024], F32)
        pH = PS("pH", [128, 512], F32)

        for q4 in range(4):
            P.dma("sp", G[:, q4 * 16:(q4 + 1) * 16, :],
                  scr["gates"][q4 * 2048:(q4 + 1) * 2048, :].rearrange("(c p) g -> p c g", p=128), w=[("G", q4)])
        gk = [("G", q4) for q4 in range(4)]
        P.dma("sp", bi[:], w["b_igate"].partition_broadcast(128), w=["bi"])
        P.dma("sp", bf_[:], w["b_fgate"].partition_broadcast(128), w=["bf"])
        for (wt, key) in ((wq, "w_mq"), (wk, "w_mk"), (wvv, "w_mv")):
            P.dma("pool", wt[:], w[key].rearrange("h d e -> d h e"), w=[key])
        for jj in range(5):
            P.dma("sp", cw[:, :, jj], w["conv_w"][jj].rearrange("(k p) -> p k", p=128), w=[("cw", jj)],
                  allow_slow_non_contiguous=True)
        P.dma("sp", cbias[:], w["conv_b"].rearrange("(k p) -> p k", p=128), w=["cb"], allow_slow_non_contiguous=True)
        P.dma("sp", mnw[:], w["m_norm_w"].rearrange("(k p) -> p k", p=128), w=["mnw"], allow_slow_non_contiguous=True)
        P.dma("sp", msk[:], w["m_skip"].rearrange("(k p) -> p k", p=128), w=["msk"], allow_slow_non_contiguous=True)

        dgw = A("dgw", [128, 4, 5, 128], BF16)
        cbr = A("cbr", [1, 512], F32)
        cbr2 = A("cbr2", [1, 512], F32)
        brow = A("brow", [1, 2, 512], BF16)
        onesb = A("onesb", [1, 128], BF16)
        P.dma("sp", cbr[:], w["conv_b"].rearrange("(o n) -> o n", o=1), w=["cbr"])
        P.dve(lambda e: e.memset(onesb[:], 1.0), w=["onesb"])
        P.fence()
        for hc in range(4):
            for jj in range(5):
                P.dve(lambda e, hc=hc, jj=jj: e.tensor_scalar(out=dgw[:, hc, jj, :], in0=identf,
                                                              scalar1=cw[:, hc, jj:jj + 1], scalar2=None,
                                                              op0=ALU.mult), w=[("dgw", hc, jj)])
        P.dve(lambda e: e.tensor_copy(out=brow[:, 0, :], in_=cbr[:]), w=["brow0"])
        P.dve(lambda e: e.tensor_copy(out=cbr2[:], in_=brow[:, 0, :]), r=["brow0"], w=["cbr2"])
        P.dve(lambda e: e.tensor_tensor(out=cbr2[:], in0=cbr[:], in1=cbr2[:], op=ALU.subtract), r=["cbr2"], w=["cbr2"])
        P.dve(lambda e: e.tensor_copy(out=brow[:, 1, :], in_=cbr2[:]), r=["cbr2"], w=["brow1"])
        for d in range(2):
            P.dve(lambda e, d=d: e.tensor_tensor(out=GI[:, d], in0=G[:, :, d * 4:d * 4 + 4],
                                                 in1=bc(bi[:, d * 4:d * 4 + 4], [128, NCH, 4], 1), op=ALU.add),
                  r=gk + ["bi"], w=[("GI", d)])
            P.dve(lambda e, d=d: e.tensor_tensor(out=GF[:, d], in0=G[:, :, 8 + d * 4:12 + d * 4],
                                                 in1=bc(bf_[:, d * 4:d * 4 + 4], [128, NCH, 4], 1), op=ALU.add),
                  r=gk + ["bf"], w=[("GF", d)])
        GFf = GF[:].rearrange("p d c h -> p (d c h)")
        GIf = GI[:].rearrange("p d c h -> p (d c h)")
        gfk = [("GF", 0), ("GF", 1)]
        gik = [("GI", 0), ("GI", 1)]
        P.dve(lambda e: e.scalar_tensor_tensor(out=t1[:], in0=GFf, scalar=-1.0, in1=GFf, op0=ALU.mult, op1=ALU.max),
              r=gfk, w=["t1"])
        P.act(lambda e: e.activation(out=t1[:], in_=t1[:], func=AF.Exp, scale=-1.0), r=["t1"], w=["t1"])
        P.act(lambda e: e.activation(out=t1[:], in_=t1[:], func=AF.Ln, bias=1.0), r=["t1"], w=["t1"])
        P.dve(lambda e: e.tensor_scalar(out=t2[:], in0=GFf, scalar1=0.0, scalar2=None, op0=ALU.min), r=gfk, w=["t2"])
        P.dve(lambda e: e.tensor_tensor(out=LF[:], in0=t2[:], in1=t1[:], op=ALU.subtract), r=["t1", "t2"], w=["LF"])
        P.pe(lambda e: e.matmul(p0[:, 0:256], lhsT=triLE, rhs=LF[:, 0:256], start=True, stop=True),
             r=["LF", "cf"], w=["p0a"])
        P.pe(lambda e: e.matmul(p0[:, 256:512], lhsT=triGE, rhs=LF[:, 256:512], start=True, stop=True),
             r=["LF", "cf"], w=["p0b"])
        P.pe(lambda e: e.matmul(p1[:], lhsT=onesf, rhs=LF[:], start=True, stop=True), r=["LF", "cf"], w=["p1"])
        P.dve(lambda e: e.tensor_copy(out=Bc[:], in_=p0[:]), r=["p0a", "p0b"], w=["Bc"])
        P.act(lambda e: e.copy(out=BL[:], in_=p1[:]), r=["p1"], w=["BL"])
        P.dve(lambda e: e.tensor_tensor(out=CS[:], in0=GIf, in1=Bc[:], op=ALU.subtract), r=gik + ["Bc"], w=["CS"])

        def trf(e):
            inst = None
            for k in range(4):
                inst = e.transpose(p0[:, k * 128:(k + 1) * 128], CS[:, k * 128:(k + 1) * 128], identf)
            return inst

        P.pe(trf, r=["CS", "cf", "Bc"], w=["p0a", "p0b"])
        P.dve(lambda e: e.tensor_reduce(out=mxcol[:], in_=p0[:].rearrange("p (k s) -> p k s", k=4), axis=AX.X,
                                        op=ALU.max), r=["p0a", "p0b"], w=["mxcol"])
        for k in range(4):
            P.dve(lambda e, k=k: e.tensor_scalar(out=dg[:, k * 128:(k + 1) * 128], in0=identf,
                                                 scalar1=mxcol[:, k:k + 1], scalar2=None, op0=ALU.mult),
                  r=["mxcol", "cf"], w=[("dg", k)])
        P.pe(lambda e: e.matmul(p1[:], lhsT=onesf, rhs=dg[:], start=True, stop=True),
             r=[("dg", k) for k in range(4)] + ["cf", "BL"], w=["p1"])
        P.dve(lambda e: e.tensor_copy(out=MXB[:], in_=p1[:]), r=["p1"], w=["MXB"])
        P.dve(lambda e: e.memset(mcur[:], 0.0), w=[("mcur", 0), ("mcur", 1)])
        for jstep in range(NCH):
            for d in range(2):
                c = jstep if d == 0 else NCH - 1 - jstep
                sl = slice(d * 256 + c * 4, d * 256 + c * 4 + 4)
                mc = mcur[:, d * 4:d * 4 + 4]
                eng = P.dve
                eng(lambda e, sl=sl, mc=mc: e.tensor_tensor(out=M_all[:, sl], in0=mc, in1=MXB[:, sl], op=ALU.max),
                    r=[("mcur", d), "MXB"], w=[("M", d, c)])
                eng(lambda e, sl=sl, mc=mc: e.tensor_tensor(out=DARG[:, sl], in0=mc, in1=M_all[:, sl],
                                                            op=ALU.subtract),
                    r=[("mcur", d), ("M", d, c)], w=[("DARG", d, c)])
                eng(lambda e, sl=sl, mc=mc: e.tensor_tensor(out=mc, in0=M_all[:, sl], in1=BL[:, sl], op=ALU.add),
                    r=[("M", d, c), "BL", ("DARG", d, c)], w=[("mcur", d)])
        mk = [("M", d, c) for d in range(2) for c in range(NCH)]
        dk = [("DARG", d, c) for d in range(2) for c in range(NCH)]
        P.act(lambda e: e.activation(out=DEC[:], in_=DARG[:], func=AF.Exp), r=dk, w=["DEC"])
        P.dve(lambda e: e.tensor_tensor(out=t1[:], in0=CS[:], in1=M_all[:], op=ALU.subtract), r=["CS"] + mk, w=["t1"])
        P.act(lambda e: e.activation(out=A_all[:], in_=t1[:], func=AF.Exp), r=["t1"], w=["A_all"])
        P.dve(lambda e: e.tensor_tensor(out=t2[:], in0=Bc[:], in1=M_all[:], op=ALU.add), r=["Bc"] + mk, w=["t2"])
        P.act(lambda e: e.activation(out=THR[:], in_=t2[:], func=AF.Exp, scale=-1.0), r=["t2"], w=["THR"])
        P.flush()

        mt = Rot(A, "mt", 5, [128, 4, 132], F32)
        xcT = Rot(A, "xcT", 3, [128, 4, 128], F32)
        xcb = Rot(A, "xcb", 2, [128, 4, 128], BF16)
        mb = Rot(A, "mb", 2, [128, 4, 132], BF16)
        qTb = Rot(A, "qTb", 2, [128, 4, 128], BF16)
        kTb = Rot(A, "kTb", 2, [128, 4, 128], BF16)
        kb = Rot(A, "kb", 2, [128, 4, 128], BF16)
        vab = Rot(A, "vab", 2, [128, 4, 129], BF16)
        scm = Rot(A, "scm", 2, [128, 4, 128], BF16)
        Cd = Rot(A, "Cd", 2, [128, 4, 129], F32)
        KVs = Rot(A, "KVs", 2, [128, 4, 129], F32)
        Cdb = Rot(A, "Cdb", 2, [128, 4, 129], BF16)
        Call = A("Call", [128, 4, 129], F32)
        sm = Rot(A, "sm", 2, [128, 4, 4], F32)
        hout = Rot(A, "hout", 2, [128, 4, 128], F32)
        hbt = Rot(A, "hbt", 5, [128, 4, 128], F32)
        sopt = Rot(A, "sopt", 5, [128, 4, 128], F32)
        hsq = A("hsq", [128, 4, 128], F32)
        hss = Rot(A, "hss", 2, [128, 2, 4], F32)
        ut = Rot(A, "ut", 2, [128, 4, 128], F32)
        bmst = Rot(A, "bmst", 2, [128, 4, T], BF16)
        mask = {0: triLE, 1: triGE}

        for d in (1, 0):
            P.dve(lambda e: e.memset(Call[:], 0.0), w=["Call"])
            order = range(NCH) if d == 0 else range(NCH - 1, -1, -1)
            bst = {"bms": None, "key": None}

            def chunk_body(c, d=d, bst=bst):
                t0 = c * 128
                sl = slice(d * 256 + c * 4, d * 256 + c * 4 + 4)
                m_, mkey = mt.next()
                lo, hi = max(t0 - 2, 0), min(t0 + 130, S)
                o0 = lo - (t0 - 2)
                wl = [mkey]
                if c == 0:
                    P.pool(lambda e, m_=m_: e.memset(m_[:, :, 0:2], 0.0), w=[mkey + ("z",)])
                    wl.append(mkey + ("z",))
                if c == NCH - 1:
                    P.pool(lambda e, m_=m_: e.memset(m_[:, :, 130:132], 0.0), w=[mkey + ("z",)])
                    wl.append(mkey + ("z",))
                P.dma("sp", m_[:, :, o0:o0 + (hi - lo)], scr["minT"][:, lo:hi].rearrange("(k p) t -> p k t", p=128),
                      w=[mkey], r=[mkey + ("z",)])
                mkeys = [mkey]
                if d == 0:
                    hb_, hbkey = hbt.next()
                    so_, sokey = sopt.next()
                    P.dma("sp", hb_[:].rearrange("p h e -> p (h e)"), scr["hb"][t0:t0 + 128, :], w=[hbkey])
                    P.dma("sp", so_[:], scr["sopT"][:, t0:t0 + 128].rearrange("(k p) t -> p k t", p=128), w=[sokey])
                yield
                mbb, mbkey = mb.next()
                P.pool(lambda e, mbb=mbb, m_=m_: e.tensor_copy(out=mbb[:], in_=m_[:]), r=mkeys, w=[mbkey])

                def mm_conv(e, mbb=mbb):
                    inst = None
                    for hc in range(4):
                        o_ = p0[:, hc * 128:(hc + 1) * 128]
                        for jj in range(5):
                            e.matmul(o_, lhsT=dgw[:, hc, jj, :], rhs=mbb[:, hc, jj:jj + 128], start=(jj == 0), stop=False)
                        e.matmul(o_, lhsT=brow[0:1, 0, hc * 128:(hc + 1) * 128], rhs=onesb[0:1, :], start=False, stop=False)
                        inst = e.matmul(o_, lhsT=brow[0:1, 1, hc * 128:(hc + 1) * 128], rhs=onesb[0:1, :], start=False,
                                        stop=True)
                    return inst

                P.pe(mm_conv, r=[mbkey, "dgw", "brow"], w=["p0"])
                xb, xbkey = xcb.next()
                P.act(lambda e, xb=xb: e.activation(out=xb[:].rearrange("p h t -> p (h t)"), in_=p0[:], func=AF.Silu),
                      r=["p0"], w=[xbkey])
                if d == 0:
                    xf, xfkey = xcT.next()
                    P.act(lambda e, xf=xf: e.activation(out=xf[:].rearrange("p h t -> p (h t)"), in_=p0[:],
                                                        func=AF.Silu), r=["p0"], w=[xfkey])
                qT_, qTkey = qTb.next()
                kT_, kTkey = kTb.next()
                k_, kkey = kb.next()
                va_, vakey = vab.next()

                def proj(e, wt, x, out_p, tmaj):
                    inst = None
                    for h in range(4):
                        if tmaj:
                            inst = e.matmul(out_p[:, h * 128:(h + 1) * 128], lhsT=x[:, h, :], rhs=wt[:, h, :],
                                            start=True, stop=True)
                        else:
                            inst = e.matmul(out_p[:, h * 128:(h + 1) * 128], lhsT=wt[:, h, :], rhs=x[:, h, :],
                                            start=True, stop=True)
                    return inst

                P.pe(lambda e, xb=xb: proj(e, wq, xb, p1, False), r=[xbkey, "w_mq"], w=["p1"])
                P.act(lambda e, qT_=qT_: e.copy(out=qT_[:].rearrange("p h t -> p (h t)"), in_=p1[:]), r=["p1"],
                      w=[qTkey])
                P.pe(lambda e, xb=xb: proj(e, wk, xb, p0, False), r=[xbkey, "w_mk"], w=["p0"])
                P.act(lambda e, kT_=kT_: e.mul(out=kT_[:].rearrange("p h t -> p (h t)"), in_=p0[:], mul=128 ** -0.5),
                      r=["p0"], w=[kTkey])
                P.pe(lambda e, xb=xb: proj(e, wk, xb, p1, True), r=[xbkey, "w_mk"], w=["p1"])
                P.act(lambda e, k_=k_: e.mul(out=k_[:].rearrange("p h t -> p (h t)"), in_=p1[:], mul=128 ** -0.5),
                      r=["p1"], w=[kkey])
                P.pe(lambda e, mbb=mbb: proj(e, wvv, mbb[:, :, 2:130], p0, True), r=[mbkey, "w_mv"], w=["p0"])
                P.dve(lambda e, va_=va_, sl=sl: e.tensor_tensor(
                    out=va_[:, :, 0:128], in0=p0[:].rearrange("p (h e) -> p h e", h=4),
                    in1=bc(A_all[:, sl], [128, 4, 128], 2), op=ALU.mult), r=["p0", "A_all"], w=[vakey + (0,)])
                P.dve(lambda e, va_=va_, sl=sl: e.tensor_copy(out=va_[:, :, 128:129], in_=A_all[:, sl].unsqueeze(2)),
                      r=["A_all"], w=[vakey + (1,)])
                vakeys = [vakey + (0,), vakey + (1,)]

                def mm_s(e, kT_=kT_, qT_=qT_):
                    inst = None
                    for h in range(4):
                        inst = e.matmul(pS_[:, h * 128:(h + 1) * 128], lhsT=kT_[:, h, :], rhs=qT_[:, h, :],
                                        start=True, stop=True)
                    return inst

                P.pe(mm_s, r=[kTkey, qTkey], w=["pS"])
                sc_, sckey = scm.next()
                P.dve(lambda e, sc_=sc_, d=d: e.tensor_tensor(out=sc_[:], in0=pS_[:].rearrange("p (h t) -> p h t", h=4),
                                                              in1=bc(mask[d], [128, 4, 128], 1), op=ALU.mult),
                      r=["pS", "cf"], w=[sckey])

                def mm_kv(e, k_=k_, va_=va_):
                    inst = None
                    for h in range(4):
                        inst = e.matmul(pKV[:, h * 256:h * 256 + 129], lhsT=k_[:, h, :], rhs=va_[:, h, :],
                                        start=True, stop=True)
                    return inst

                P.pe(mm_kv, r=[kkey] + vakeys, w=["pKV"])
                kv3 = pKV[:].rearrange("p (h x) -> p h x", h=4)[:, :, 0:129]
                kvs_, kvskey = KVs.next()
                P.dve(lambda e, kvs_=kvs_: e.tensor_copy(out=kvs_[:], in_=kv3), r=["pKV"], w=[kvskey])
                yield
                cd_, cdkey = Cd.next()
                cdb_, cdbkey = Cdb.next()
                P.dve(lambda e, cd_=cd_, sl=sl: e.tensor_tensor(out=cd_[:], in0=Call[:],
                                                                in1=bc(DEC[:, sl], [128, 4, 129], 2), op=ALU.mult),
                      r=["Call", "DEC"], w=[cdkey])
                P.dve(lambda e, cd_=cd_, kvs_=kvs_: e.tensor_tensor(out=Call[:], in0=cd_[:], in1=kvs_[:], op=ALU.add),
                      r=[cdkey, kvskey], w=["Call"])
                P.act(lambda e, cd_=cd_, cdb_=cdb_: e.copy(out=cdb_[:], in_=cd_[:]), r=[cdkey], w=[cdbkey])
                yield

                def mm_n(e, sc_=sc_, va_=va_, qT_=qT_, cdb_=cdb_):
                    inst = None
                    for h in range(4):
                        e.matmul(pN[:, h * 256:h * 256 + 129], lhsT=sc_[:, h, :], rhs=va_[:, h, :], start=True,
                                 stop=False)
                        inst = e.matmul(pN[:, h * 256:h * 256 + 129], lhsT=qT_[:, h, :], rhs=cdb_[:, h, :],
                                        start=False, stop=True)
                    return inst

                P.pe(mm_n, r=[sckey, qTkey, cdbkey] + vakeys, w=["pN"])
                n3 = pN[:].rearrange("p (h x) -> p h x", h=4)
                yield
                s_, skey = sm.next()
                den = n3[:, :, 128]
                P.dve(lambda e, s_=s_: e.tensor_copy(out=s_[:, 3, :], in_=den), r=["pN"], w=[skey + (3,)])
                P.dve(lambda e, s_=s_: e.scalar_tensor_tensor(out=s_[:, 0, :], in0=s_[:, 3, :], scalar=-1.0,
                                                              in1=s_[:, 3, :], op0=ALU.mult, op1=ALU.max),
                      r=[skey + (3,)], w=[skey + (0,)])
                P.dve(lambda e, s_=s_, sl=sl: e.tensor_tensor(out=s_[:, 1, :], in0=s_[:, 0, :], in1=THR[:, sl],
                                                              op=ALU.max), r=[skey + (0,), "THR"], w=[skey + (1,)])
                P.dve(lambda e, s_=s_: e.reciprocal(out=s_[:, 2, :], in_=s_[:, 1, :]), r=[skey + (1,)],
                      w=[skey + (2,)])
                ho, hokey = hout.next()
                P.dve(lambda e, ho=ho, s_=s_: e.tensor_tensor(out=ho[:], in0=n3[:, :, 0:128],
                                                              in1=bc(s_[:, 2, :], [128, 4, 128], 2), op=ALU.mult),
                      r=["pN", skey + (2,)], w=[hokey])
                if d == 1:
                    P.dma("sp", scr["hb"][t0:t0 + 128, :], ho[:].rearrange("p h e -> p (h e)"), r=[hokey])
                    return
                P.pool(lambda e, ho=ho, hb_=hb_: e.tensor_tensor(out=hb_[:], in0=ho[:], in1=hb_[:], op=ALU.add),
                       r=[hokey, hbkey], w=[hbkey])
                P.pool(lambda e, hb_=hb_: e.tensor_tensor(out=hsq[:], in0=hb_[:], in1=hb_[:], op=ALU.mult),
                       r=[hbkey], w=["hsq"])
                hs_, hskey = hss.next()
                P.dve(lambda e, hs_=hs_: e.tensor_reduce(out=hs_[:, 0, :], in_=hsq[:], axis=AX.X, op=ALU.add),
                      r=["hsq"], w=[hskey + (0,)])
                rstd_pool(P, hs_[:, 1, :], hs_[:, 0, :], 128, epsb[:, 0:4], [hskey + (0,)], [hskey + (1,)])
                P.pool(lambda e, hb_=hb_, hs_=hs_: e.tensor_tensor(out=hb_[:], in0=hb_[:],
                                                                   in1=bc(hs_[:, 1, :], [128, 4, 128], 2),
                                                                   op=ALU.mult), r=[hbkey, hskey + (1,)], w=[hbkey])

                def trh(e, hb_=hb_):
                    inst = None
                    for h in range(4):
                        inst = e.transpose(pH[:, h * 128:(h + 1) * 128], hb_[:, h, :], identf)
                    return inst

                P.pe(trh, r=[hbkey, "cf"], w=["pH"])
                u_, ukey = ut.next()
                for h in range(4):
                    P.act(lambda e, u_=u_, h=h: e.activation(out=u_[:, h, :], in_=pH[:, h * 128:(h + 1) * 128],
                                                             func=AF.Identity, scale=mnw[:, h:h + 1]),
                          r=["pH", "mnw"], w=[ukey + (h,)])
                    P.dve(lambda e, u_=u_, h=h, xf=xf: e.scalar_tensor_tensor(
                        out=u_[:, h, :], in0=xf[:, h, :], scalar=msk[:, h:h + 1], in1=u_[:, h, :], op0=ALU.mult,
                        op1=ALU.add), r=[ukey + (h,), xfkey, "msk"], w=[ukey + (h,)])
                if c % 4 == 0:
                    bst["bms"], bst["key"] = bmst.next()
                bms, bmskey = bst["bms"], bst["key"]
                cj = c % 4
                P.dve(lambda e, u_=u_, so_=so_, bms=bms, cj=cj: e.tensor_tensor(
                    out=bms[:, :, cj * 128:(cj + 1) * 128], in0=u_[:], in1=so_[:], op=ALU.mult),
                    r=[ukey + (h,) for h in range(4)] + [sokey], w=[bmskey + (cj,)])
                if cj == 3:
                    tt0 = (c - 3) * 128
                    P.dma("sp", scr["bmT"][:, tt0:tt0 + T].rearrange("(k p) t -> p k t", p=128), bms[:],
                          r=[bmskey + (q,) for q in range(4)])

            order = list(order)
            n_ = len(order)
            gens = {c: chunk_body(c) for c in order}
            LA = 3
            for k in range(min(LA, n_)):
                next(gens[order[k]])
            next(gens[order[0]])
            for t in range(n_ + 1):
                if t < n_:
                    next(gens[order[t]])
                if t >= 1:
                    for _ in gens.pop(order[t - 1]):
                        pass
                if t < n_:
                    next(gens[order[t]])
                if t + 1 < n_:
                    next(gens[order[t + 1]])
                if t + LA < n_:
                    next(gens[order[t + LA]])
            P.flush()


def phase_merge(P, nc, x1s, w, scr, cins):
    name = "mg"
    with ExitStack() as st:
        def A(nm, shape, dt):
            return st.enter_context(nc.sbuf_tensor(f"{name}_{nm}", shape, dt))

        def PS(nm, shape, dt):
            return st.enter_context(nc.psum_tensor(f"{name}_{nm}", shape, dt))

        wa = A("wa", [128, 4, D], BF16)
        wb = A("wb", [128, 4, D], BF16)
        wo = A("wo", [128, 8, D], BF16)
        xt = Rot(A, "xt", 2, [128, 4, D], F32)
        at = Rot(A, "at", 2, [128, 4, T], BF16)
        bt = Rot(A, "bt", 2, [128, 4, T], BF16)
        sga = Rot(A, "sga", 2, [128, 8, T], F32)
        sgb = Rot(A, "sgb", 2, [128, 8, T], F32)
        mT = Rot(A, "mT", 2, [128, 8, T], BF16)
        ta = Rot(A, "ta", 2, [128, T], F32)
        tb = Rot(A, "tb", 2, [128, T], F32)
        pA_ = [PS(f"pA{b}", [128, T], F32) for b in range(2)]
        pB_ = [PS(f"pB{b}", [128, T], F32) for b in range(2)]
        pD = [PS(f"pD{b}", [128, T], F32) for b in range(2)]
        P.dma("pool", wa[:], w["w_branch_a"].rearrange("(k p) n -> p k n", p=128), w=["wa"])
        P.dma("pool", wb[:], w["w_branch_b"].rearrange("(k p) n -> p k n", p=128), w=["wb"])
        P.dma("pool", wo[:], w["w_out"].rearrange("(k p) n -> p k n", p=128), w=["wo"])

        def xview(ap, i):
            return ap[i * T:(i + 1) * T, :].rearrange("(j p) d -> p j d", p=128)

        bufs = {}

        def load(i):
            t0 = i * T
            x_, xk = xt.next()
            a_, ak = at.next()
            b_, bk = bt.next()
            ga_, gak = sga.next()
            gb_, gbk = sgb.next()
            P.dma("sp", a_[:], scr["aT"][:, t0:t0 + T].rearrange("(k p) t -> p k t", p=128), w=[ak])
            P.dma("sp", b_[:], scr["bmT"][:, t0:t0 + T].rearrange("(k p) t -> p k t", p=128), w=[bk])
            P.dma("sp", ga_[:], scr["sgT"][0:1024, t0:t0 + T].rearrange("(k p) t -> p k t", p=128), w=[gak])
            P.dma("sp", gb_[:], scr["sgT"][1024:2048, t0:t0 + T].rearrange("(k p) t -> p k t", p=128), w=[gbk])
            P.dma("sp", x_[:], xview(x1s, i), w=[xk + (j,) for j in range(4)])
            bufs[i] = (x_, xk, a_, ak, b_, bk, ga_, gak, gb_, gbk)

        load(0)
        na = 0
        nd = 0
        for i in range(NT):
            if i + 1 < NT:
                load(i + 1)
            x_, xk, a_, ak, b_, bk, ga_, gak, gb_, gbk = bufs.pop(i)
            m_, mk = mT.next()
            for cc in range(8):
                pa, pb = pA_[na % 2], pB_[na % 2]
                pak, pbk = ("pA", na % 2), ("pB", na % 2)
                na += 1

                def mm_a(e, pa=pa, cc=cc, a_=a_):
                    inst = None
                    for k in range(4):
                        inst = e.matmul(pa[:], lhsT=wa[:, k, cc * 128:(cc + 1) * 128], rhs=a_[:, k, :],
                                        start=(k == 0), stop=(k == 3))
                    return inst

                def mm_b(e, pb=pb, cc=cc, b_=b_):
                    inst = None
                    for k in range(4):
                        inst = e.matmul(pb[:], lhsT=wb[:, k, cc * 128:(cc + 1) * 128], rhs=b_[:, k, :],
                                        start=(k == 0), stop=(k == 3))
                    return inst

                P.pe(mm_a, r=[ak, "wa"], w=[pak])
                P.pe(mm_b, r=[bk, "wb"], w=[pbk])
                ta_, tak = ta.next()
                tb_, tbk = tb.next()
                P.dve(lambda e, ta_=ta_, pa=pa, ga_=ga_, cc=cc: e.tensor_tensor(out=ta_[:], in0=pa[:],
                                                                                in1=ga_[:, cc, :], op=ALU.mult),
                      r=[pak, gak], w=[tak])
                P.dve(lambda e, tb_=tb_, pb=pb, gb_=gb_, cc=cc: e.tensor_tensor(out=tb_[:], in0=pb[:],
                                                                                in1=gb_[:, cc, :], op=ALU.mult),
                      r=[pbk, gbk], w=[tbk])
                P.pool(lambda e, ta_=ta_, tb_=tb_, m_=m_, cc=cc: e.tensor_tensor(out=m_[:, cc, :], in0=ta_[:],
                                                                                 in1=tb_[:], op=ALU.add),
                       r=[tak, tbk], w=[mk + (cc,)])
            mkeys = [mk + (cc,) for cc in range(8)]
            for j in range(4):
                for half in range(2):
                    d_ = pD[nd % 2]
                    dk_ = ("pD", nd % 2)
                    nd += 1

                    def mm_o(e, d_=d_, j=j, half=half, m_=m_):
                        inst = None
                        for k in range(8):
                            inst = e.matmul(d_[:], lhsT=m_[:, k, j * 128:(j + 1) * 128],
                                            rhs=wo[:, k, half * 512:(half + 1) * 512], start=(k == 0), stop=(k == 7))
                        return inst

                    P.pe(mm_o, r=mkeys + ["wo"], w=[dk_])
                    xs = x_[:, j, half * 512:(half + 1) * 512]
                    P.dve(lambda e, d_=d_, xs=xs: e.tensor_tensor(out=xs, in0=d_[:], in1=xs, op=ALU.add),
                          r=[dk_, xk + (j,)], w=[xk + (j,)])
            P.dma("sp", xview(x1s, i), x_[:], r=[xk + (j,) for j in range(4)])
        P.flush()


W_NAMES = ["ffn1_norm_w", "ffn1_w_gate", "ffn1_w_up", "ffn1_w_down", "mix_norm_w", "w_in", "q_a_norm_w", "w_uq",
           "kv_a_norm_w", "w_uk", "w_uv", "q_norm_w", "k_norm_w", "w_branch_a", "conv_w", "conv_b", "w_mq", "w_mk",
           "w_mv", "b_igate", "b_fgate", "m_norm_w", "m_skip", "w_branch_b", "w_out", "ffn2_norm_w", "ffn2_w_gate",
           "ffn2_w_up", "ffn2_w_down", "final_norm_w"]
W_SHAPES = {
    "ffn1_norm_w": [D], "ffn1_w_gate": [D, FF], "ffn1_w_up": [D, FF], "ffn1_w_down": [FF, D], "mix_norm_w": [D],
    "w_in": [D, IN_DIM], "q_a_norm_w": [256], "w_uq": [256, 768], "kv_a_norm_w": [128], "w_uk": [128, 512],
    "w_uv": [128, 512], "q_norm_w": [96], "k_norm_w": [96], "w_branch_a": [512, D], "conv_w": [5, 512],
    "conv_b": [512], "w_mq": [4, 128, 128], "w_mk": [4, 128, 128], "w_mv": [4, 128, 128], "b_igate": [8],
    "b_fgate": [8], "m_norm_w": [512], "m_skip": [512], "w_branch_b": [512, D], "w_out": [D, D],
    "ffn2_norm_w": [D], "ffn2_w_gate": [D, FF], "ffn2_w_up": [D, FF], "ffn2_w_down": [FF, D], "final_norm_w": [D],
}


def build_program(phases=("ffn1", "inproj", "attn", "mlstm", "merge", "ffn2"), debug=()):
    nc = bass.Bass("TRN2", target_bir_lowering=False)
    x = nc.dram_tensor("x", [S, D], F32, kind="ExternalInput").ap()
    pos = nc.dram_tensor("positions", [S], I32, kind="ExternalInput").ap()
    w = {k: nc.dram_tensor(k, W_SHAPES[k], F32, kind="ExternalInput").ap() for k in W_NAMES}
    cins = {
        "c_ident": nc.dram_tensor("c_ident", [128, 128], BF16, kind="ExternalInput").ap(),
        "c_f32": nc.dram_tensor("c_f32", [128, 512], F32, kind="ExternalInput").ap(),
        "c_inv": nc.dram_tensor("c_inv", [16], F32, kind="ExternalInput").ap(),
    }
    out = nc.dram_tensor("out", [S, D], F32, kind="ExternalOutput").ap()

    def scratch(nm, shape, dt):
        kind = "ExternalOutput" if nm in debug else "Internal"
        return nc.dram_tensor("s_" + nm, shape, dt, kind=kind).ap()

    x1s = scratch("x1", [S, D], F32)
    scr = {
        "sgT": scratch("sgT", [2048, S], F32),
        "minT": scratch("minT", [512, S], F32),
        "sopT": scratch("sopT", [512, S], F32),
        "gates": scratch("gates", [S, 16], F32),
        "qkT": scratch("qkT", [16, 96, S], BF16),
        "vS": scratch("vS", [S, 512], BF16),
        "aT": scratch("aT", [512, S], BF16),
        "bmT": scratch("bmT", [512, S], BF16),
        "hb": scratch("hb", [S, 512], F32),
    }
    with ExitStack() as top:
        sems = [top.enter_context(nc.semaphore(f"sem{i}")) for i in range(96)]
        P = Prog(nc, sems)
        if "ffn1" in phases:
            phase_ffn(P, nc, "f1", x, x1s, w["ffn1_norm_w"], w["ffn1_w_gate"], w["ffn1_w_up"], w["ffn1_w_down"], cins)
        if "inproj" in phases:
            phase_inproj(P, nc, x1s, pos, w, scr, cins)
        if "attn" in phases:
            phase_attn(P, nc, scr, cins)
        if "mlstm" in phases:
            phase_mlstm(P, nc, w, scr, cins)
        if "merge" in phases:
            phase_merge(P, nc, x1s, w, scr, cins)
        if "ffn2" in phases:
            phase_ffn(P, nc, "f2", x1s, out, w["ffn2_norm_w"], w["ffn2_w_gate"], w["ffn2_w_up"], w["ffn2_w_down"],
                      cins, final_w=w["final_norm_w"])
    return nc


def make_consts():
    ident = np.eye(128, dtype=np.float32)
    s_idx = np.arange(128)[:, None]
    t_idx = np.arange(128)[None, :]
    cf = np.concatenate([ident, (s_idx <= t_idx).astype(np.float32), (s_idx >= t_idx).astype(np.float32),
                         np.ones((128, 128), np.float32)], axis=1)
    half = 16
    inv = (np.float32(10000.0) ** (-np.arange(half, dtype=np.float32) / np.float32(half))).astype(np.float32)
    return {"c_ident": ident.astype(ml_dtypes.bfloat16), "c_f32": np.ascontiguousarray(cf), "c_inv": inv}


def make_in_maps(inputs, n_cores=8):
    consts = make_consts()
    maps = []
    for b in range(n_cores):
        m = {"x": np.ascontiguousarray(inputs["x"][b]), "positions": np.ascontiguousarray(inputs["positions"][b])}
        for k in W_NAMES:
            m[k] = np.ascontiguousarray(np.asarray(inputs[k])[0])
        m.update(consts)
        maps.append(m)
    return maps


def kernel(**inputs):
    inputs = {k: np.asarray(v) for k, v in inputs.items()}
    nc = build_program()
    in_maps = make_in_maps(inputs, 8)
    res = run_bass_kernel_spmd(nc, in_maps, core_ids=list(range(8)))
    return np.stack([np.asarray(r["out"]) for r in res.results], axis=0).astype(np.float32)
```
